# Optimizing a Trainium2 kernel written in Bass

```python
import jax
import jax.numpy as jnp
from jax import lax
import numpy as np

D_MODEL = 1024
BATCH = 4
SEQ = 4096
DEPTH = 2

GRID_W = 64
CTX_LEN = 256
HEAD_DIM = 64
QBLK = 128
ROPE_THETA = 10000.0
LN_EPS = 1e-5
RMS_EPS = 1e-6
NEG_INF = -1e30

CONV_CH = 512
CONV_WIDTH = 31
WIN_HEADS = 8
WIN_KV_HEADS = 2
WIN_GROUP = WIN_HEADS // WIN_KV_HEADS
WINDOW = 128
GQA_HEADS = 8
GQA_KV_HEADS = 2
GQA_GROUP = GQA_HEADS // GQA_KV_HEADS
MLA_HEADS = 8
MLA_Q_RANK = 256
MLA_KV_RANK = 128
MLA_NOPE = 64
MLA_ROPE = 32
MLA_V = 64
N_GROUPS = 4
EXP_PER_GROUP = 8
N_EXPERTS = N_GROUPS * EXP_PER_GROUP
EXPERT_FF = 512
TOP_K = 2

N_EVEN = (DEPTH + 1) // 2
N_ODD = DEPTH // 2
DN_ALPHA = float((2 * DEPTH) ** 0.25)
DN_BETA = float((8 * DEPTH) ** -0.25)

EVEN_CUTS = (CONV_CH, 2 * CONV_CH, 2 * CONV_CH + WIN_HEADS * HEAD_DIM,
             2 * CONV_CH + (WIN_HEADS + WIN_KV_HEADS) * HEAD_DIM)
EVEN_KV_START = EVEN_CUTS[2]
EVEN_IN = 2 * CONV_CH + (WIN_HEADS + 2 * WIN_KV_HEADS) * HEAD_DIM
EVEN_MIX = CONV_CH + WIN_HEADS * HEAD_DIM
ODD_CUTS = (GQA_HEADS * HEAD_DIM,
            GQA_HEADS * HEAD_DIM + MLA_Q_RANK,
            GQA_HEADS * HEAD_DIM + MLA_Q_RANK + GQA_KV_HEADS * HEAD_DIM,
            GQA_HEADS * HEAD_DIM + MLA_Q_RANK + 2 * GQA_KV_HEADS * HEAD_DIM,
            GQA_HEADS * HEAD_DIM + MLA_Q_RANK + 2 * GQA_KV_HEADS * HEAD_DIM + MLA_KV_RANK)
ODD_KV_START = ODD_CUTS[1]
ODD_KV_CUTS = tuple(cut - ODD_KV_START for cut in ODD_CUTS[2:])
ODD_IN = ODD_CUTS[4] + MLA_ROPE
ODD_MIX = GQA_HEADS * HEAD_DIM + MLA_HEADS * MLA_V

kernel_name = 'hybrid_conv_window_mla_hmoe_dit'


def _layer_norm(x, g, b):
    xf = x.astype(jnp.float32)
    mu = jnp.mean(xf, axis=-1, keepdims=True)
    var = jnp.mean(jnp.square(xf - mu), axis=-1, keepdims=True)
    y = (xf - mu) * lax.rsqrt(var + LN_EPS) * g.astype(jnp.float32) + b.astype(jnp.float32)
    return y.astype(x.dtype)


def _rms_norm(x, g):
    xf = x.astype(jnp.float32)
    y = xf * lax.rsqrt(jnp.mean(jnp.square(xf), axis=-1, keepdims=True) + RMS_EPS) * g.astype(jnp.float32)
    return y.astype(x.dtype)


def _axial_tables(row, col, rot_dim):
    axis_dim = rot_dim // 2
    inv_freq = ROPE_THETA ** (-jnp.arange(0, axis_dim, 2, dtype=jnp.float32) / axis_dim)
    ang_r = row.astype(jnp.float32)[:, None] * inv_freq[None, :]
    ang_c = col.astype(jnp.float32)[:, None] * inv_freq[None, :]
    return (jnp.cos(ang_r), jnp.sin(ang_r), jnp.cos(ang_c), jnp.sin(ang_c))


def _rotate_half(x, cos, sin):
    x1, x2 = jnp.split(x, 2, axis=-1)
    return jnp.concatenate([x1 * cos - x2 * sin, x1 * sin + x2 * cos], axis=-1)


def _axial_rope(x, tables):
    n_mid = x.ndim - 3
    cos_r, sin_r, cos_c, sin_c = (t.reshape((t.shape[0],) + (1,) * n_mid + (t.shape[1],)).astype(x.dtype)
                                  for t in tables)
    x_r, x_c = jnp.split(x, 2, axis=-1)
    return jnp.concatenate([_rotate_half(x_r, cos_r, sin_r), _rotate_half(x_c, cos_c, sin_c)], axis=-1)


def _softmax_with_sink(s, sink):
    sink = jnp.broadcast_to(sink.astype(jnp.float32), s.shape[:-1] + (1,))
    return jax.nn.softmax(jnp.concatenate([sink, s], axis=-1), axis=-1)[..., 1:]


def _sweep_query_blocks(fn, *qs):
    bsz, n_q = qs[0].shape[:2]
    n_blk = n_q // QBLK
    blocks = tuple(jnp.moveaxis(q.reshape((bsz, n_blk, QBLK) + q.shape[2:]), 1, 0) for q in qs)
    out = lax.map(lambda blk: fn(*blk), blocks)
    return jnp.moveaxis(out, 0, 1).reshape((bsz, n_q) + out.shape[3:])


def _conv_module(a_val, a_gate, conv_w, conv_b, ln_g, ln_b):
    h = a_val * jax.nn.sigmoid(a_gate)
    h = lax.conv_general_dilated(h, conv_w, window_strides=(1,),
                                 padding=[(CONV_WIDTH // 2, CONV_WIDTH // 2)],
                                 dimension_numbers=('NWC', 'WIO', 'NWC'),
                                 feature_group_count=CONV_CH) + conv_b
    return jax.nn.silu(_layer_norm(h, ln_g, ln_b))


def _window_attention(q, k, v, k_ctx, v_ctx, sink):
    bsz, n, n_kv, n_g, hd = q.shape
    n_blk = n // WINDOW
    n_ctx = k_ctx.shape[1]
    scale = hd ** -0.5
    qb = q.reshape(bsz, n_blk, WINDOW, n_kv, n_g, hd)
    pad = ((0, 0), (WINDOW, WINDOW), (0, 0), (0, 0))

    def band(t):
        tb = jnp.pad(t, pad).reshape(bsz, n_blk + 2, WINDOW, n_kv, hd)
        return jnp.concatenate([tb[:, :-2], tb[:, 1:-1], tb[:, 2:]], axis=2)

    kb, vb = band(k), band(v)
    q_pos = (jnp.arange(n_blk) * WINDOW)[:, None, None] + jnp.arange(WINDOW)[None, :, None]
    k_pos = (jnp.arange(n_blk) * WINDOW - WINDOW)[:, None, None] + jnp.arange(3 * WINDOW)[None, None, :]
    valid = (jnp.abs(q_pos - k_pos) <= WINDOW) & (k_pos >= 0) & (k_pos < n)
    s_loc = jnp.einsum('bnqhgd,bnkhd->bnhgqk', qb, kb).astype(jnp.float32) * scale
    s_loc = jnp.where(valid[None, :, None, None], s_loc, NEG_INF)
    s_ctx = jnp.einsum('bnqhgd,bchd->bnhgqc', qb, k_ctx).astype(jnp.float32) * scale
    p = _softmax_with_sink(jnp.concatenate([s_ctx, s_loc], axis=-1), sink[None, None, :, :, None, None])
    p = p.astype(v.dtype)
    o = (jnp.einsum('bnhgqc,bchd->bnqhgd', p[..., :n_ctx], v_ctx)
         + jnp.einsum('bnhgqk,bnkhd->bnqhgd', p[..., n_ctx:], vb))
    return o.reshape(bsz, n, n_kv * n_g * hd)


def _context_sink_attention(q, k, v, sink):
    bsz, n, n_kv, n_g, hd = q.shape
    s = jnp.einsum('bqhgd,bkhd->bhgqk', q, k).astype(jnp.float32) * hd ** -0.5
    p = _softmax_with_sink(s, sink[None, :, :, None, None]).astype(v.dtype)
    return jnp.einsum('bhgqk,bkhd->bqhgd', p, v).reshape(bsz, n, n_kv * n_g * hd)


def _dense_gqa(q, k, v):
    scale = q.shape[-1] ** -0.5

    def block(qb):
        s = jnp.einsum('bqhgd,bkhd->bhgqk', qb, k).astype(jnp.float32) * scale
        p = jax.nn.softmax(s, axis=-1).astype(v.dtype)
        return jnp.einsum('bhgqk,bkhd->bqhgd', p, v)

    return _sweep_query_blocks(block, q)


def _dense_mla(q_nope, q_rope, k_nope, k_rope, v):
    scale = (MLA_NOPE + MLA_ROPE) ** -0.5

    def block(qn, qr):
        s = (jnp.einsum('bqhd,bkhd->bhqk', qn, k_nope)
             + jnp.einsum('bqhr,bkr->bhqk', qr, k_rope)).astype(jnp.float32) * scale
        p = jax.nn.softmax(s, axis=-1).astype(v.dtype)
        return jnp.einsum('bhqk,bkhd->bqhd', p, v)

    return _sweep_query_blocks(block, q_nope, q_rope)


def _even_mixer(u, uc, w_in, b_in, conv_w, conv_b, conv_ln_g, conv_ln_b, sink, w_out, b_out,
                tabs_hd, need_ctx):
    bsz, n, _ = u.shape
    n_ctx = uc.shape[1]
    sink_hg = sink.reshape(WIN_KV_HEADS, WIN_GROUP)
    a_val, a_gate, q, k, v = jnp.split(u @ w_in + b_in, EVEN_CUTS, axis=-1)
    conv_out = _conv_module(a_val, a_gate, conv_w, conv_b, conv_ln_g, conv_ln_b)
    q = _axial_rope(q.reshape(bsz, n, WIN_KV_HEADS, WIN_GROUP, HEAD_DIM), tabs_hd)
    k = _axial_rope(k.reshape(bsz, n, WIN_KV_HEADS, HEAD_DIM), tabs_hd)
    v = v.reshape(bsz, n, WIN_KV_HEADS, HEAD_DIM)
    if need_ctx:
        ac_val, ac_gate, qc, kc, vc = jnp.split(uc @ w_in + b_in, EVEN_CUTS, axis=-1)
    else:
        kc, vc = jnp.split(uc @ w_in[:, EVEN_KV_START:] + b_in[EVEN_KV_START:], 2, axis=-1)
    kc = kc.reshape(bsz, n_ctx, WIN_KV_HEADS, HEAD_DIM)
    vc = vc.reshape(bsz, n_ctx, WIN_KV_HEADS, HEAD_DIM)
    attn = _window_attention(q, k, v, kc, vc, sink_hg)
    y = jnp.concatenate([conv_out, attn], axis=-1) @ w_out + b_out
    if not need_ctx:
        return y, None
    conv_c = _conv_module(ac_val, ac_gate, conv_w, conv_b, conv_ln_g, conv_ln_b)
    attn_c = _context_sink_attention(qc.reshape(bsz, n_ctx, WIN_KV_HEADS, WIN_GROUP, HEAD_DIM), kc, vc, sink_hg)
    yc = jnp.concatenate([conv_c, attn_c], axis=-1) @ w_out + b_out
    return y, yc


def _odd_queries(q_g, q_c, q_norm, mla_q_norm, w_uq, tabs_hd, tabs_mla):
    bsz, n, _ = q_g.shape
    q = _rms_norm(q_g.reshape(bsz, n, GQA_KV_HEADS, GQA_GROUP, HEAD_DIM), q_norm)
    qm = (_rms_norm(q_c, mla_q_norm) @ w_uq).reshape(bsz, n, MLA_HEADS, MLA_NOPE + MLA_ROPE)
    q_nope, q_rope = qm[..., :MLA_NOPE], qm[..., MLA_NOPE:]
    if tabs_hd is not None:
        q = _axial_rope(q, tabs_hd)
        q_rope = _axial_rope(q_rope, tabs_mla)
    return q, q_nope, q_rope


def _odd_keys(k_g, v_g, kv_c, k_r, k_norm, mla_kv_norm, w_ukv, tabs_hd, tabs_mla):
    bsz, n, _ = k_g.shape
    k = _rms_norm(k_g.reshape(bsz, n, GQA_KV_HEADS, HEAD_DIM), k_norm)
    v = v_g.reshape(bsz, n, GQA_KV_HEADS, HEAD_DIM)
    kv = (_rms_norm(kv_c, mla_kv_norm) @ w_ukv).reshape(bsz, n, MLA_HEADS, MLA_NOPE + MLA_V)
    k_nope, v_m = kv[..., :MLA_NOPE], kv[..., MLA_NOPE:]
    if tabs_hd is not None:
        k = _axial_rope(k, tabs_hd)
        k_r = _axial_rope(k_r, tabs_mla)
    return k, v, k_nope, k_r, v_m


def _odd_mixer(u, uc, w_in, b_in, q_norm, k_norm, mla_q_norm, mla_kv_norm, w_uq, w_ukv, w_out, b_out,
               tabs_hd, tabs_mla, need_ctx):
    bsz, n, _ = u.shape
    q_g, q_c, k_g, v_g, kv_c, k_r = jnp.split(u @ w_in + b_in, ODD_CUTS, axis=-1)
    q, q_nope, q_rope = _odd_queries(q_g, q_c, q_norm, mla_q_norm, w_uq, tabs_hd, tabs_mla)
    k, v, k_nope, k_rope, v_m = _odd_keys(k_g, v_g, kv_c, k_r, k_norm, mla_kv_norm, w_ukv, tabs_hd, tabs_mla)
    if need_ctx:
        qc_g, qc_c, kc_g, vc_g, kvc_c, kc_r = jnp.split(uc @ w_in + b_in, ODD_CUTS, axis=-1)
    else:
        kc_g, vc_g, kvc_c, kc_r = jnp.split(uc @ w_in[:, ODD_KV_START:] + b_in[ODD_KV_START:], ODD_KV_CUTS, axis=-1)
    kc, vc, kc_nope, kc_rope, vc_m = _odd_keys(kc_g, vc_g, kvc_c, kc_r, k_norm, mla_kv_norm, w_ukv, None, None)
    o_g = _dense_gqa(q, jnp.concatenate([k, kc], axis=1), jnp.concatenate([v, vc], axis=1))
    o_m = _dense_mla(q_nope, q_rope, jnp.concatenate([k_nope, kc_nope], axis=1),
                     jnp.concatenate([k_rope, kc_rope], axis=1), jnp.concatenate([v_m, vc_m], axis=1))
    y = jnp.concatenate([o_g.reshape(bsz, n, -1), o_m.reshape(bsz, n, -1)], axis=-1) @ w_out + b_out
    if not need_ctx:
        return y, None
    n_ctx = uc.shape[1]
    qc, qc_nope, qc_rope = _odd_queries(qc_g, qc_c, q_norm, mla_q_norm, w_uq, None, None)
    oc_g = _dense_gqa(qc, kc, vc)
    oc_m = _dense_mla(qc_nope, qc_rope, kc_nope, kc_rope, vc_m)
    yc = jnp.concatenate([oc_g.reshape(bsz, n_ctx, -1), oc_m.reshape(bsz, n_ctx, -1)], axis=-1) @ w_out + b_out
    return y, yc


def _hier_moe(h, w_rg, b_rg, w_re, b_re, w1, w3, w2):
    p_grp = jax.nn.softmax((h @ w_rg + b_rg).astype(jnp.float32), axis=-1)
    g_w, g_idx = lax.top_k(p_grp, 1)
    e_logit = (jnp.einsum('td,gde->tge', h, w_re) + b_re).astype(jnp.float32)
    e_logit = jnp.take_along_axis(e_logit, g_idx[:, :, None], axis=1)[:, 0]
    e_val, e_idx = lax.top_k(e_logit, TOP_K)
    e_w = jax.nn.softmax(e_val, axis=-1) * g_w
    in_group = jnp.sum(jax.nn.one_hot(e_idx, EXP_PER_GROUP, dtype=jnp.float32) * e_w[..., None], axis=1)
    out = jnp.zeros_like(h)
    for g in range(N_GROUPS):
        sl = slice(g * EXP_PER_GROUP, (g + 1) * EXP_PER_GROUP)
        w_tok = jnp.where(g_idx == g, in_group, 0.0).astype(h.dtype)
        hid = jax.nn.silu(jnp.einsum('td,edf->tef', h, w1[sl])) * jnp.einsum('td,edf->tef', h, w3[sl])
        out = out + jnp.einsum('tef,efd->td', hid * w_tok[:, :, None], w2[sl])
    return out


def setup_inputs(seed: int = 0) -> dict:
    key = jax.random.key(seed)
    keys = iter(jax.random.split(key, 64))

    def nrm(shape, scale):
        return scale * jax.random.normal(next(keys), shape, dtype=jnp.float32)

    def gain(shape):
        return 1.0 + nrm(shape, 0.02)

    d = D_MODEL
    return {
        'x': nrm((BATCH, SEQ, d), 1.0),
        'c': nrm((BATCH, d), 1.0),
        'ctx': nrm((BATCH, CTX_LEN, d), 1.0),
        'c_ctx': nrm((d,), 1.0),
        'even_w_in': nrm((N_EVEN, d, EVEN_IN), d ** -0.5),
        'even_b_in': nrm((N_EVEN, EVEN_IN), 0.02),
        'even_conv_w': nrm((N_EVEN, CONV_WIDTH, 1, CONV_CH), CONV_WIDTH ** -0.5),
        'even_conv_b': nrm((N_EVEN, CONV_CH), 0.02),
        'even_conv_ln_g': gain((N_EVEN, CONV_CH)),
        'even_conv_ln_b': nrm((N_EVEN, CONV_CH), 0.02),
        'even_sink': nrm((N_EVEN, WIN_HEADS), 0.5),
        'even_w_out': nrm((N_EVEN, EVEN_MIX, d), DN_BETA * EVEN_MIX ** -0.5),
        'even_b_out': nrm((N_EVEN, d), 0.02),
        'odd_w_in': nrm((N_ODD, d, ODD_IN), d ** -0.5),
        'odd_b_in': nrm((N_ODD, ODD_IN), 0.02),
        'odd_q_norm': gain((N_ODD, HEAD_DIM)),
        'odd_k_norm': gain((N_ODD, HEAD_DIM)),
        'odd_mla_q_norm': gain((N_ODD, MLA_Q_RANK)),
        'odd_mla_kv_norm': gain((N_ODD, MLA_KV_RANK)),
        'odd_mla_w_uq': nrm((N_ODD, MLA_Q_RANK, MLA_HEADS * (MLA_NOPE + MLA_ROPE)), MLA_Q_RANK ** -0.5),
        'odd_mla_w_ukv': nrm((N_ODD, MLA_KV_RANK, MLA_HEADS * (MLA_NOPE + MLA_V)), MLA_KV_RANK ** -0.5),
        'odd_w_out': nrm((N_ODD, ODD_MIX, d), DN_BETA * ODD_MIX ** -0.5),
        'odd_b_out': nrm((N_ODD, d), 0.02),
        'ada_w': nrm((DEPTH, d, 6 * d), 0.5 * d ** -0.5),
        'ada_b': nrm((DEPTH, 6 * d), 0.02),
        'ln1_g': gain((DEPTH, d)),
        'ln1_b': nrm((DEPTH, d), 0.02),
        'ln2_g': gain((DEPTH, d)),
        'ln2_b': nrm((DEPTH, d), 0.02),
        'moe_w_rg': nrm((DEPTH, d, N_GROUPS), d ** -0.5),
        'moe_b_rg': nrm((DEPTH, N_GROUPS), 0.01),
        'moe_w_re': nrm((DEPTH, N_GROUPS, d, EXP_PER_GROUP), d ** -0.5),
        'moe_b_re': nrm((DEPTH, N_GROUPS, EXP_PER_GROUP), 0.01),
        'moe_w1': nrm((DEPTH, N_EXPERTS, d, EXPERT_FF), d ** -0.5),
        'moe_w3': nrm((DEPTH, N_EXPERTS, d, EXPERT_FF), d ** -0.5),
        'moe_w2': nrm((DEPTH, N_EXPERTS, EXPERT_FF, d), DN_BETA * EXPERT_FF ** -0.5),
    }


def reference(x, c, ctx, c_ctx,
              even_w_in, even_b_in, even_conv_w, even_conv_b, even_conv_ln_g, even_conv_ln_b, even_sink,
              even_w_out, even_b_out,
              odd_w_in, odd_b_in, odd_q_norm, odd_k_norm, odd_mla_q_norm, odd_mla_kv_norm, odd_mla_w_uq,
              odd_mla_w_ukv, odd_w_out, odd_b_out,
              ada_w, ada_b, ln1_g, ln1_b, ln2_g, ln2_b,
              moe_w_rg, moe_b_rg, moe_w_re, moe_b_re, moe_w1, moe_w3, moe_w2):
    bsz, n_lat, d = x.shape
    n_rows = n_lat // GRID_W
    row = jnp.repeat(jnp.arange(n_rows), GRID_W)
    col = jnp.tile(jnp.arange(GRID_W), n_rows)
    tabs_hd = _axial_tables(row, col, HEAD_DIM)
    tabs_mla = _axial_tables(row, col, MLA_ROPE)
    silu_c = jax.nn.silu(c)
    silu_cc = jax.nn.silu(c_ctx)
    for i in range(DEPTH):
        last = i == DEPTH - 1
        sh1, sc1, g1, sh2, sc2, g2 = (m[:, None, :] for m in jnp.split(silu_c @ ada_w[i] + ada_b[i], 6, axis=-1))
        shc1, scc1, gc1, shc2, scc2, gc2 = jnp.split(silu_cc @ ada_w[i] + ada_b[i], 6, axis=-1)
        u = x * (1 + sc1) + sh1
        uc = ctx * (1 + scc1) + shc1
        j = i // 2
        if i % 2 == 0:
            y, yc = _even_mixer(u, uc, even_w_in[j], even_b_in[j], even_conv_w[j], even_conv_b[j],
                                even_conv_ln_g[j], even_conv_ln_b[j], even_sink[j], even_w_out[j], even_b_out[j],
                                tabs_hd, not last)
        else:
            y, yc = _odd_mixer(u, uc, odd_w_in[j], odd_b_in[j], odd_q_norm[j], odd_k_norm[j], odd_mla_q_norm[j],
                               odd_mla_kv_norm[j], odd_mla_w_uq[j], odd_mla_w_ukv[j], odd_w_out[j], odd_b_out[j],
                               tabs_hd, tabs_mla, not last)
        x = _layer_norm(DN_ALPHA * x + (1 + g1) * y, ln1_g[i], ln1_b[i])
        u2 = x * (1 + sc2) + sh2
        if last:
            f = _hier_moe(u2.reshape(-1, d), moe_w_rg[i], moe_b_rg[i], moe_w_re[i], moe_b_re[i],
                          moe_w1[i], moe_w3[i], moe_w2[i])
            x = _layer_norm(DN_ALPHA * x + (1 + g2) * f.reshape(x.shape), ln2_g[i], ln2_b[i])
        else:
            ctx = _layer_norm(DN_ALPHA * ctx + (1 + gc1) * yc, ln1_g[i], ln1_b[i])
            u2c = ctx * (1 + scc2) + shc2
            f = _hier_moe(jnp.concatenate([u2.reshape(-1, d), u2c.reshape(-1, d)], axis=0),
                          moe_w_rg[i], moe_b_rg[i], moe_w_re[i], moe_b_re[i], moe_w1[i], moe_w3[i], moe_w2[i])
            f_lat = f[:bsz * n_lat].reshape(x.shape)
            f_ctx = f[bsz * n_lat:].reshape(ctx.shape)
            x = _layer_norm(DN_ALPHA * x + (1 + g2) * f_lat, ln2_g[i], ln2_b[i])
            ctx = _layer_norm(DN_ALPHA * ctx + (1 + gc2) * f_ctx, ln2_g[i], ln2_b[i])
    return x
```

```python
import numpy as np
from contextlib import ExitStack
import concourse.bass as bass
import concourse.mybir as mybir
from concourse.bass_utils import run_bass_kernel_spmd


ENGS = ("pe", "act", "dve", "pool", "sp")


class Tile:
    __slots__ = ("name", "w", "r")

    def __init__(self, name):
        self.name = name
        self.w = {}
        self.r = {}


class Op:
    __slots__ = ("eng", "fn", "idx", "deps", "signal", "ordinal", "dma", "chan", "grp", "gidx")

    def __init__(self, eng, fn):
        self.eng = eng
        self.fn = fn
        self.deps = []
        self.signal = False
        self.ordinal = None
        self.dma = False
        self.chan = None
        self.grp = None


class DmaGroup:
    __slots__ = ("last", "closed")

    def __init__(self):
        self.last = 0
        self.closed = False


class Sched:
    def __init__(self, nc, n_chan_sems=60):
        self.nc = nc
        self.ops = {e: [] for e in ENGS}
        self.nops = {e: 0 for e in ENGS}
        self.nsig = {e: 0 for e in ENGS}
        self.seen = {e: {} for e in ENGS}
        self.esem = {}
        self.chan_sem = {}
        self.chan_cnt = {}
        self.chan_lastgrp = {}
        self.chan_lastop = {}
        self._sem_cms = []
        for e in ("pe", "act", "dve", "pool"):
            cm = nc.semaphore("e_" + e)
            self.esem[e] = cm.__enter__()
            self._sem_cms.append(cm)
        self.free_chan = []
        for i in range(n_chan_sems):
            cm = nc.semaphore("c_%d" % i)
            self.free_chan.append(cm.__enter__())
            self._sem_cms.append(cm)
        self.pending_dma = {e: [] for e in ENGS}

    def close(self):
        for cm in reversed(self._sem_cms):
            cm.__exit__(None, None, None)

    def _stream(self, o):
        return ("dma", o.chan) if o.dma else o.eng

    def _add_dep(self, o, p, raw):
        if p is o:
            return
        if p.dma and o.dma and p.grp is o.grp:
            return
        ps = self._stream(p)
        if (not p.dma) and (not o.dma) and p.eng == o.eng == "pe" and not raw:
            return
        seen = self.seen[o.eng]
        if seen.get(ps, -1) >= p.idx:
            return
        seen[ps] = p.idx
        if p.dma:
            p.grp.closed = True
        else:
            p.signal = True
        o.deps.append(p)

    def op(self, eng, fn, reads=(), writes=(), chan=None, grp=None):
        o = Op(eng, fn)
        if chan is not None:
            o.dma = True
            o.chan = chan
            if chan not in self.chan_sem:
                self.chan_sem[chan] = self.free_chan.pop()
                self.chan_cnt[chan] = 0
            self.chan_cnt[chan] += 1
            o.idx = self.chan_cnt[chan]
            lg = self.chan_lastgrp.get(chan)
            if lg is not None and (not lg[1].closed) and lg[0] == grp and grp is not None:
                o.grp = lg[1]
            else:
                prev = self.chan_lastop.get(chan)
                o.grp = DmaGroup()
                self.chan_lastgrp[chan] = (grp, o.grp)
                if prev is not None:
                    self._add_dep_dma_prev(o, prev)
            o.grp.last = o.idx
            self.chan_lastop[chan] = o
            self.pending_dma[eng].append(o)
        else:
            o.idx = self.nops[eng]
        self.nops[eng] += 1
        for t in reads:
            for p in t.w.values():
                self._add_dep(o, p, True)
        for t in writes:
            for p in t.w.values():
                self._add_dep(o, p, False)
            for p in t.r.values():
                self._add_dep(o, p, False)
        st = self._stream(o)
        for t in reads:
            t.r[st] = o
        for t in writes:
            t.w[st] = o
        self.ops[eng].append(o)
        return o

    def _add_dep_dma_prev(self, o, prev):
        seen = self.seen[o.eng]
        ps = ("dma", prev.chan)
        if seen.get(ps, -1) >= prev.idx:
            return
        seen[ps] = prev.idx
        prev.grp.closed = True
        o.deps.append(prev)

    def flush(self, name=None, drain_dma=True):
        nc = self.nc
        tail = {e: [] for e in ENGS}
        if drain_dma:
            for e in ENGS:
                chans = {}
                for o in self.pending_dma[e]:
                    chans[o.chan] = o
                for ch, o in chans.items():
                    o.grp.closed = True
                    tail[e].append((self.chan_sem[ch], 16 * o.grp.last))
                    self.seen[e][("dma", ch)] = max(self.seen[e].get(("dma", ch), -1), o.idx)
                self.pending_dma[e] = []
        for e in ENGS:
            for o in self.ops[e]:
                if (not o.dma) and o.signal:
                    self.nsig[e] += 1
                    o.ordinal = self.nsig[e]
        sched = self

        def emit(e):
            def body(engine):
                for o in sched.ops[e]:
                    for p in o.deps:
                        if p.dma:
                            engine.wait_ge(sched.chan_sem[p.chan], 16 * p.grp.last)
                        else:
                            engine.wait_ge(sched.esem[p.eng], p.ordinal)
                    ins = o.fn(engine)
                    if o.dma:
                        ins.then_inc(sched.chan_sem[o.chan], 16)
                    elif o.signal:
                        ins.then_inc(sched.esem[e], 1)
                for (sem, val) in tail[e]:
                    engine.wait_ge(sem, val)
            return body

        with nc.Block() as block:
            if self.ops["sp"] or tail["sp"]:
                block.sync(emit("sp"))
            if self.ops["pe"] or tail["pe"]:
                block.tensor(emit("pe"))
            if self.ops["act"] or tail["act"]:
                block.scalar(emit("act"))
            if self.ops["dve"] or tail["dve"]:
                block.vector(emit("dve"))
            if self.ops["pool"] or tail["pool"]:
                block.gpsimd(emit("pool"))
        for e in ENGS:
            for s in ("pe", "act", "dve", "pool"):
                self.seen[e][s] = self.nops[s]
            self.ops[e] = []


F32 = mybir.dt.float32; BF16 = mybir.dt.bfloat16
AF = mybir.ActivationFunctionType
ALU = mybir.AluOpType
AX = mybir.AxisListType
LN_EPS = 1e-5
DN_ALPHA = float(4 ** 0.25)
NEG = -30000.0


def mkE(S):
    def E(eng, meth, *args, reads=(), writes=(), chan=None, grp=None, **kw):
        return S.op(eng, lambda e: getattr(e, meth)(*args, **kw), reads=reads, writes=writes, chan=chan, grp=grp)
    return E


def attn_pipeline(E, groups, SC, tSC, PT, tPT, n):
    its = [(gi, kt) for gi, g in enumerate(groups) for kt in range(g["nkt"])]

    def qk(j):
        gi, kt = its[j]
        g = groups[gi]
        if kt == 0 and g.get("pre") is not None:
            g["pre"]()
        g["qk"](kt, SC[j % 2], tSC[j % 2])

    pend = []
    qk(0)
    for j, (gi, kt) in enumerate(its):
        g = groups[gi]
        if j + 1 < len(its):
            qk(j + 1)
        E("act", "activation", out=PT[j % 3][:, 0:n], in_=SC[j % 2][:, 0:n], func=AF.Exp, scale=g["scale"], reads=[tSC[j % 2]], writes=[tPT[j % 3]])
        g["pv"](kt, PT[j % 3], tPT[j % 3], kt == 0, kt == g["nkt"] - 1)
        while pend and pend[0][0] <= j:
            pend.pop(0)[1]()
        if kt == g["nkt"] - 1:
            pend += [(j + 1, g["fin1"]), (j + 2, g["fin2"]), (j + 3, g["fin3"])]
            pend.sort(key=lambda t: t[0])
    for item in pend:
        item[1]()


def emit_l0_mixer(nc, S, NL, d, li=0, tag=""):
    E = mkE(S)
    EXT = NL + 256
    NC = EXT + 256
    T = NL + 256
    es = ExitStack()
    sb = lambda name, shape, dt: es.enter_context(nc.sbuf_tensor("m0" + tag + "_" + name, shape, dt))
    ps = lambda name, shape, dt: es.enter_context(nc.psum_tensor("m0p" + tag + "_" + name, shape, dt))
    XB = sb("XB", [128, 8, 512], F32)
    UB = sb("UB", [128, 8, 512], BF16)
    HL = sb("HL", [128, 4, EXT], BF16)
    HC = sb("HC", [128, 4, 286], BF16)
    KT = [sb("KT%d" % g, [128, NC], BF16) for g in range(2)]
    VA = sb("VA", [128, NC // 128, 2, 65], BF16)
    WA = sb("WA", [128, 8, 1408], BF16)
    WOC = sb("WOC", [128, 4, 1024], BF16)
    WOA = sb("WOA", [64, 8, 1024], BF16)
    DG = sb("DG", [128, 124, 128], BF16)
    QT = [sb("QT%d" % g, [128, 4, 4, 128], BF16) for g in range(2)]
    PT = [sb("PT%d" % i, [128, 512], BF16) for i in range(3)]
    AO = sb("AO", [64, 8, 512], BF16)
    CV = sb("CV", [128, 4, 512], F32)
    CO = sb("CO", [128, 4, 512], BF16)
    LT = [sb("LT%d" % i, [128, 512], F32) for i in range(4)]
    TB = [sb("TB%d" % i, [64, 512], F32) for i in range(2)]
    R1 = sb("R1", [128, 512], F32)
    R2 = sb("R2", [128, 512], F32)
    OTS = sb("OTS", [65, 512], F32)
    DROW = sb("DROW", [65, 512], F32)
    OUTA = [sb("OUTA%d" % i, [128, 512], F32) for i in range(1)]
    OUTB = [sb("OUTB%d" % i, [128, 512], F32) for i in range(1)]
    modv = sb("modv", [128, 48, 2], F32)
    sc1p1 = sb("sc1p1", [128, 8, 2], F32)
    g1p1 = sb("g1p1", [128, 8, 2], F32)
    sc2p1 = sb("sc2p1", [128, 8, 2], F32)
    b128 = sb("b128", [128, 8], F32)
    b64 = sb("b64", [64, 20], F32)
    bvb = sb("bvb", [128, 128], F32)
    cw = sb("cw", [128, 4, 31], F32)
    cvec = sb("cvec", [128, 3, 4], F32)
    bout = sb("bout", [128, 8], F32)
    ln1 = sb("ln1", [128, 2, 8], F32)
    vmask = sb("vmask", [128, 256], BF16)
    MB = sb("MB", [128, 2, 512], BF16)
    identb = sb("identb", [128, 128], BF16)
    identf = sb("identf", [128, 128], F32)
    ones1k = sb("ones1k", [128, 128], F32)
    ones512 = sb("ones512", [128, 128], F32)
    mhalf = sb("mhalf", [128, 8], F32)
    epsc = sb("epsc", [128, 2], F32)
    ones65 = sb("ones65", [65, 128], F32)
    sink8 = sb("sink8", [65, 8], F32)
    P = [ps("P%d" % i, [128, 512], F32) for i in range(8)]
    tP = [Tile("P%d" % i) for i in range(8)]
    tC = Tile("const")
    tXB = Tile("XB"); tUB = Tile("UB")
    NCB = (NC + 511) // 512
    tH = [Tile("H%d" % i) for i in range(NCB + 1)]
    tKT = [Tile("KT%d" % i) for i in range(NCB)]
    tVA = [Tile("VA%d" % i) for i in range(NCB)]
    tWA = Tile("WA"); tWO = Tile("WO"); tDG = Tile("DG")
    tQT = [Tile("QT%d" % g) for g in range(2)]
    tPT = [Tile("PT%d" % i) for i in range(3)]
    tAO = Tile("AO"); tCV = Tile("CV"); tCO = Tile("CO")
    tLT = [Tile("LT%d" % i) for i in range(4)]
    tTB = Tile("TB"); tR1 = Tile("R1"); tR2 = Tile("R2"); tOTS = Tile("OTS"); tDROW = Tile("DROW")
    tOUTA = [Tile("OUTA%d" % i) for i in range(1)]
    tOUTB = [Tile("OUTB%d" % i) for i in range(1)]

    cst = dict(writes=[tC], chan="k0", grp=0)
    E("sp", "dma_start", out=modv[:], in_=d["modv"], **cst)
    E("sp", "dma_start", out=b128[:], in_=d["b128"], **cst)
    E("sp", "dma_start", out=b64[:], in_=d["b64"], **cst)
    E("sp", "dma_start", out=bvb[:], in_=bass.AP(d["bv"].tensor, 0, [[0, 128], [1, 128]]), **cst)
    E("sp", "dma_start", out=cw[:], in_=d["cw"], **cst)
    E("sp", "dma_start", out=cvec[:], in_=d["cvec"], **cst)
    E("sp", "dma_start", out=bout[:], in_=d["bout"], **cst)
    E("sp", "dma_start", out=ln1[:], in_=d["ln1"], **cst)
    E("sp", "dma_start", out=identf[:], in_=d["ident"], **cst)
    E("sp", "dma_start", out=sink8[64:65, :], in_=bass.AP(d["sink"].tensor, 0, [[0, 1], [1, 8]]), **cst)
    cst2 = dict(writes=[tC], chan="k1", grp=0)
    E("pool", "dma_start", out=vmask[:], in_=d["vmask"], **cst2)
    E("pool", "dma_start", out=MB[:], in_=d["mb"], **cst2)
    E("pool", "dma_start", out=identb[:], in_=d["ident"], **cst2)
    E("pool", "dma_start", out=WA[:], in_=d["win"][:, :, 0:1408], writes=[tWA], chan="k2")
    E("pool", "dma_start", out=WOC[:], in_=d["woc"], writes=[tWO], chan="k3", grp=0)
    E("pool", "dma_start", out=WOA[:], in_=d["woa"], writes=[tWO], chan="k3", grp=0)
    for g in range(2):
        E("pool", "memset", KT[g][64:128, :], 0.0, writes=[tKT[i] for i in range(NCB)])
        E("pool", "memset", QT[g][64:128, :, :, :], 0.0, writes=[tQT[g]])
    for g in range(2):
        E("pool", "dma_start", out=KT[g][64:65, :], in_=bass.AP(d["kbias"].tensor, 0, [[0, 1], [1, NC]]), writes=[tKT[i] for i in range(NCB)], chan="k4", grp=0)
    E("dve", "memset", ones1k[:], 1.0 / 1024.0, writes=[tC])
    E("dve", "memset", ones512[:], 1.0 / 512.0, writes=[tC])
    E("dve", "memset", epsc[:, 0:1], LN_EPS, writes=[tC])
    E("dve", "memset", epsc[:, 1:2], 1e-6, writes=[tC])
    E("dve", "memset", ones65[:], 1.0, writes=[tC])
    E("dve", "memset", HC[:], 0.0, writes=[tH[NCB]])
    E("pool", "memset", VA[:, :, :, 64:65], 1.0, writes=tVA)
    for g in range(2):
        E("pool", "memset", QT[g][64:65, :, :, :], 1.0, writes=[tQT[g]])
    E("dve", "tensor_scalar", sc1p1[:], modv[:, 8:16, :], 1.0, None, ALU.add, reads=[tC], writes=[tC])
    E("dve", "tensor_scalar", g1p1[:], modv[:, 16:24, :], 1.0, None, ALU.add, reads=[tC], writes=[tC])
    E("dve", "tensor_scalar", sc2p1[:], modv[:, 32:40, :], 1.0, None, ALU.add, reads=[tC], writes=[tC])
    E("act", "activation", out=sink8[64:65, :], in_=sink8[64:65, :], func=AF.Exp, reads=[tC], writes=[tC])
    for c in range(4):
        for k in range(31):
            if (c * 31 + k) % 2 == 0:
                E("dve", "tensor_scalar", DG[:, c * 31 + k, :], identf[:], cw[:, c, k:k + 1], None, ALU.mult, reads=[tC], writes=[tDG])
            else:
                E("act", "activation", out=DG[:, c * 31 + k, :], in_=identf[:], func=AF.Identity, scale=cw[:, c, k:k + 1], reads=[tC], writes=[tDG])

    def make_u(c0, n):
        segs = []
        if c0 < EXT:
            segs.append((0, min(n, EXT - c0), 0))
        if c0 + n > EXT:
            s0 = max(0, EXT - c0)
            segs.append((s0, n, 1))
        for k in range(8):
            for (a, b, m) in segs:
                eng = ("dve", "act", "pool", "dve", "act", "dve", "act", "pool")[k]
                if eng == "act":
                    E("act", "activation", out=UB[:, k, a:b], in_=XB[:, k, a:b], func=AF.Identity, scale=sc1p1[:, k, m:m + 1], bias=modv[:, k, m:m + 1],
                      reads=[tXB, tC], writes=[tUB])
                else:
                    E(eng, "tensor_scalar", UB[:, k, a:b], XB[:, k, a:b], sc1p1[:, k, m:m + 1], modv[:, k, m:m + 1], ALU.mult, ALU.add,
                      reads=[tXB, tC], writes=[tUB])
        return segs

    cblocks = [(c0, min(512, NC - c0)) for c0 in range(0, NC, 512)]
    for bi, (c0, n) in enumerate(cblocks):
        E("sp", "dma_start", out=XB[:, :, 0:n], in_=d["xe"][:, :, c0:c0 + n], writes=[tXB], chan="xb")
        E("sp", "dma_start", out=TB[0][:, 0:n], in_=d["tabc"][:, c0:c0 + n], writes=[tTB], chan="tb", grp=bi)
        E("sp", "dma_start", out=TB[1][:, 0:n], in_=d["tabs"][:, c0:c0 + n], writes=[tTB], chan="tb", grp=bi)
        segs = make_u(c0, n)
        for c in range(4):
            pg, pv = P[0], P[1]
            for k in range(8):
                E("pe", "matmul", pg[:, 0:n], WA[:, k, 512 + c * 128:512 + (c + 1) * 128], UB[:, k, 0:n], start=(k == 0), stop=(k == 7),
                  reads=[tWA, tUB], writes=[tP[0]])
            for k in range(8):
                E("pe", "matmul", pv[:, 0:n], WA[:, k, c * 128:(c + 1) * 128], UB[:, k, 0:n], start=(k == 0), stop=(k == 7),
                  reads=[tWA, tUB], writes=[tP[1]])
            E("act", "activation", out=R1[:, 0:n], in_=pg[:, 0:n], func=AF.Sigmoid, bias=b128[:, 4 + c:5 + c], reads=[tP[0], tC], writes=[tR1])
            for (a, b, m) in segs:
                if m == 0:
                    dst = HL[:, c, c0 + a:c0 + b]
                    th = tH[bi]
                else:
                    dst = HC[:, c, 15 + c0 + a - EXT:15 + c0 + b - EXT]
                    th = tH[NCB]
                E("dve", "scalar_tensor_tensor", dst, pv[:, a:b], b128[:, c:c + 1], R1[:, a:b], ALU.add, ALU.mult,
                  reads=[tP[1], tR1, tC], writes=[th])
        for g in range(2):
            pk, pkp = P[2], P[3]
            for k in range(8):
                E("pe", "matmul", pk[0:64, 0:n], WA[:, k, 1024 + g * 64:1024 + (g + 1) * 64], UB[:, k, 0:n], start=(k == 0), stop=(k == 7),
                  reads=[tWA, tUB], writes=[tP[2]])
            for k in range(8):
                E("pe", "matmul", pkp[0:64, 0:n], WA[:, k, 1152 + g * 64:1152 + (g + 1) * 64], UB[:, k, 0:n], start=(k == 0), stop=(k == 7),
                  reads=[tWA, tUB], writes=[tP[3]])
            E("dve", "scalar_tensor_tensor", R2[0:64, 0:n], pk[0:64, 0:n], b64[:, 16 + g:17 + g], TB[0][:, 0:n], ALU.add, ALU.mult,
              reads=[tP[2], tTB, tC], writes=[tR2])
            E("dve", "scalar_tensor_tensor", LT[3][0:64, 0:n], pkp[0:64, 0:n], b64[:, 18 + g:19 + g], TB[1][:, 0:n], ALU.add, ALU.mult,
              reads=[tP[3], tTB, tC], writes=[tLT[3]])
            E("pool", "tensor_tensor", KT[g][0:64, c0:c0 + n], R2[0:64, 0:n], LT[3][0:64, 0:n], ALU.add, reads=[tR2, tLT[3]], writes=[tKT[bi]])
        for tt in range(n // 128):
            for k in range(8):
                E("pe", "matmul", P[4][:, 0:128], UB[:, k, tt * 128:(tt + 1) * 128], WA[:, k, 1280:1408], start=(k == 0), stop=(k == 7),
                  reads=[tWA, tUB], writes=[tP[4]])
            E("dve", "tensor_tensor", VA[:, c0 // 128 + tt, :, 0:64], P[4][:, 0:128].rearrange("p (g e) -> p g e", g=2),
              bvb[:].rearrange("p (g e) -> p g e", g=2), ALU.add, reads=[tP[4], tC], writes=[tVA[bi]])
    for c in range(4):
        E("dve", "tensor_tensor", HL[:, c, 0:128], HL[:, c, 0:128], vmask[:, 0:128], ALU.mult, reads=[tH[0], tC], writes=[tH[0]])
        lastb = (EXT - 1) // 512
        E("dve", "tensor_tensor", HL[:, c, EXT - 128:EXT], HL[:, c, EXT - 128:EXT], vmask[:, 128:256], ALU.mult, reads=[tH[lastb], tC], writes=[tH[lastb]])
    E("pool", "dma_start", out=WA[:, :, 0:1024], in_=d["win"][:, :, 1408:2432], writes=[tWA], chan="k2")

    oblocks = [(t0, 512, 0) for t0 in range(0, NL, 512)] + [(NL, 256, 1)]
    allH = tH
    allKT = tKT
    allVA = tVA
    for bi, (t0, n, m) in enumerate(oblocks):
        c0 = (128 + t0) if m == 0 else EXT
        E("sp", "dma_start", out=XB[:, :, 0:n], in_=d["xe"][:, :, c0:c0 + n], writes=[tXB], chan="xb")
        E("sp", "dma_start", out=TB[0][:, 0:n], in_=d["tabc"][:, c0:c0 + n], writes=[tTB], chan="tb", grp=100 + bi)
        E("sp", "dma_start", out=TB[1][:, 0:n], in_=d["tabs"][:, c0:c0 + n], writes=[tTB], chan="tb", grp=100 + bi)
        make_u(c0, n)
        ntl = n // 128
        for hh in range(8):
            g, i = hh // 4, hh % 4
            for k in range(8):
                E("pe", "matmul", P[0][0:64, 0:n], WA[:, k, hh * 64:(hh + 1) * 64], UB[:, k, 0:n], start=(k == 0), stop=(k == 7),
                  reads=[tWA, tUB], writes=[tP[0]])
            for k in range(8):
                E("pe", "matmul", P[1][0:64, 0:n], WA[:, k, 512 + hh * 64:512 + (hh + 1) * 64], UB[:, k, 0:n], start=(k == 0), stop=(k == 7),
                  reads=[tWA, tUB], writes=[tP[1]])
            E("dve", "scalar_tensor_tensor", R1[0:64, 0:n], P[0][0:64, 0:n], b64[:, hh:hh + 1], TB[0][:, 0:n], ALU.add, ALU.mult,
              reads=[tP[0], tTB, tC], writes=[tR1])
            E("dve", "scalar_tensor_tensor", R2[0:64, 0:n], P[1][0:64, 0:n], b64[:, 8 + hh:9 + hh], TB[1][:, 0:n], ALU.add, ALU.mult,
              reads=[tP[1], tTB, tC], writes=[tR2])
            E("pool", "tensor_tensor", QT[g][0:64, 0:ntl, i, :], R1[0:64, 0:n].rearrange("p (t q) -> p t q", q=128),
              R2[0:64, 0:n].rearrange("p (t q) -> p t q", q=128), ALU.add, reads=[tR1, tR2], writes=[tQT[g]])
        groups = []
        for tl in range(ntl):
            for g in range(2):
                gi = len(groups)
                acc, tacc = P[4 + 2 * (gi % 2)], tP[4 + 2 * (gi % 2)]
                if m == 0:
                    qi = t0 // 128 + tl
                    chunks = [(EXT // 128, None), (EXT // 128 + 1, None), (qi, 0), (qi + 1, None), (qi + 2, 1)]
                else:
                    chunks = [(EXT // 128, None), (EXT // 128 + 1, None)]
                rhs_q = QT[g][:, tl, :, :].rearrange("p i q -> p (i q)")

                def qk(ci, ps_, tps, g=g, rhs_q=rhs_q, chunks=chunks):
                    kt, mk = chunks[ci]
                    E("pe", "matmul", ps_[:, :], KT[g][:, kt * 128:(kt + 1) * 128], rhs_q, start=True, stop=(mk is None),
                      reads=[tQT[g], allKT[(kt * 128) // 512]], writes=[tps])
                    if mk is not None:
                        E("pe", "matmul", ps_[:, :], identb[:], MB[:, mk, :], start=False, stop=True, reads=[tC], writes=[tps])

                def pv(ci, pt, tpt, st, sp, g=g, chunks=chunks, acc=acc, tacc=tacc):
                    kt = chunks[ci][0]
                    E("pe", "matmul", acc[0:65, :], VA[:, kt, g, :], pt[:], start=st, stop=sp, reads=[tpt, allVA[(kt * 128) // 512]], writes=[tacc])

                def fin1(g=g, acc=acc, tacc=tacc):
                    for i4 in range(4):
                        E("dve", "tensor_scalar", DROW[64:65, i4 * 128:(i4 + 1) * 128], acc[64:65, i4 * 128:(i4 + 1) * 128],
                          sink8[64:65, g * 4 + i4:g * 4 + i4 + 1], None, ALU.add, reads=[tacc, tC], writes=[tDROW])
                    E("dve", "reciprocal", DROW[64:65, :], DROW[64:65, :], reads=[tDROW], writes=[tDROW])
                    E("act", "copy", out=OTS[0:64, :], in_=acc[0:64, :], reads=[tacc], writes=[tOTS])

                def fin2():
                    E("pe", "matmul", P[5][0:64, :], ones65[64:65, 0:64], DROW[64:65, :], start=True, stop=True, reads=[tDROW, tC], writes=[tP[5]])

                def fin3(g=g, tl=tl):
                    E("dve", "tensor_tensor", AO[:, g * 4:(g + 1) * 4, tl * 128:(tl + 1) * 128], OTS[0:64, :].rearrange("p (i q) -> p i q", q=128),
                      P[5][0:64, :].rearrange("p (i q) -> p i q", q=128), ALU.mult, reads=[tOTS, tP[5]], writes=[tAO])

                groups.append(dict(pre=None, nkt=len(chunks), qk=qk, scale=0.125, pv=pv, fin1=fin1, fin2=fin2, fin3=fin3))
        attn_pipeline(E, groups, [P[2], P[3]], [tP[2], tP[3]], PT, tPT, 512)
        for c in range(4):
            for k in range(31):
                if m == 0:
                    rhs = HL[:, c, c0 + k - 15:c0 + k - 15 + n]
                else:
                    rhs = HC[:, c, k:k + n]
                E("pe", "matmul", P[6][:, 0:n], DG[:, c * 31 + k, :], rhs, start=(k == 0), stop=(k == 30), reads=[tDG] + allH, writes=[tP[6]])
            E("act", "activation", out=CV[:, c, 0:n], in_=P[6][:, 0:n], func=AF.Identity, bias=cvec[:, 0, c:c + 1], reads=[tP[6], tC], writes=[tCV])
        for c in range(4):
            E("pe", "matmul", P[5][:, 0:n], ones512[:], CV[:, c, 0:n], start=(c == 0), stop=(c == 3), reads=[tCV, tC], writes=[tP[5]])
        for c in range(4):
            E("act", "activation", out=LT[2][:, 0:n], in_=CV[:, c, 0:n], func=AF.Square, reads=[tCV], writes=[tLT[2]])
            E("pe", "matmul", P[7][:, 0:n], ones512[:], LT[2][:, 0:n], start=(c == 0), stop=(c == 3), reads=[tLT[2], tC], writes=[tP[7]])
        E("act", "copy", out=LT[0][:, 0:n], in_=P[5][:, 0:n], reads=[tP[5]], writes=[tLT[0]])
        E("dve", "tensor_tensor", LT[1][:, 0:n], LT[0][:, 0:n], LT[0][:, 0:n], ALU.mult, reads=[tLT[0]], writes=[tLT[1]])
        E("dve", "tensor_tensor", LT[1][:, 0:n], P[7][:, 0:n], LT[1][:, 0:n], ALU.subtract, reads=[tP[7], tLT[1]], writes=[tLT[1]])
        E("act", "activation", out=LT[1][:, 0:n], in_=LT[1][:, 0:n], func=AF.Sqrt, bias=epsc[:, 0:1], reads=[tLT[1], tC], writes=[tLT[1]])
        E("dve", "reciprocal", LT[1][:, 0:n], LT[1][:, 0:n], reads=[tLT[1]], writes=[tLT[1]])
        for c in range(4):
            E("dve", "tensor_tensor", CV[:, c, 0:n], CV[:, c, 0:n], LT[0][:, 0:n], ALU.subtract, reads=[tCV, tLT[0]], writes=[tCV])
            E("pool", "tensor_tensor", CV[:, c, 0:n], CV[:, c, 0:n], LT[1][:, 0:n], ALU.mult, reads=[tCV, tLT[1]], writes=[tCV])
            E("act", "activation", out=CO[:, c, 0:n], in_=CV[:, c, 0:n], func=AF.Silu, scale=cvec[:, 1, c:c + 1], bias=cvec[:, 2, c:c + 1],
              reads=[tCV, tC], writes=[tCO])
        for dk in range(8):
            pb = 6 + (dk % 2)
            for c in range(4):
                E("pe", "matmul", P[pb][:, 0:n], WOC[:, c, dk * 128:(dk + 1) * 128], CO[:, c, 0:n], start=(c == 0), stop=False,
                  reads=[tWO, tCO], writes=[tP[pb]])
            for hh in range(8):
                E("pe", "matmul", P[pb][:, 0:n], WOA[:, hh, dk * 128:(dk + 1) * 128], AO[:, hh, 0:n], start=False, stop=(hh == 7),
                  reads=[tWO, tAO], writes=[tP[pb]])
            E("dve", "tensor_scalar", LT[3][:, 0:n], P[pb][:, 0:n], bout[:, dk:dk + 1], g1p1[:, dk, m:m + 1], ALU.add, ALU.mult,
              reads=[tP[pb], tC], writes=[tLT[3]])
            E("dve", "scalar_tensor_tensor", XB[:, dk, 0:n], XB[:, dk, 0:n], DN_ALPHA, LT[3][:, 0:n], ALU.mult, ALU.add,
              reads=[tXB, tLT[3]], writes=[tXB])
        for k in range(8):
            E("pe", "matmul", P[5][:, 0:n], ones1k[:], XB[:, k, 0:n], start=(k == 0), stop=(k == 7), reads=[tXB, tC], writes=[tP[5]])
        for k in range(8):
            E("act", "activation", out=LT[2][:, 0:n], in_=XB[:, k, 0:n], func=AF.Square, reads=[tXB], writes=[tLT[2]])
            E("pe", "matmul", P[4][:, 0:n], ones1k[:], LT[2][:, 0:n], start=(k == 0), stop=(k == 7), reads=[tLT[2], tC], writes=[tP[4]])
        E("act", "copy", out=LT[0][:, 0:n], in_=P[5][:, 0:n], reads=[tP[5]], writes=[tLT[0]])
        E("dve", "tensor_tensor", LT[1][:, 0:n], LT[0][:, 0:n], LT[0][:, 0:n], ALU.mult, reads=[tLT[0]], writes=[tLT[1]])
        E("dve", "tensor_tensor", LT[1][:, 0:n], P[4][:, 0:n], LT[1][:, 0:n], ALU.subtract, reads=[tP[4], tLT[1]], writes=[tLT[1]])
        E("act", "activation", out=LT[1][:, 0:n], in_=LT[1][:, 0:n], func=AF.Sqrt, bias=epsc[:, 0:1], reads=[tLT[1], tC], writes=[tLT[1]])
        E("dve", "reciprocal", LT[1][:, 0:n], LT[1][:, 0:n], reads=[tLT[1]], writes=[tLT[1]])
        for k in range(8):
            ob = 0
            E("dve", "tensor_tensor", XB[:, k, 0:n], XB[:, k, 0:n], LT[0][:, 0:n], ALU.subtract, reads=[tXB, tLT[0]], writes=[tXB])
            E("pool", "tensor_tensor", XB[:, k, 0:n], XB[:, k, 0:n], LT[1][:, 0:n], ALU.mult, reads=[tXB, tLT[1]], writes=[tXB])
            E("act", "activation", out=XB[:, k, 0:n], in_=XB[:, k, 0:n], func=AF.Identity, scale=ln1[:, 0, k:k + 1], bias=ln1[:, 1, k:k + 1],
              reads=[tXB, tC], writes=[tXB])
            E("act", "mul", OUTA[ob][:, 0:n], XB[:, k, 0:n], DN_ALPHA, reads=[tXB], writes=[tOUTA[ob]])
            E("dve", "tensor_scalar", OUTB[ob][:, 0:n], XB[:, k, 0:n], sc2p1[:, k, m:m + 1], modv[:, 24 + k, m:m + 1], ALU.mult, ALU.add,
              reads=[tXB, tC], writes=[tOUTB[ob]])
            E("sp", "dma_start", out=d["xa"][:, k, t0:t0 + n], in_=OUTA[ob][:, 0:n], reads=[tOUTA[ob]], chan="oa%d" % ob)
            E("sp", "dma_start", out=d["u2f"][:, k, t0:t0 + n], in_=OUTB[ob][:, 0:n], reads=[tOUTB[ob]], chan="ob%d" % ob)
    S.flush()
    es.close()


F32 = mybir.dt.float32; BF16 = mybir.dt.bfloat16
AF = mybir.ActivationFunctionType
ALU = mybir.AluOpType
LN_EPS = 1e-5
RMS_EPS = 1e-6
DN_ALPHA = float(4 ** 0.25)


def emit_l1_mixer(nc, S, NL, SEQ, d):
    E = mkE(S)
    NK = SEQ + 256
    NKT = NK // 128
    T = NL
    es = ExitStack()
    sb = lambda name, shape, dt: es.enter_context(nc.sbuf_tensor("m1_" + name, shape, dt))
    ps = lambda name, shape, dt: es.enter_context(nc.psum_tensor("m1p_" + name, shape, dt))
    XB = sb("XB", [128, 8, 512], F32)
    UB = sb("UB", [128, 8, 512], BF16)
    KT = sb("KT", [128, NK], BF16)
    VG = sb("VG", [128, NKT, 2, 65], BF16)
    KVN = sb("KVN", [128, NK], BF16)
    KR = sb("KR", [128, NK], BF16)
    VM = sb("VM", [128, NKT, 4, 65], BF16)
    WQ = sb("WQ", [128, 8, 1280], BF16)
    WUQ = sb("WUQ", [128, 2, 8, 128], BF16)
    WUKT = sb("WUKT", [64, 8, 128], BF16)
    WUV = sb("WUV", [128, 512], BF16)
    WOS = [sb("WOS%d" % i, [64, 16, 128], BF16) for i in range(2)]
    QTZ = [sb("QTZ%d" % g, [128, 4, 4, 128], BF16) for g in range(2)]
    QCN = sb("QCN", [128, 2, 512], BF16)
    QN = sb("QN", [64, 512], BF16)
    QABS = sb("QABS", [128, 512], BF16)
    QR = sb("QR", [128, 512], BF16)
    PT = [sb("PT%d" % i, [128, 512], BF16) for i in range(3)]
    AO = sb("AO", [64, 16, 512], BF16)
    LT = [sb("LT%d" % i, [128, 512], F32) for i in range(4)]
    R1 = sb("R1", [128, 512], F32)
    R2 = sb("R2", [128, 512], F32)
    RS = sb("RS", [128, 512], F32)
    CG = sb("CG", [128, 512], F32)
    SG = sb("SG", [128, 512], F32)
    TB = [sb("TB%d" % i, [128, 512], F32) for i in range(2)]
    TM = [sb("TM%d" % i, [32, 512], F32) for i in range(2)]
    OTS = sb("OTS", [65, 512], F32)
    DROW = sb("DROW", [65, 512], F32)
    OUTA = [sb("OUTA%d" % i, [128, 512], F32) for i in range(2)]
    OUTB = [sb("OUTB%d" % i, [128, 512], F32) for i in range(2)]
    modv = sb("modv", [128, 48, 2], F32)
    sc1p1 = sb("sc1p1", [128, 8, 2], F32)
    g1p1 = sb("g1p1", [128, 8, 2], F32)
    sc2p1 = sb("sc2p1", [128, 8, 2], F32)
    cvec = sb("cvec", [128, 24], F32)
    bvb = sb("bvb", [128, 128], F32)
    bout = sb("bout", [128, 8], F32)
    ln1 = sb("ln1", [128, 2, 8], F32)
    bd64 = sb("bd64", [128, 128], F32)
    ones128 = sb("ones128", [128, 128], F32)
    ones256 = sb("ones256", [128, 128], F32)
    ones1k = sb("ones1k", [128, 128], F32)
    mhalf = sb("mhalf", [128, 8], F32)
    epsc = sb("epsc", [128, 2], F32)
    ones65 = sb("ones65", [65, 64], F32)
    P = [ps("P%d" % i, [128, 512], F32) for i in range(8)]
    tP = [Tile("P%d" % i) for i in range(8)]
    tC = Tile("const"); tXB = Tile("XB"); tUB = Tile("UB")
    NKB = (NK + 511) // 512
    tKT = [Tile("KT%d" % i) for i in range(NKB)]
    tVG = [Tile("VG%d" % i) for i in range(NKB)]
    tKVN = [Tile("KVN%d" % i) for i in range(NKB)]
    tKR = [Tile("KR%d" % i) for i in range(NKB)]
    tVM = Tile("VM")
    tWQ = Tile("WQ"); tWU = Tile("WU"); tWOS = [Tile("WOS%d" % i) for i in range(2)]
    tQT = Tile("QT"); tQCN = Tile("QCN"); tQN = Tile("QN"); tQABS = Tile("QABS"); tQR = Tile("QR")
    tPT = [Tile("PT%d" % i) for i in range(3)]
    tAO = Tile("AO")
    tLT = [Tile("LT%d" % i) for i in range(4)]
    tR1 = Tile("R1"); tR2 = Tile("R2"); tRS = Tile("RS"); tCG = Tile("CG"); tTB = Tile("TB"); tTM = Tile("TM")
    tOTS = Tile("OTS"); tDROW = Tile("DROW")
    tOUTA = [Tile("OUTA%d" % i) for i in range(2)]
    tOUTB = [Tile("OUTB%d" % i) for i in range(2)]

    cst = dict(writes=[tC], chan="k0", grp=0)
    E("sp", "dma_start", out=modv[:], in_=d["modv"], **cst)
    E("sp", "dma_start", out=cvec[:], in_=d["cvec"], **cst)
    E("sp", "dma_start", out=bvb[:], in_=bass.AP(d["bv"].tensor, 0, [[0, 128], [1, 128]]), **cst)
    E("sp", "dma_start", out=bout[:], in_=d["bout"], **cst)
    E("sp", "dma_start", out=ln1[:], in_=d["ln1"], **cst)
    E("sp", "dma_start", out=bd64[:], in_=d["bd64"], **cst)
    E("pool", "dma_start", out=WQ[:, :, 0:576], in_=d["wa"], writes=[tWQ], chan="k2")
    cu = dict(writes=[tWU], chan="k3", grp=0)
    E("pool", "dma_start", out=WUQ[:], in_=d["wuq"], **cu)
    E("pool", "dma_start", out=WUKT[:], in_=d["wukt"], **cu)
    E("pool", "dma_start", out=WUV[:], in_=d["wuv"], **cu)
    E("dve", "memset", ones128[:], 1.0 / 128.0, writes=[tC])
    E("dve", "memset", ones256[:], 1.0 / 256.0, writes=[tC])
    E("dve", "memset", ones1k[:], 1.0 / 1024.0, writes=[tC])
    E("dve", "memset", epsc[:, 0:1], LN_EPS, writes=[tC])
    E("dve", "memset", epsc[:, 1:2], 1e-6, writes=[tC])
    E("dve", "memset", ones65[:], 1.0, writes=[tC])
    E("pool", "memset", VG[:, :, :, 64:65], 1.0, writes=tVG)
    E("pool", "memset", VM[:, :, :, 64:65], 1.0, writes=[tVM])
    E("pool", "memset", KR[:], 0.0, writes=tKR)
    E("pool", "memset", QR[:], 0.0, writes=[tQR])
    for g in range(2):
        E("pool", "memset", QTZ[g][:], 0.0, writes=[tQT])
    E("dve", "tensor_scalar", sc1p1[:], modv[:, 8:16, :], 1.0, None, ALU.add, reads=[tC], writes=[tC])
    E("dve", "tensor_scalar", g1p1[:], modv[:, 16:24, :], 1.0, None, ALU.add, reads=[tC], writes=[tC])
    E("dve", "tensor_scalar", sc2p1[:], modv[:, 32:40, :], 1.0, None, ALU.add, reads=[tC], writes=[tC])

    def make_u(n, m):
        for k in range(8):
            eng = ("dve", "act", "pool", "dve", "act", "dve", "act", "pool")[k]
            if eng == "act":
                E("act", "activation", out=UB[:, k, 0:n], in_=XB[:, k, 0:n], func=AF.Identity, scale=sc1p1[:, k, m:m + 1], bias=modv[:, k, m:m + 1],
                  reads=[tXB, tC], writes=[tUB])
            else:
                E(eng, "tensor_scalar", UB[:, k, 0:n], XB[:, k, 0:n], sc1p1[:, k, m:m + 1], modv[:, k, m:m + 1], ALU.mult, ALU.add,
                  reads=[tXB, tC], writes=[tUB])

    def rstd_from(psrc, np_, n, bias_ap, ones_ap, extra=None):
        for j, (pap, ptile, bap) in enumerate(psrc):
            E("act", "activation", out=LT[2][0:np_, 0:n], in_=pap, func=AF.Square, bias=bap, reads=[ptile, tC], writes=[tLT[2]])
            E("pe", "matmul", P[5][0:np_, 0:n], ones_ap, LT[2][0:np_, 0:n], start=(j == 0), stop=(j == len(psrc) - 1), reads=[tLT[2], tC], writes=[tP[5]])
        E("act", "activation", out=RS[0:np_, 0:n], in_=P[5][0:np_, 0:n], func=AF.Sqrt, bias=epsc[0:np_, 1:2], reads=[tP[5], tC], writes=[tRS])
        E("dve", "reciprocal", RS[0:np_, 0:n], RS[0:np_, 0:n], reads=[tRS], writes=[tRS])

    kblocks = [(c0, min(512, NK - c0)) for c0 in range(0, NK, 512)]
    for bi, (c0, n) in enumerate(kblocks):
        m = 0 if c0 < SEQ else 1
        E("sp", "dma_start", out=XB[:, :, 0:n], in_=(d["xk"](c0, n) if callable(d["xk"]) else d["xk"][:, :, c0:c0 + n]), writes=[tXB], chan="xb")
        tg = dict(writes=[tTB], chan="tb", grp=bi)
        E("sp", "dma_start", out=TB[0][:, 0:n], in_=d["tkc"][:, c0:c0 + n], **tg)
        E("sp", "dma_start", out=TB[1][:, 0:n], in_=d["tks"][:, c0:c0 + n], **tg)
        tg2 = dict(writes=[tTM], chan="tm", grp=bi)
        E("sp", "dma_start", out=TM[0][:, 0:n], in_=d["tmc"][:, c0:c0 + n], **tg2)
        E("sp", "dma_start", out=TM[1][:, 0:n], in_=d["tms"][:, c0:c0 + n], **tg2)
        make_u(n, m)
        for k in range(8):
            E("pe", "matmul", P[0][:, 0:n], WQ[:, k, 0:128], UB[:, k, 0:n], start=(k == 0), stop=(k == 7), reads=[tWQ, tUB], writes=[tP[0]])
        for k in range(8):
            E("pe", "matmul", P[1][:, 0:n], WQ[:, k, 128:256], UB[:, k, 0:n], start=(k == 0), stop=(k == 7), reads=[tWQ, tUB], writes=[tP[1]])
        rstd_from([(P[0][:, 0:n], tP[0], cvec[:, 0:1])], 128, n, None, bd64[:])
        E("dve", "tensor_scalar", CG[:, 0:n], TB[0][:, 0:n], cvec[:, 3:4], None, ALU.mult, reads=[tTB, tC], writes=[tCG])
        E("dve", "tensor_scalar", SG[:, 0:n], TB[1][:, 0:n], cvec[:, 4:5], None, ALU.mult, reads=[tTB, tC], writes=[tCG])
        E("dve", "scalar_tensor_tensor", R1[:, 0:n], P[0][:, 0:n], cvec[:, 0:1], CG[:, 0:n], ALU.add, ALU.mult, reads=[tP[0], tCG, tC], writes=[tR1])
        E("dve", "scalar_tensor_tensor", R2[:, 0:n], P[1][:, 0:n], cvec[:, 1:2], SG[:, 0:n], ALU.add, ALU.mult, reads=[tP[1], tCG, tC], writes=[tR2])
        E("pool", "tensor_tensor", R1[:, 0:n], R1[:, 0:n], R2[:, 0:n], ALU.add, reads=[tR1, tR2], writes=[tR1])
        E("pool", "tensor_tensor", KT[:, c0:c0 + n], R1[:, 0:n], RS[:, 0:n], ALU.mult, reads=[tR1, tRS], writes=[tKT[bi]])
        for tt in range(n // 128):
            for k in range(8):
                E("pe", "matmul", P[4][:, 0:128], UB[:, k, tt * 128:(tt + 1) * 128], WQ[:, k, 256:384], start=(k == 0), stop=(k == 7),
                  reads=[tWQ, tUB], writes=[tP[4]])
            E("dve", "tensor_tensor", VG[:, c0 // 128 + tt, :, 0:64], P[4][:, 0:128].rearrange("p (g e) -> p g e", g=2),
              bvb[:].rearrange("p (g e) -> p g e", g=2), ALU.add, reads=[tP[4], tC], writes=[tVG[bi]])
        for k in range(8):
            E("pe", "matmul", P[2][:, 0:n], WQ[:, k, 384:512], UB[:, k, 0:n], start=(k == 0), stop=(k == 7), reads=[tWQ, tUB], writes=[tP[2]])
        rstd_from([(P[2][:, 0:n], tP[2], cvec[:, 2:3])], 128, n, None, ones128[:])
        E("dve", "scalar_tensor_tensor", R1[:, 0:n], P[2][:, 0:n], cvec[:, 2:3], RS[:, 0:n], ALU.add, ALU.mult, reads=[tP[2], tRS, tC], writes=[tR1])
        E("act", "activation", out=KVN[:, c0:c0 + n], in_=R1[:, 0:n], func=AF.Identity, scale=cvec[:, 5:6], reads=[tR1, tC], writes=[tKVN[bi]])
        for k in range(8):
            E("pe", "matmul", P[3][0:32, 0:n], WQ[:, k, 512:544], UB[:, k, 0:n], start=(k == 0), stop=(k == 7), reads=[tWQ, tUB], writes=[tP[3]])
        for k in range(8):
            E("pe", "matmul", P[6][0:32, 0:n], WQ[:, k, 544:576], UB[:, k, 0:n], start=(k == 0), stop=(k == 7), reads=[tWQ, tUB], writes=[tP[6]])
        E("dve", "scalar_tensor_tensor", R1[0:32, 0:n], P[3][0:32, 0:n], cvec[0:32, 20:21], TM[0][:, 0:n], ALU.add, ALU.mult, reads=[tP[3], tTM, tC], writes=[tR1])
        E("dve", "scalar_tensor_tensor", R2[0:32, 0:n], P[6][0:32, 0:n], cvec[0:32, 21:22], TM[1][:, 0:n], ALU.add, ALU.mult, reads=[tP[6], tTM, tC], writes=[tR2])
        E("pool", "tensor_tensor", KR[0:32, c0:c0 + n], R1[0:32, 0:n], R2[0:32, 0:n], ALU.add, reads=[tR1, tR2], writes=[tKR[bi]])
    E("pool", "dma_start", out=WQ[:], in_=d["wq"], writes=[tWQ], chan="k2")

    pcnt = [0]

    def normalise(nq_ap_out, reads_extra=()):
        E("dve", "reciprocal", DROW[64:65, :], P[4][64:65, :], reads=[tP[4]], writes=[tDROW])
        E("pe", "matmul", P[5][0:64, :], ones65[64:65, :], DROW[64:65, :], start=True, stop=True, reads=[tDROW, tC], writes=[tP[5]])
        E("act", "copy", out=OTS[0:64, :], in_=P[4][0:64, :], reads=[tP[4]], writes=[tOTS])

    oblocks = [(t0, 512) for t0 in range(0, NL, 512)]
    for bi, (t0, n) in enumerate(oblocks):
        E("sp", "dma_start", out=XB[:, :, 0:n], in_=d["xo"][:, :, t0:t0 + n], writes=[tXB], chan="xb")
        tg = dict(writes=[tTB], chan="tb", grp=100 + bi)
        E("sp", "dma_start", out=TB[0][:, 0:n], in_=d["tqc"][:, t0:t0 + n], **tg)
        E("sp", "dma_start", out=TB[1][:, 0:n], in_=d["tqs"][:, t0:t0 + n], **tg)
        tg2 = dict(writes=[tTM], chan="tm", grp=100 + bi)
        E("sp", "dma_start", out=TM[0][:, 0:n], in_=d["tmqc"][:, t0:t0 + n], **tg2)
        E("sp", "dma_start", out=TM[1][:, 0:n], in_=d["tmqs"][:, t0:t0 + n], **tg2)
        make_u(n, 0)
        E("dve", "tensor_scalar", CG[:, 0:n], TB[0][:, 0:n], cvec[:, 16:17], None, ALU.mult, reads=[tTB, tC], writes=[tCG])
        E("dve", "tensor_scalar", SG[:, 0:n], TB[1][:, 0:n], cvec[:, 17:18], None, ALU.mult, reads=[tTB, tC], writes=[tCG])
        for i in range(4):
            for k in range(8):
                E("pe", "matmul", P[0][:, 0:n], WQ[:, k, i * 128:(i + 1) * 128], UB[:, k, 0:n], start=(k == 0), stop=(k == 7), reads=[tWQ, tUB], writes=[tP[0]])
            for k in range(8):
                E("pe", "matmul", P[1][:, 0:n], WQ[:, k, 512 + i * 128:512 + (i + 1) * 128], UB[:, k, 0:n], start=(k == 0), stop=(k == 7), reads=[tWQ, tUB], writes=[tP[1]])
            rstd_from([(P[0][:, 0:n], tP[0], cvec[:, 6 + i:7 + i])], 128, n, None, bd64[:])
            E("dve", "scalar_tensor_tensor", R1[:, 0:n], P[0][:, 0:n], cvec[:, 6 + i:7 + i], CG[:, 0:n], ALU.add, ALU.mult, reads=[tP[0], tCG, tC], writes=[tR1])
            E("dve", "scalar_tensor_tensor", R2[:, 0:n], P[1][:, 0:n], cvec[:, 10 + i:11 + i], SG[:, 0:n], ALU.add, ALU.mult, reads=[tP[1], tCG, tC], writes=[tR2])
            E("pool", "tensor_tensor", R1[:, 0:n], R1[:, 0:n], R2[:, 0:n], ALU.add, reads=[tR1, tR2], writes=[tR1])
            for g in range(2):
                E("pool", "tensor_tensor", QTZ[g][g * 64:(g + 1) * 64, :, i, :], R1[g * 64:(g + 1) * 64, 0:n].rearrange("p (t q) -> p t q", q=128),
                  RS[g * 64:(g + 1) * 64, 0:n].rearrange("p (t q) -> p t q", q=128), ALU.mult, reads=[tR1, tRS], writes=[tQT])
        groups = []
        for tl in range(n // 128):
            for g in range(2):
                gi = len(groups)
                acc, tacc, bc, tbc = P[4 + 2 * (gi % 2)], tP[4 + 2 * (gi % 2)], P[5 + 2 * (gi % 2)], tP[5 + 2 * (gi % 2)]
                rhs_q = QTZ[g][:, tl, :, :].rearrange("p i q -> p (i q)")

                def qk(kt, ps_, tps, g=g, rhs_q=rhs_q):
                    E("pe", "matmul", ps_[:, :], KT[:, kt * 128:(kt + 1) * 128], rhs_q, start=True, stop=True,
                      reads=[tQT, tKT[kt // 4]], writes=[tps])

                def pv(kt, pt, tpt, st, sp, g=g, acc=acc, tacc=tacc):
                    E("pe", "matmul", acc[0:65, :], VG[:, kt, g, :], pt[:], start=st, stop=sp, reads=[tpt, tVG[kt // 4]], writes=[tacc])

                def fin1(acc=acc, tacc=tacc):
                    E("dve", "reciprocal", DROW[64:65, :], acc[64:65, :], reads=[tacc], writes=[tDROW])
                    E("act", "copy", out=OTS[0:64, :], in_=acc[0:64, :], reads=[tacc], writes=[tOTS])

                def fin2(bc=bc, tbc=tbc):
                    E("pe", "matmul", bc[0:64, :], ones65[64:65, :], DROW[64:65, :], start=True, stop=True, reads=[tDROW, tC], writes=[tbc])

                def fin3(bc=bc, tbc=tbc, g=g, tl=tl):
                    E("dve", "tensor_tensor", AO[:, g * 4:(g + 1) * 4, tl * 128:(tl + 1) * 128], OTS[0:64, :].rearrange("p (i q) -> p i q", q=128),
                      bc[0:64, :].rearrange("p (i q) -> p i q", q=128), ALU.mult, reads=[tOTS, tbc], writes=[tAO])

                groups.append(dict(pre=None, nkt=NKT, qk=qk, scale=0.125, pv=pv, fin1=fin1, fin2=fin2, fin3=fin3))
        attn_pipeline(E, groups, [P[2], P[3]], [tP[2], tP[3]], PT, tPT, 512)
        for j in range(2):
            for k in range(8):
                E("pe", "matmul", P[j][:, 0:n], WQ[:, k, 1024 + j * 128:1024 + (j + 1) * 128], UB[:, k, 0:n], start=(k == 0), stop=(k == 7),
                  reads=[tWQ, tUB], writes=[tP[j]])
        rstd_from([(P[0][:, 0:n], tP[0], cvec[:, 14:15]), (P[1][:, 0:n], tP[1], cvec[:, 15:16])], 128, n, None, ones256[:])
        for j in range(2):
            E("dve", "scalar_tensor_tensor", R1[:, 0:n], P[j][:, 0:n], cvec[:, 14 + j:15 + j], RS[:, 0:n], ALU.add, ALU.mult, reads=[tP[j], tRS, tC], writes=[tR1])
            E("act", "activation", out=QCN[:, j, 0:n], in_=R1[:, 0:n], func=AF.Identity, scale=cvec[:, 18 + j:19 + j], reads=[tR1, tC], writes=[tQCN])
        for hp in range(2):
            for kt in range(NKT):
                pb = 6 + (kt % 2)
                E("pe", "matmul", P[pb][:, 0:256], KVN[:, kt * 128:(kt + 1) * 128], WUV[:, hp * 256:(hp + 1) * 256], start=True, stop=True,
                  reads=[tKVN[kt // 4], tWU], writes=[tP[pb]])
                eng = "act" if kt % 2 == 0 else "dve"
                if eng == "act":
                    E("act", "copy", out=VM[:, kt, :, 0:64], in_=P[pb][:, 0:256].rearrange("p (h e) -> p h e", h=4), reads=[tP[pb]], writes=[tVM])
                else:
                    E("dve", "tensor_copy", out=VM[:, kt, :, 0:64], in_=P[pb][:, 0:256].rearrange("p (h e) -> p h e", h=4), reads=[tP[pb]], writes=[tVM])
            groups = []
            for hl in range(4):
                h = hp * 4 + hl
                acc, tacc = P[4 + (hl % 2)], tP[4 + (hl % 2)]

                def pre(h=h):
                    for j in range(2):
                        E("pe", "matmul", P[7][0:32, 0:n], WUQ[:, j, h, 64:96], QCN[:, j, 0:n], start=(j == 0), stop=(j == 1), reads=[tWU, tQCN], writes=[tP[7]])
                    for j in range(2):
                        E("pe", "matmul", P[1][0:32, 0:n], WUQ[:, j, h, 96:128], QCN[:, j, 0:n], start=(j == 0), stop=(j == 1), reads=[tWU, tQCN], writes=[tP[1]])
                    for j in range(2):
                        E("pe", "matmul", P[0][0:64, 0:n], WUQ[:, j, h, 0:64], QCN[:, j, 0:n], start=(j == 0), stop=(j == 1), reads=[tWU, tQCN], writes=[tP[0]])
                    E("act", "copy", out=QN[:, 0:n], in_=P[0][0:64, 0:n], reads=[tP[0]], writes=[tQN])
                    E("dve", "tensor_tensor", R1[0:32, 0:n], P[7][0:32, 0:n], TM[0][:, 0:n], ALU.mult, reads=[tP[7], tTM], writes=[tR1])
                    E("dve", "tensor_tensor", R2[0:32, 0:n], P[1][0:32, 0:n], TM[1][:, 0:n], ALU.mult, reads=[tP[1], tTM], writes=[tR2])
                    E("pool", "tensor_tensor", QR[0:32, 0:n], R1[0:32, 0:n], R2[0:32, 0:n], ALU.add, reads=[tR1, tR2], writes=[tQR])
                    E("pe", "matmul", P[1][:, 0:n], WUKT[:, h, :], QN[:, 0:n], start=True, stop=True, reads=[tWU, tQN], writes=[tP[1]])
                    E("act", "copy", out=QABS[:, 0:n], in_=P[1][:, 0:n], reads=[tP[1]], writes=[tQABS])

                def qk(kt, ps_, tps):
                    E("pe", "matmul", ps_[:, 0:n], KVN[:, kt * 128:(kt + 1) * 128], QABS[:, 0:n], start=True, stop=False,
                      reads=[tQABS, tKVN[kt // 4]], writes=[tps])
                    E("pe", "matmul", ps_[:, 0:n], KR[:, kt * 128:(kt + 1) * 128], QR[:, 0:n], start=False, stop=True,
                      reads=[tQR, tKR[kt // 4]], writes=[tps])

                def pv(kt, pt, tpt, st, sp, hl=hl, acc=acc, tacc=tacc):
                    E("pe", "matmul", acc[0:65, 0:n], VM[:, kt, hl, :], pt[:, 0:n], start=st, stop=sp, reads=[tpt, tVM], writes=[tacc])

                def fin1(acc=acc, tacc=tacc):
                    E("dve", "reciprocal", DROW[64:65, :], acc[64:65, :], reads=[tacc], writes=[tDROW])
                    E("act", "copy", out=OTS[0:64, :], in_=acc[0:64, :], reads=[tacc], writes=[tOTS])

                def fin2():
                    E("pe", "matmul", P[6][0:64, :], ones65[64:65, :], DROW[64:65, :], start=True, stop=True, reads=[tDROW, tC], writes=[tP[6]])

                def fin3(h=h):
                    E("dve", "tensor_tensor", AO[:, 8 + h, 0:n], OTS[0:64, 0:n], P[6][0:64, 0:n], ALU.mult, reads=[tOTS, tP[6]], writes=[tAO])

                groups.append(dict(pre=pre, nkt=NKT, qk=qk, scale=float(96 ** -0.5), pv=pv, fin1=fin1, fin2=fin2, fin3=fin3))
            attn_pipeline(E, groups, [P[2], P[3]], [tP[2], tP[3]], PT, tPT, n)
        for dk in range(8):
            pb = 6 + (dk % 2); wb = dk % 2
            E("pool", "dma_start", out=WOS[wb][:], in_=d["wo"][:, :, dk * 128:(dk + 1) * 128], writes=[tWOS[wb]], chan="wo%d" % wb)
            for hh in range(16):
                E("pe", "matmul", P[pb][:, 0:n], WOS[wb][:, hh, :], AO[:, hh, 0:n], start=(hh == 0), stop=(hh == 15),
                  reads=[tWOS[wb], tAO], writes=[tP[pb]])
            E("dve", "tensor_scalar", LT[3][:, 0:n], P[pb][:, 0:n], bout[:, dk:dk + 1], g1p1[:, dk, 0:1], ALU.add, ALU.mult,
              reads=[tP[pb], tC], writes=[tLT[3]])
            E("dve", "scalar_tensor_tensor", XB[:, dk, 0:n], XB[:, dk, 0:n], DN_ALPHA, LT[3][:, 0:n], ALU.mult, ALU.add,
              reads=[tXB, tLT[3]], writes=[tXB])
        for k in range(8):
            E("pe", "matmul", P[5][:, 0:n], ones1k[:], XB[:, k, 0:n], start=(k == 0), stop=(k == 7), reads=[tXB, tC], writes=[tP[5]])
        for k in range(8):
            E("act", "activation", out=LT[2][:, 0:n], in_=XB[:, k, 0:n], func=AF.Square, reads=[tXB], writes=[tLT[2]])
            E("pe", "matmul", P[4][:, 0:n], ones1k[:], LT[2][:, 0:n], start=(k == 0), stop=(k == 7), reads=[tLT[2], tC], writes=[tP[4]])
        E("act", "copy", out=LT[0][:, 0:n], in_=P[5][:, 0:n], reads=[tP[5]], writes=[tLT[0]])
        E("dve", "tensor_tensor", LT[1][:, 0:n], LT[0][:, 0:n], LT[0][:, 0:n], ALU.mult, reads=[tLT[0]], writes=[tLT[1]])
        E("dve", "tensor_tensor", LT[1][:, 0:n], P[4][:, 0:n], LT[1][:, 0:n], ALU.subtract, reads=[tP[4], tLT[1]], writes=[tLT[1]])
        E("act", "activation", out=LT[1][:, 0:n], in_=LT[1][:, 0:n], func=AF.Sqrt, bias=epsc[:, 0:1], reads=[tLT[1], tC], writes=[tLT[1]])
        E("dve", "reciprocal", LT[1][:, 0:n], LT[1][:, 0:n], reads=[tLT[1]], writes=[tLT[1]])
        for k in range(8):
            ob = k % 2
            E("dve", "tensor_tensor", XB[:, k, 0:n], XB[:, k, 0:n], LT[0][:, 0:n], ALU.subtract, reads=[tXB, tLT[0]], writes=[tXB])
            E("pool", "tensor_tensor", XB[:, k, 0:n], XB[:, k, 0:n], LT[1][:, 0:n], ALU.mult, reads=[tXB, tLT[1]], writes=[tXB])
            E("act", "activation", out=XB[:, k, 0:n], in_=XB[:, k, 0:n], func=AF.Identity, scale=ln1[:, 0, k:k + 1], bias=ln1[:, 1, k:k + 1],
              reads=[tXB, tC], writes=[tXB])
            E("act", "mul", OUTA[ob][:, 0:n], XB[:, k, 0:n], DN_ALPHA, reads=[tXB], writes=[tOUTA[ob]])
            E("dve", "tensor_scalar", OUTB[ob][:, 0:n], XB[:, k, 0:n], sc2p1[:, k, 0:1], modv[:, 24 + k, 0:1], ALU.mult, ALU.add,
              reads=[tXB, tC], writes=[tOUTB[ob]])
            E("sp", "dma_start", out=d["xa"][:, k, t0:t0 + n], in_=OUTA[ob][:, 0:n], reads=[tOUTA[ob]], chan="oa%d" % ob)
            E("sp", "dma_start", out=d["u2f"][:, k, t0:t0 + n], in_=OUTB[ob][:, 0:n], reads=[tOUTB[ob]], chan="ob%d" % ob)
    S.flush()
    es.close()


F32 = mybir.dt.float32; BF16 = mybir.dt.bfloat16
AF = mybir.ActivationFunctionType
ALU = mybir.AluOpType
AX = mybir.AxisListType
NE = 32
LN_EPS = 1e-5


def emit_moe(nc, S, T, d, nctx=0, tag=""):
    blocks = []
    t0 = 0
    while t0 < T - nctx:
        n = min(512, T - nctx - t0)
        blocks.append((t0, n))
        t0 += n
    bmode = [0] * len(blocks)
    if nctx:
        blocks.append((T - nctx, nctx))
        bmode.append(1)
    NB = len(blocks)
    es = ExitStack()
    sb = lambda name, shape, dt: es.enter_context(nc.sbuf_tensor("moe" + tag + "_" + name, shape, dt))
    ps = lambda name, shape, dt: es.enter_context(nc.psum_tensor("moep" + tag + "_" + name, shape, dt))
    XACC = sb("XACC", [128, 8, T], F32)
    U2 = sb("U2", [128, 8, T], BF16)
    WTT = sb("WTT", [64, T], BF16)
    u2f = [sb("u2f%d" % i, [128, 8, 128], F32) for i in range(2)]
    wr = sb("wr", [128, 8, 36], F32)
    brow = sb("brow", [128, 36], F32)
    ident = sb("ident", [128, 128], BF16)
    sel = sb("sel", [64, NE, 128], BF16)
    g2p1 = sb("g2p1", [128, 8, 2], F32)
    modv = sb("modv", [128, 48, 2], F32)
    ln2 = sb("ln2", [128, 2, 8], F32)
    ones_f = sb("ones_f", [128, 128], F32)
    mhalf = sb("mhalf", [128, 8], F32)
    epsc = sb("epsc", [128, 2], F32)
    W1 = [sb("W1_%d" % i, [128, 8, 512], BF16) for i in range(2)]
    W3 = [sb("W3_%d" % i, [128, 8, 512], BF16) for i in range(2)]
    W2 = [sb("W2_%d" % i, [128, 4, 1024], BF16) for i in range(2)]
    HID = [sb("HID%d" % i, [128, 4, 512], BF16) for i in range(2)]
    SIG = [sb("SIG%d" % i, [128, 512], BF16) for i in range(2)]
    TM = [sb("TM%d" % i, [128, 512], F32) for i in range(2)]
    MSK = [sb("MSK%d" % i, [128, 512], F32) for i in range(2)]
    RS = sb("RS", [128, 128], F32)
    WHL = sb("WHL", [128, 64], BF16)
    RS2 = sb("RS2", [128, 128], F32)
    WHL2 = sb("WHL2", [128, 64], BF16)
    RS3 = sb("RS3", [128, 128], F32)
    WHL3 = sb("WHL3", [128, 64], BF16)
    RS4 = sb("RS4", [128, 128], F32)
    WHL4 = sb("WHL4", [128, 64], BF16)
    PH1 = [ps("PH1_%d" % i, [128, 512], F32) for i in range(2)]
    PH3 = [ps("PH3_%d" % i, [128, 512], F32) for i in range(2)]
    PM = ps("PM", [128, 512], F32)
    PO = [ps("PO%d" % i, [128, 512], F32) for i in range(2)]
    PTR = ps("PTR", [128, 128], BF16)

    tXACC = [[Tile("xacc%d_%d" % (k, b)) for b in range(NB)] for k in range(8)]
    tU2 = [Tile("u2_%d" % b) for b in range(NB)]
    tWTT = [Tile("wtt_%d" % b) for b in range(NB)]
    tu2f = [Tile("u2f%d" % i) for i in range(2)]
    tconst = Tile("const")
    tW1 = [Tile("w1_%d" % i) for i in range(2)]
    tW3 = [Tile("w3_%d" % i) for i in range(2)]
    tW2 = [Tile("w2_%d" % i) for i in range(2)]
    tHID = [[Tile("hid%d_%d" % (i, f)) for f in range(4)] for i in range(2)]
    tSIG = [Tile("sig%d" % i) for i in range(2)]
    tTM = [Tile("tm%d" % i) for i in range(2)]
    tMSK = [Tile("msk%d" % i) for i in range(2)]
    SQ = MSK[1]; tSQ = tMSK[1]; LT = [TM[0], TM[1], MSK[0]]; tLT = [tTM[0], tTM[1], tMSK[0]]
    tRS = Tile("rs"); tWHL = Tile("whl")
    tPH1 = [Tile("ph1_%d" % i) for i in range(2)]
    tPH3 = [Tile("ph3_%d" % i) for i in range(2)]
    tPM = Tile("pm")
    tPO = [Tile("po%d" % i) for i in range(2)]
    tPTR = Tile("ptr")

    S.op("sp", lambda e: e.dma_start(out=wr[:], in_=d["wr"]), writes=[tconst], chan="c0", grp=0)
    S.op("sp", lambda e: e.dma_start(out=brow[:], in_=bass.AP(d["brow"].tensor, 0, [[0, 128], [1, 36]])), writes=[tconst], chan="c0", grp=0)
    S.op("sp", lambda e: e.dma_start(out=modv[:], in_=d["modv"]), writes=[tconst], chan="c0", grp=0)
    S.op("sp", lambda e: e.dma_start(out=ln2[:], in_=d["ln2"]), writes=[tconst], chan="c0", grp=0)
    S.op("pool", lambda e: e.dma_start(out=ident[:], in_=d["ident"]), writes=[tconst], chan="c1", grp=0)
    S.op("pool", lambda e: e.dma_start(out=sel[:], in_=d["sel"]), writes=[tconst], chan="c1", grp=0)
    S.op("dve", lambda e: e.tensor_scalar(g2p1[:], modv[:, 40:48, :], 1.0, None, ALU.add), reads=[tconst], writes=[tconst])
    S.op("dve", lambda e: e.memset(ones_f[:], 1.0 / 1024.0), writes=[tconst])
    S.op("dve", lambda e: e.memset(epsc[:, 0:1], LN_EPS), writes=[tconst])
    for k in range(8):
        for b, (t0, n) in enumerate(blocks):
            pass
    S.op("pool", lambda e: e.dma_start(out=W1[0][:], in_=d["w1"][0].rearrange("(k p) f -> p k f", p=128)), writes=[tW1[0]], chan="w1_0")
    S.op("pool", lambda e: e.dma_start(out=W3[0][:], in_=d["w3"][0].rearrange("(k p) f -> p k f", p=128)), writes=[tW3[0]], chan="w3_0")
    S.op("pool", lambda e: e.dma_start(out=W2[0][:], in_=d["w2"][0].rearrange("(k p) f -> p k f", p=128)), writes=[tW2[0]], chan="w2_0")

    NS = 4
    RSs = [RS, RS2, RS3, RS4]; WHLs = [WHL, WHL2, WHL3, WHL4]
    tRSs = [Tile("rs%d" % j) for j in range(NS)]; tWHLs = [Tile("whl%d" % j) for j in range(NS)]
    tPMs = [tPM for j in range(NS)]; tPTRs = [tPTR for j in range(NS)]; tSQs = [Tile("sqr%d" % j) for j in range(NS)]
    rtiles = [(b, t0, tt * 128) for b, (t0, n) in enumerate(blocks) for tt in range(n // 128)]

    def router_tile(idx, b, t0, c0):
        i = idx % NS
        ub = idx % 2
        ops = []
        A = ops.append
        RSj = RSs[i]; WHLj = WHLs[i]
        PMj = PM[:, 0:36]; PTRj = PTR[0:64, 0:128]
        R = [tRSs[i]]; a = t0 + c0
        A(lambda: S.op("sp", lambda e: e.dma_start(out=u2f[ub][:], in_=d["u2f"][:, :, a:a + 128]), writes=[tu2f[ub]], chan="u2f%d" % ub))
        if ub == 0:
            A(lambda: S.op("act", lambda e: e.copy(out=U2[:, :, a:a + 128], in_=u2f[ub][:]), reads=[tu2f[ub]], writes=[tU2[b]]))
        else:
            A(lambda: S.op("pool", lambda e: e.tensor_copy(out=U2[:, :, a:a + 128], in_=u2f[ub][:]), reads=[tu2f[ub]], writes=[tU2[b]]))

        def mm():
            for k in range(8):
                S.op("pe", (lambda k: lambda e: e.matmul(PMj, u2f[ub][:, k, :], wr[:, k, :], start=(k == 0), stop=(k == 7)))(k),
                     reads=[tu2f[ub], tconst], writes=[tPMs[i]])
        A(mm)
        L = RSj[:, 0:36]; gmax = RSj[:, 36:37]; ohg = RSj[:, 40:44]; dg = RSj[:, 44:48]; gsum = RSj[:, 48:49]; gw = RSj[:, 49:50]
        el = RSj[:, 52:60]; m1 = RSj[:, 60:61]; oh1 = RSj[:, 64:72]; el2 = RSj[:, 72:80]; m2 = RSj[:, 80:81]; oh2 = RSj[:, 84:92]
        d12 = RSj[:, 92:93]; ee = RSj[:, 93:94]; den = RSj[:, 94:95]; w1s = RSj[:, 95:96]; w2s = RSj[:, 96:97]; a1 = RSj[:, 97:98]; a2 = RSj[:, 98:99]
        tq = RSj[:, 100:108]; ing = RSj[:, 108:116]
        wtok = SQ[:, 32 * i:32 * i + 32]
        D = lambda fn, reads=R, writes=R: A(lambda: S.op("dve", fn, reads=reads, writes=writes))
        D(lambda e: e.tensor_tensor(L, PMj, brow[:], ALU.add), reads=[tPMs[i], tconst])
        D(lambda e: e.tensor_reduce(gmax, RSj[:, 0:4], AX.X, ALU.max))
        D(lambda e: e.tensor_scalar(ohg, RSj[:, 0:4], gmax, None, ALU.is_equal))
        D(lambda e: e.tensor_scalar(dg, RSj[:, 0:4], gmax, None, ALU.subtract))
        A(lambda: S.op("act", lambda e: e.activation(out=dg, in_=dg, func=AF.Exp, accum_out=gsum), reads=R, writes=R))
        D(lambda e: e.reciprocal(gw, gsum))
        D(lambda e: e.tensor_scalar(el, RSj[:, 4:12], RSj[:, 40:41], None, ALU.mult))
        for g in range(1, 4):
            D((lambda g: lambda e: e.scalar_tensor_tensor(el, RSj[:, 4 + 8 * g:12 + 8 * g], RSj[:, 40 + g:41 + g], el, ALU.mult, ALU.add))(g))
        D(lambda e: e.tensor_reduce(m1, el, AX.X, ALU.max))
        D(lambda e: e.tensor_scalar(oh1, el, m1, None, ALU.is_equal))
        D(lambda e: e.scalar_tensor_tensor(el2, oh1, -1e30, el, ALU.mult, ALU.add))
        D(lambda e: e.tensor_reduce(m2, el2, AX.X, ALU.max))
        D(lambda e: e.tensor_scalar(oh2, el2, m2, None, ALU.is_equal))
        D(lambda e: e.tensor_tensor(d12, m2, m1, ALU.subtract))
        A(lambda: S.op("act", lambda e: e.activation(out=ee, in_=d12, func=AF.Exp), reads=R, writes=R))
        D(lambda e: e.tensor_scalar(den, ee, 1.0, None, ALU.add))
        D(lambda e: e.reciprocal(w1s, den))
        D(lambda e: e.tensor_tensor(w2s, ee, w1s, ALU.mult))
        D(lambda e: e.tensor_tensor(a1, w1s, gw, ALU.mult))
        D(lambda e: e.tensor_tensor(a2, w2s, gw, ALU.mult))
        D(lambda e: e.tensor_scalar(tq, oh1, a1, None, ALU.mult))
        D(lambda e: e.scalar_tensor_tensor(ing, oh2, a2, tq, ALU.mult, ALU.add))
        for g in range(4):
            D((lambda g: lambda e: e.tensor_scalar(wtok[:, 8 * g:8 * g + 8], ing, RSj[:, 40 + g:41 + g], None, ALU.mult))(g), writes=[tSQs[i]])
        D(lambda e: e.tensor_copy(out=WHLj[:, 0:32], in_=wtok), reads=[tSQs[i]], writes=[tWHLs[i]])
        D(lambda e: e.tensor_tensor(WHLj[:, 32:64], wtok, WHLj[:, 0:32], ALU.subtract), reads=[tSQs[i], tWHLs[i]], writes=[tWHLs[i]])
        A(lambda: S.op("pe", lambda e: e.transpose(PTRj, WHLj[:, :], ident[:]), reads=[tWHLs[i], tconst], writes=[tPTRs[i]]))
        A(lambda: S.op("act", lambda e: e.copy(out=WTT[:, a:a + 128], in_=PTRj), reads=[tPTRs[i]], writes=[tWTT[b]]))
        return ops

    for base in range(0, len(rtiles), NS):
        lists = [router_tile(base + u, *rtiles[base + u]) for u in range(NS) if base + u < len(rtiles)]
        LAG = 3
        for step in range(max(len(l) for l in lists) + LAG * (len(lists) - 1)):
            for u, l in enumerate(lists):
                if 0 <= step - LAG * u < len(l):
                    l[step - LAG * u]()

    def load_w(e_, slot):
        S.op("pool", lambda e: e.dma_start(out=W1[slot][:], in_=d["w1"][e_].rearrange("(k p) f -> p k f", p=128)), writes=[tW1[slot]], chan="w1_%d" % slot)
        S.op("pool", lambda e: e.dma_start(out=W3[slot][:], in_=d["w3"][e_].rearrange("(k p) f -> p k f", p=128)), writes=[tW3[slot]], chan="w3_%d" % slot)
        S.op("pool", lambda e: e.dma_start(out=W2[slot][:], in_=d["w2"][e_].rearrange("(k p) f -> p k f", p=128)), writes=[tW2[slot]], chan="w2_%d" % slot)

    def load_w13(e_, slot):
        S.op("pool", lambda e: e.dma_start(out=W1[slot][:], in_=d["w1"][e_].rearrange("(k p) f -> p k f", p=128)), writes=[tW1[slot]], chan="w1_%d" % slot)
        S.op("pool", lambda e: e.dma_start(out=W3[slot][:], in_=d["w3"][e_].rearrange("(k p) f -> p k f", p=128)), writes=[tW3[slot]], chan="w3_%d" % slot)

    def load_w2(e_, slot):
        S.op("pool", lambda e: e.dma_start(out=W2[slot][:], in_=d["w2"][e_].rearrange("(k p) f -> p k f", p=128)), writes=[tW2[slot]], chan="w2_%d" % slot)

    ocnt = [0]

    def phase1(ex, b, hb):
        slot = ex % 2
        t0, n = blocks[b]
        S.op("pe", (lambda ex, t0, n: lambda e: e.matmul(PM[:, 0:n], sel[:, ex, :], WTT[:, t0:t0 + n], start=True, stop=True))(ex, t0, n),
             reads=[tconst, tWTT[b]], writes=[tPM] + tPMs)
        S.op("act", (lambda hb, n: lambda e: e.copy(out=MSK[hb][:, 0:n], in_=PM[:, 0:n]))(hb, n), reads=[tPM], writes=[tMSK[hb]] + (tSQs if hb == 1 else []))
        for fc in range(4):
            pb = fc % 2
            for k in range(8):
                S.op("pe", (lambda slot, fc, k, pb, t0, n: lambda e: e.matmul(PH1[pb][:, 0:n], W1[slot][:, k, fc * 128:(fc + 1) * 128], U2[:, k, t0:t0 + n], start=(k == 0), stop=(k == 7)))(slot, fc, k, pb, t0, n),
                     reads=[tW1[slot], tU2[b]], writes=[tPH1[pb]])
            for k in range(8):
                S.op("pe", (lambda slot, fc, k, pb, t0, n: lambda e: e.matmul(PH3[pb][:, 0:n], W3[slot][:, k, fc * 128:(fc + 1) * 128], U2[:, k, t0:t0 + n], start=(k == 0), stop=(k == 7)))(slot, fc, k, pb, t0, n),
                     reads=[tW3[slot], tU2[b]], writes=[tPH3[pb]])
            S.op("act", (lambda pb, n: lambda e: e.activation(out=SIG[pb][:, 0:n], in_=PH1[pb][:, 0:n], func=AF.Silu))(pb, n), reads=[tPH1[pb]], writes=[tSIG[pb]])
            S.op("dve", (lambda pb, hb, n: lambda e: e.tensor_tensor(TM[pb][:, 0:n], PH3[pb][:, 0:n], MSK[hb][:, 0:n], ALU.mult))(pb, hb, n), reads=[tPH3[pb], tMSK[hb]], writes=[tTM[pb]])
            S.op("pool", (lambda pb, hb, fc, n: lambda e: e.tensor_tensor(HID[hb][:, fc, 0:n], SIG[pb][:, 0:n], TM[pb][:, 0:n], ALU.mult))(pb, hb, fc, n), reads=[tSIG[pb], tTM[pb]], writes=[tHID[hb][fc]])

    def phase2(ex, b, hb):
        slot = ex % 2
        t0, n = blocks[b]
        for dk in range(8):
            ob = ocnt[0] % 2
            ocnt[0] += 1
            for fc in range(4):
                S.op("pe", (lambda slot, fc, dk, ob, hb, n: lambda e: e.matmul(PO[ob][:, 0:n], W2[slot][:, fc, dk * 128:(dk + 1) * 128], HID[hb][:, fc, 0:n], start=(fc == 0), stop=(fc == 3)))(slot, fc, dk, ob, hb, n),
                     reads=[tW2[slot], tHID[hb][fc]], writes=[tPO[ob]])
            S.op("dve", (lambda dk, ob, t0, n, m: lambda e: e.scalar_tensor_tensor(XACC[:, dk, t0:t0 + n], PO[ob][:, 0:n], g2p1[:, dk, m:m + 1], XACC[:, dk, t0:t0 + n], ALU.mult, ALU.add))(dk, ob, t0, n, bmode[b]),
                 reads=[tPO[ob], tconst, tXACC[dk][b]], writes=[tXACC[dk][b]])

    for k in range(8):
        S.op("sp", (lambda k: lambda e: e.dma_start(out=XACC[:, k, :], in_=d["xa"][:, k, :]))(k),
             writes=[tXACC[k][b] for b in range(NB)], chan="xa%d" % (k % 2), grp=0)
    items = [(ex, b) for ex in range(NE) for b in range(NB)]
    prev = None
    for idx, (ex, b) in enumerate(items):
        hb = idx % 2
        if b == 0 and ex + 1 < NE:
            load_w13(ex + 1, (ex + 1) % 2)
        phase1(ex, b, hb)
        if prev is not None:
            phase2(*prev)
        if b == 0 and ex + 1 < NE:
            load_w2(ex + 1, (ex + 1) % 2)
        prev = (ex, b, hb)
    phase2(*prev)

    for b, (t0, n) in enumerate(blocks):
        for k in range(8):
            S.op("pe", (lambda k, t0, n: lambda e: e.matmul(PH1[0][:, 0:n], ones_f[:], XACC[:, k, t0:t0 + n], start=(k == 0), stop=(k == 7)))(k, t0, n),
                 reads=[tconst, tXACC[k][b]], writes=[tPH1[0]])
        for k in range(8):
            S.op("act", (lambda k, t0, n: lambda e: e.activation(out=SQ[:, 0:n], in_=XACC[:, k, t0:t0 + n], func=AF.Square))(k, t0, n), reads=[tXACC[k][b]], writes=[tSQ])
            S.op("pe", (lambda k, n: lambda e: e.matmul(PH3[0][:, 0:n], ones_f[:], SQ[:, 0:n], start=(k == 0), stop=(k == 7)))(k, n), reads=[tconst, tSQ], writes=[tPH3[0]])
        mean, var, rstd = LT[0], LT[1], LT[2]
        S.op("act", (lambda n: lambda e: e.copy(out=mean[:, 0:n], in_=PH1[0][:, 0:n]))(n), reads=[tPH1[0]], writes=[tLT[0]])
        S.op("dve", (lambda n: lambda e: e.tensor_tensor(var[:, 0:n], mean[:, 0:n], mean[:, 0:n], ALU.mult))(n), reads=[tLT[0]], writes=[tLT[1]])
        S.op("dve", (lambda n: lambda e: e.tensor_tensor(var[:, 0:n], PH3[0][:, 0:n], var[:, 0:n], ALU.subtract))(n), reads=[tPH3[0], tLT[1]], writes=[tLT[1]])
        S.op("act", (lambda n: lambda e: e.activation(out=rstd[:, 0:n], in_=var[:, 0:n], func=AF.Sqrt, bias=epsc[:, 0:1]))(n), reads=[tLT[1], tconst], writes=[tLT[2]])
        S.op("dve", (lambda n: lambda e: e.reciprocal(rstd[:, 0:n], rstd[:, 0:n]))(n), reads=[tLT[2]], writes=[tLT[2]])
        for k in range(8):
            S.op("dve", (lambda k, t0, n: lambda e: e.tensor_tensor(XACC[:, k, t0:t0 + n], XACC[:, k, t0:t0 + n], mean[:, 0:n], ALU.subtract))(k, t0, n),
                 reads=[tXACC[k][b], tLT[0]], writes=[tXACC[k][b]])
            S.op("pool", (lambda k, t0, n: lambda e: e.tensor_tensor(XACC[:, k, t0:t0 + n], XACC[:, k, t0:t0 + n], rstd[:, 0:n], ALU.mult))(k, t0, n),
                 reads=[tXACC[k][b], tLT[2]], writes=[tXACC[k][b]])
            S.op("act", (lambda k, t0, n: lambda e: e.activation(out=XACC[:, k, t0:t0 + n], in_=XACC[:, k, t0:t0 + n], func=AF.Identity, scale=ln2[:, 0, k:k + 1], bias=ln2[:, 1, k:k + 1]))(k, t0, n),
                 reads=[tXACC[k][b], tconst], writes=[tXACC[k][b]])
            if bmode[b] == 0:
                dst = d["xs"][:, k, t0:t0 + n]
            else:
                dst = d["xsc"][:, k, 0:n]
            S.op("sp", (lambda dst, k, t0, n: lambda e: e.dma_start(out=dst, in_=XACC[:, k, t0:t0 + n]))(dst, k, t0, n),
                 reads=[tXACC[k][b]], chan="out%d" % (k % 2), grp=b)
    S.flush()
    es.close()


F32 = mybir.dt.float32
AF = mybir.ActivationFunctionType
ALU = mybir.AluOpType


def emit_ada(nc, S, d):
    E = mkE(S)
    es = ExitStack()
    sb = lambda name, shape, dt: es.enter_context(nc.sbuf_tensor("ada_" + name, shape, dt))
    ps = lambda name, shape, dt: es.enter_context(nc.psum_tensor("adap_" + name, shape, dt))
    SC = sb("SC", [128, 8, 2], F32)
    AB = sb("AB", [128, 2, 48], F32)
    AW = [sb("AW%d" % i, [128, 8, 1024], F32) for i in range(2)]
    MV = sb("MV", [128, 48, 2], F32)
    PA = ps("PA", [128, 96], F32)
    tC = Tile("c"); tAW = [Tile("aw%d" % i) for i in range(2)]; tPA = Tile("pa"); tMV = Tile("mv")
    E("sp", "dma_start", out=SC[:], in_=d["cin"], writes=[tC], chan="a0", grp=0)
    E("sp", "dma_start", out=AB[:], in_=d["adab"], writes=[tC], chan="a0", grp=0)
    E("act", "activation", out=SC[:], in_=SC[:], func=AF.Silu, reads=[tC], writes=[tC])
    cnt = 0
    for l in range(2):
        for fg in range(6):
            i = cnt % 2
            cnt += 1
            src = d["adaw"][l].rearrange("(k p) f -> p k f", p=128)[:, :, fg * 1024:(fg + 1) * 1024]
            E("sp" if i == 0 else "act", "dma_start", out=AW[i][:], in_=src, writes=[tAW[i]], chan="aw%d" % i)
            for jj in range(8):
                j = fg * 8 + jj
                for k in range(8):
                    E("pe", "matmul", PA[:, 2 * j:2 * j + 2], AW[i][:, k, jj * 128:(jj + 1) * 128], SC[:, k, :], start=(k == 0), stop=(k == 7),
                      reads=[tAW[i], tC], writes=[tPA])
        for col in range(2):
            E("dve", "tensor_tensor", MV[:, :, col], PA[:].rearrange("p (j c) -> p j c", c=2)[:, :, col], AB[:, l, :], ALU.add,
              reads=[tPA, tC], writes=[tMV])
        E("sp", "dma_start", out=d["modv%d" % l], in_=MV[:], reads=[tMV], chan="a1")
    S.flush()
    es.close()

D_MODEL = 1024
SEQ_FULL = 4096
NLC = 2048
NCTX = 256
PERM = np.array([d + 16 if (d % 32) < 16 else d - 16 for d in range(64)])
PERM32 = np.array([d + 8 if (d % 16) < 8 else d - 8 for d in range(32)])


def fm(a):
    Dd = a.shape[1]
    return np.ascontiguousarray(a.T.reshape(Dd // 128, 128, -1).transpose(1, 0, 2))


def unfm(a):
    return a.transpose(1, 0, 2).reshape(a.shape[0] * a.shape[1], -1).T


def pv(v, p=128):
    return np.ascontiguousarray(np.asarray(v).reshape(-1, p).T)


def rope_tabs(pos, rot):
    axis = rot // 2
    inv = (10000.0 ** (-np.arange(0, axis, 2, dtype=np.float32) / axis)).astype(np.float32)
    row = (pos // 64).astype(np.float32); col = (pos % 64).astype(np.float32)
    ar = row[None, :] * inv[:, None]; ac = col[None, :] * inv[:, None]
    C = np.concatenate([np.cos(ar), np.cos(ar), np.cos(ac), np.cos(ac)], 0)
    Sg = np.concatenate([-np.sin(ar), np.sin(ar), -np.sin(ac), np.sin(ac)], 0)
    return C.astype(np.float32), Sg.astype(np.float32)


def prep_l0_weights(p, j=0):
    w_in = np.asarray(p['even_w_in'][j]); b_in = np.asarray(p['even_b_in'][j])
    qcols = 1024 + np.arange(512); kcols = 1536 + np.arange(128); vcols = 1664 + np.arange(128)
    def permcols(cols):
        c = cols.reshape(-1, 64)
        return c[:, PERM].reshape(-1)
    order = np.concatenate([np.arange(1024), kcols, permcols(kcols), vcols, qcols, permcols(qcols)])
    win = np.ascontiguousarray(w_in[:, order].reshape(8, 128, -1).transpose(1, 0, 2))
    b64 = np.zeros((64, 20), np.float32)
    for hh in range(8):
        b64[:, hh] = b_in[1024 + hh * 64 + np.arange(64)]
        b64[:, 8 + hh] = b_in[1024 + hh * 64 + PERM]
    for g in range(2):
        b64[:, 16 + g] = b_in[1536 + g * 64 + np.arange(64)]
        b64[:, 18 + g] = b_in[1536 + g * 64 + PERM]
    b128 = pv(b_in[0:1024])
    w_out = np.asarray(p['even_w_out'][j])
    mb = np.zeros((128, 2, 4, 128), np.float32)
    kk = np.arange(128)[:, None]; qq = np.arange(128)[None, :]
    mb[:, 0] = np.where(kk >= qq, 0.0, -30000.0)[:, None, :]
    mb[:, 1] = np.where(kk <= qq, 0.0, -30000.0)[:, None, :]
    return dict(win=win, b64=b64, b128=b128, bv=np.ascontiguousarray(b_in[1664:1792]),
                cw=np.ascontiguousarray(np.asarray(p['even_conv_w'][j])[:, 0, :].reshape(31, 4, 128).transpose(2, 1, 0)),
                cvec0=np.ascontiguousarray(np.stack([pv(p['even_conv_b'][j]), pv(p['even_conv_ln_g'][j]), pv(p['even_conv_ln_b'][j])], 1)),
                sink=np.ascontiguousarray(np.asarray(p['even_sink'][j])),
                woc=np.ascontiguousarray(w_out[0:512].reshape(4, 128, 1024).transpose(1, 0, 2)),
                woa=np.ascontiguousarray(w_out[512:1024].reshape(8, 64, 1024).transpose(1, 0, 2)),
                bout0=pv(p['even_b_out'][j]),
                ln1_0=np.ascontiguousarray(np.stack([pv(p['ln1_g'][0]), pv(p['ln1_b'][0])], 1)),
                mb=np.ascontiguousarray(mb.reshape(128, 2, 512)), ident=np.eye(128, dtype=np.float32))


def prep_l0_core(h, NL, S, x_b, ctx_b):
    EXT = NL + 256; NC = EXT + 256
    pos = h * NL - 128 + np.arange(EXT)
    valid = (pos >= 0) & (pos < S)
    xe = np.zeros((NC, D_MODEL), np.float32)
    xe[:EXT][valid] = x_b[pos[valid]]
    xe[EXT:] = ctx_b
    C, Sg = rope_tabs(np.clip(pos, 0, S - 1), 64)
    tabc = np.concatenate([C, np.ones((64, 256), np.float32)], 1)
    tabs = np.concatenate([Sg, np.zeros((64, 256), np.float32)], 1)
    kbias = np.concatenate([np.where(valid, 0.0, -30000.0), np.zeros(256)]).astype(np.float32)
    vm = np.concatenate([valid[:128], valid[EXT - 128:]]).astype(np.float32)
    return dict(xe=fm(xe), tabc=np.ascontiguousarray(tabc), tabs=np.ascontiguousarray(tabs), kbias=kbias,
                vmask=np.ascontiguousarray(np.broadcast_to(vm[None, :], (128, 256))))


def prep_l1_weights(p, j=0, li=1):
    w_in = np.asarray(p['odd_w_in'][j]); b_in = np.asarray(p['odd_b_in'][j])
    def permc(cols, perm=PERM):
        return cols.reshape(-1, len(perm))[:, perm].reshape(-1)
    kc = 768 + np.arange(128); vc = 896 + np.arange(128); kvc = 1024 + np.arange(128); krc = 1152 + np.arange(32)
    qpair = np.concatenate([np.concatenate([(0 * 4 + i) * 64 + np.arange(64), (1 * 4 + i) * 64 + np.arange(64)]) for i in range(4)])
    qc = 512 + np.arange(256)
    wa_cols = np.concatenate([kc, permc(kc), vc, kvc, krc, permc(krc, PERM32)])
    wq_cols = np.concatenate([qpair, permc(qpair), qc])
    lay = lambda cols: np.ascontiguousarray(w_in[:, cols].reshape(8, 128, -1).transpose(1, 0, 2))
    cvec = np.zeros((128, 24), np.float32)
    cvec[:, 0] = b_in[kc]; cvec[:, 1] = b_in[permc(kc)]; cvec[:, 2] = b_in[kvc]
    gk = np.asarray(p['odd_k_norm'][j]); gq = np.asarray(p['odd_q_norm'][j])
    cvec[:, 3] = np.tile(gk, 2); cvec[:, 4] = np.tile(gk[PERM], 2); cvec[:, 5] = np.asarray(p['odd_mla_kv_norm'][j])
    pq = permc(qpair)
    for i in range(4):
        cvec[:, 6 + i] = b_in[qpair[i * 128:(i + 1) * 128]]; cvec[:, 10 + i] = b_in[pq[i * 128:(i + 1) * 128]]
    cvec[:, 14] = b_in[512:640]; cvec[:, 15] = b_in[640:768]
    cvec[:, 16] = np.tile(gq, 2); cvec[:, 17] = np.tile(gq[PERM], 2)
    gqc = np.asarray(p['odd_mla_q_norm'][j]); cvec[:, 18] = gqc[:128]; cvec[:, 19] = gqc[128:]
    cvec[:32, 20] = b_in[krc]; cvec[:32, 21] = b_in[permc(krc, PERM32)]
    w_uq = np.asarray(p['odd_mla_w_uq'][j]); w_ukv = np.asarray(p['odd_mla_w_ukv'][j]); w_out = np.asarray(p['odd_w_out'][j])
    wuq = np.zeros((128, 2, 8, 128), np.float32)
    for jj in range(2):
        for h in range(8):
            blk = w_uq[jj * 128:(jj + 1) * 128, h * 96:(h + 1) * 96]
            wuq[:, jj, h, 0:64] = blk[:, 0:64]; wuq[:, jj, h, 64:96] = blk[:, 64:96]; wuq[:, jj, h, 96:128] = blk[:, 64 + PERM32]
    wukt = np.zeros((64, 8, 128), np.float32); wuv = np.zeros((128, 512), np.float32)
    for h in range(8):
        wukt[:, h, :] = w_ukv[:, h * 128:h * 128 + 64].T
        wuv[:, h * 64:(h + 1) * 64] = w_ukv[:, h * 128 + 64:(h + 1) * 128]
    bd64 = np.zeros((128, 128), np.float32); bd64[:64, :64] = 1 / 64; bd64[64:, 64:] = 1 / 64
    return dict(wa=lay(wa_cols), wq=lay(wq_cols), cvec1=cvec, bv1=np.ascontiguousarray(b_in[vc]), wuq=wuq, wukt=wukt, wuv=wuv,
                wo=np.ascontiguousarray(w_out.reshape(16, 64, 1024).transpose(1, 0, 2)), bout1=pv(p['odd_b_out'][j]),
                ln1_1=np.ascontiguousarray(np.stack([pv(p['ln1_g'][li]), pv(p['ln1_b'][li])], 1)), bd64=bd64)


def prep_l1_tables(SEQ):
    pos = np.arange(SEQ)
    C, Sg = rope_tabs(pos, 64); Cm, Sm = rope_tabs(pos, 32)
    one = lambda r: np.ones((r, 256), np.float32); zero = lambda r: np.zeros((r, 256), np.float32)
    t2 = lambda a: np.concatenate([a, a], 0)
    shared = dict(tkc=np.ascontiguousarray(np.concatenate([t2(C), one(128)], 1)), tks=np.ascontiguousarray(np.concatenate([t2(Sg), zero(128)], 1)),
                  tmc=np.ascontiguousarray(np.concatenate([Cm, one(32)], 1)), tms=np.ascontiguousarray(np.concatenate([Sm, zero(32)], 1)))
    per_h = []
    for h in range(2):
        sl = slice(h * NLC, (h + 1) * NLC)
        per_h.append(dict(tqc=np.ascontiguousarray(t2(C)[:, sl]), tqs=np.ascontiguousarray(t2(Sg)[:, sl]),
                          tmqc=np.ascontiguousarray(Cm[:, sl]), tmqs=np.ascontiguousarray(Sm[:, sl])))
    return shared, per_h


def prep_moe(p, l):
    w_rg = np.asarray(p['moe_w_rg'][l]); w_re = np.asarray(p['moe_w_re'][l])
    wr = np.concatenate([w_rg, w_re.transpose(1, 0, 2).reshape(D_MODEL, 32)], axis=1)
    wr_l = np.ascontiguousarray(wr.reshape(8, 128, 36).transpose(1, 0, 2))
    brow = np.concatenate([np.asarray(p['moe_b_rg'][l]), np.asarray(p['moe_b_re'][l]).reshape(-1)]).astype(np.float32)
    ln2 = np.ascontiguousarray(np.stack([pv(p['ln2_g'][l]), pv(p['ln2_b'][l])], 1))
    return {"wr%d" % l: wr_l, "brow%d" % l: brow, "ln2_%d" % l: ln2,
            "w1_%d" % l: np.asarray(p['moe_w1'][l]), "w3_%d" % l: np.asarray(p['moe_w3'][l]), "w2_%d" % l: np.asarray(p['moe_w2'][l])}


def make_sel():
    sel = np.zeros((64, NE, 128), np.float32)
    for e in range(NE):
        sel[e, e, :] = 1; sel[32 + e, e, :] = 1
    return sel


_PROG = {}


def _decl(nc, d, name, shape, kind="ExternalInput"):
    if kind is None:
        d[name] = nc.dram_tensor(name, list(shape), F32).ap()
    else:
        d[name] = nc.dram_tensor(name, list(shape), F32, kind=kind).ap()


def build_fused():
    nc = bass.Bass("TRN2", target_bir_lowering=False)
    NL = NLC; EXT = NL + 256; NC = EXT + 256; T = NL + 256; SEQ = SEQ_FULL; NK = SEQ + 256
    d = {}
    I = lambda n, s: _decl(nc, d, n, s)
    I("cin", [128, 8, 2]); I("adaw", [2, 1024, 6144]); I("adab", [128, 2, 48])
    for sg in ("A", "B"):
        I("xe" + sg, [128, 8, NC]); I("tabc" + sg, [64, NC]); I("tabs" + sg, [64, NC]); I("kbias" + sg, [NC]); I("vmask" + sg, [128, 256])
    I("win", [128, 8, 2432]); I("b128", [128, 8]); I("b64", [64, 20]); I("bv", [128])
    I("cw", [128, 4, 31]); I("cvec0", [128, 3, 4]); I("sink", [8]); I("woc", [128, 4, 1024]); I("woa", [64, 8, 1024]); I("bout0", [128, 8])
    I("ln1_0", [128, 2, 8]); I("mb", [128, 2, 512]); I("ident", [128, 128])
    I("wr0", [128, 8, 36]); I("brow0", [36]); I("ln2_0", [128, 2, 8]); I("sel", [64, NE, 128])
    I("w1_0", [NE, 1024, 512]); I("w3_0", [NE, 1024, 512]); I("w2_0", [NE, 512, 1024])
    I("wa", [128, 8, 576]); I("wq", [128, 8, 1280]); I("cvec1", [128, 24])
    I("bv1", [128]); I("wuq", [128, 2, 8, 128]); I("wukt", [64, 8, 128]); I("wuv", [128, 512]); I("wo", [64, 16, 1024]); I("bout1", [128, 8])
    I("ln1_1", [128, 2, 8]); I("bd64", [128, 128])
    I("tkc", [128, NK]); I("tks", [128, NK]); I("tmc", [32, NK]); I("tms", [32, NK])
    I("tqc", [128, NL]); I("tqs", [128, NL]); I("tmqc", [32, NL]); I("tmqs", [32, NL])
    I("wr1", [128, 8, 36]); I("brow1", [36]); I("ln2_1", [128, 2, 8])
    I("w1_1", [NE, 1024, 512]); I("w3_1", [NE, 1024, 512]); I("w2_1", [NE, 512, 1024])
    for nm, shp in (("modv0", [128, 48, 2]), ("modv1", [128, 48, 2]), ("xa0", [128, 8, T]), ("u2f0", [128, 8, T]),
                    ("xl0", [128, 8, SEQ]), ("xc0", [128, 8, 256]), ("xa1", [128, 8, NL]), ("u2f1", [128, 8, NL])):
        _decl(nc, d, nm, shp, None)
    _decl(nc, d, "xs1", [128, 8, NL], "ExternalOutput")
    S = Sched(nc)
    emit_ada(nc, S, dict(cin=d["cin"], adaw=d["adaw"], adab=d["adab"], modv0=d["modv0"], modv1=d["modv1"]))
    for si, sg in enumerate(("A", "B")):
        emit_l0_mixer(nc, S, NL, dict(xe=d["xe" + sg], modv=d["modv0"], win=d["win"], b128=d["b128"], b64=d["b64"], bv=d["bv"], cw=d["cw"], cvec=d["cvec0"],
                                      sink=d["sink"], woc=d["woc"], woa=d["woa"], bout=d["bout0"], ln1=d["ln1_0"], tabc=d["tabc" + sg], tabs=d["tabs" + sg],
                                      kbias=d["kbias" + sg], vmask=d["vmask" + sg], mb=d["mb"], ident=d["ident"], xa=d["xa0"], u2f=d["u2f0"]), tag=sg)
        Tm = T if si == 0 else NL
        emit_moe(nc, S, Tm, dict(u2f=d["u2f0"][:, :, 0:Tm], xa=d["xa0"][:, :, 0:Tm], modv=d["modv0"], wr=d["wr0"], brow=d["brow0"], w1=d["w1_0"], w3=d["w3_0"], w2=d["w2_0"],
                                 ln2=d["ln2_0"], ident=d["ident"], sel=d["sel"], xs=d["xl0"][:, :, si * NL:(si + 1) * NL], xsc=d["xc0"]), nctx=(256 if si == 0 else 0), tag="0" + sg)

    def xk_src(c0, n):
        if c0 < SEQ:
            return d["xl0"][:, :, c0:c0 + n]
        return d["xc0"][:, :, c0 - SEQ:c0 - SEQ + n]

    emit_l1_mixer(nc, S, NL, SEQ, dict(xk=xk_src, xo=d["xl0"][:, :, 0:NL], modv=d["modv1"], wa=d["wa"], wq=d["wq"], cvec=d["cvec1"], bv=d["bv1"], wuq=d["wuq"],
                                       wukt=d["wukt"], wuv=d["wuv"], wo=d["wo"], bout=d["bout1"], ln1=d["ln1_1"], bd64=d["bd64"],
                                       tkc=d["tkc"], tks=d["tks"], tmc=d["tmc"], tms=d["tms"], tqc=d["tqc"], tqs=d["tqs"], tmqc=d["tmqc"], tmqs=d["tmqs"],
                                       xa=d["xa1"], u2f=d["u2f1"]))
    emit_moe(nc, S, NL, dict(u2f=d["u2f1"], xa=d["xa1"], modv=d["modv1"], wr=d["wr1"], brow=d["brow1"], w1=d["w1_1"], w3=d["w3_1"], w2=d["w2_1"],
                             ln2=d["ln2_1"], ident=d["ident"], sel=d["sel"], xs=d["xs1"]), nctx=0, tag="1")
    S.close()
    return nc


def prep_l1_tables_core(h, SEQ):
    pos = np.concatenate([h * NLC + np.arange(NLC), (1 - h) * NLC + np.arange(NLC)])
    C, Sg = rope_tabs(pos, 64); Cm, Sm = rope_tabs(pos, 32)
    one = lambda r: np.ones((r, 256), np.float32); zero = lambda r: np.zeros((r, 256), np.float32)
    t2 = lambda a: np.concatenate([a, a], 0)
    return dict(tkc=np.ascontiguousarray(np.concatenate([t2(C), one(128)], 1)), tks=np.ascontiguousarray(np.concatenate([t2(Sg), zero(128)], 1)),
                tmc=np.ascontiguousarray(np.concatenate([Cm, one(32)], 1)), tms=np.ascontiguousarray(np.concatenate([Sm, zero(32)], 1)),
                tqc=np.ascontiguousarray(t2(C)[:, :NLC]), tqs=np.ascontiguousarray(t2(Sg)[:, :NLC]),
                tmqc=np.ascontiguousarray(Cm[:, :NLC]), tmqs=np.ascontiguousarray(Sm[:, :NLC]))


def kernel(**inputs):
    p = {k: np.asarray(v) for k, v in inputs.items()}
    x = p["x"]; ctx = p["ctx"]; c = p["c"]; c_ctx = p["c_ctx"]
    B = x.shape[0]
    ncores = 8
    W0 = prep_l0_weights(p); M0 = prep_moe(p, 0)
    W1 = prep_l1_weights(p); M1 = prep_moe(p, 1)
    tabs1 = [prep_l1_tables_core(h, SEQ_FULL) for h in range(2)]
    sel = make_sel()
    adab = np.ascontiguousarray(p["ada_b"].reshape(2, 48, 128).transpose(2, 0, 1))
    in_maps = []
    for core in range(ncores):
        b, h = core // 2, core % 2
        m = dict(W0); m.update(M0); m.update(W1); m.update(M1); m.update(tabs1[h])
        for sg, hh in (("A", h), ("B", 1 - h)):
            for k, v in prep_l0_core(hh, NLC, SEQ_FULL, x[b], ctx[b]).items():
                m[k + sg] = v
        m["cin"] = np.ascontiguousarray(np.stack([pv(c[b]), pv(c_ctx)], -1))
        m["adaw"] = p["ada_w"]; m["adab"] = adab; m["sel"] = sel
        in_maps.append(m)
    if "f" not in _PROG:
        _PROG["f"] = build_fused()
    r = run_bass_kernel_spmd(_PROG["f"], in_maps, core_ids=list(range(ncores))).results
    out = np.zeros((B, SEQ_FULL, D_MODEL), np.float32)
    for core in range(ncores):
        b, h = core // 2, core % 2
        out[b, h * NLC:(h + 1) * NLC, :] = unfm(r[core]["xs1"])
    return out
```

```python
import numpy as np
from contextlib import ExitStack
import concourse.bass as bass
import concourse.mybir as mybir
from concourse.bass_utils import run_bass_kernel_spmd


ENGS = ("pe", "act", "dve", "pool", "sp")


class Tile:
    __slots__ = ("name", "w", "r")

    def __init__(self, name):
        self.name = name
        self.w = {}
        self.r = {}


class Op:
    __slots__ = ("eng", "fn", "idx", "deps", "signal", "ordinal", "dma", "chan", "grp", "gidx")

    def __init__(self, eng, fn):
        self.eng = eng
        self.fn = fn
        self.deps = []
        self.signal = False
        self.ordinal = None
        self.dma = False
        self.chan = None
        self.grp = None


class DmaGroup:
    __slots__ = ("last", "closed")

    def __init__(self):
        self.last = 0
        self.closed = False


class Sched:
    def __init__(self, nc, n_chan_sems=60):
        self.nc = nc
        self.ops = {e: [] for e in ENGS}
        self.nops = {e: 0 for e in ENGS}
        self.nsig = {e: 0 for e in ENGS}
        self.seen = {e: {} for e in ENGS}
        self.esem = {}
        self.chan_sem = {}
        self.chan_cnt = {}
        self.chan_lastgrp = {}
        self.chan_lastop = {}
        self._sem_cms = []
        for e in ("pe", "act", "dve", "pool"):
            cm = nc.semaphore("e_" + e)
            self.esem[e] = cm.__enter__()
            self._sem_cms.append(cm)
        self.free_chan = []
        for i in range(n_chan_sems):
            cm = nc.semaphore("c_%d" % i)
            self.free_chan.append(cm.__enter__())
            self._sem_cms.append(cm)
        self.pending_dma = {e: [] for e in ENGS}

    def close(self):
        for cm in reversed(self._sem_cms):
            cm.__exit__(None, None, None)

    def _stream(self, o):
        return ("dma", o.chan) if o.dma else o.eng

    def _add_dep(self, o, p, raw):
        if p is o:
            return
        if p.dma and o.dma and p.grp is o.grp:
            return
        ps = self._stream(p)
        if (not p.dma) and (not o.dma) and p.eng == o.eng == "pe" and not raw:
            return
        seen = self.seen[o.eng]
        if seen.get(ps, -1) >= p.idx:
            return
        seen[ps] = p.idx
        if p.dma:
            p.grp.closed = True
        else:
            p.signal = True
        o.deps.append(p)

    def op(self, eng, fn, reads=(), writes=(), chan=None, grp=None):
        o = Op(eng, fn)
        if chan is not None:
            o.dma = True
            o.chan = chan
            if chan not in self.chan_sem:
                self.chan_sem[chan] = self.free_chan.pop()
                self.chan_cnt[chan] = 0
            self.chan_cnt[chan] += 1
            o.idx = self.chan_cnt[chan]
            lg = self.chan_lastgrp.get(chan)
            if lg is not None and (not lg[1].closed) and lg[0] == grp and grp is not None:
                o.grp = lg[1]
            else:
                prev = self.chan_lastop.get(chan)
                o.grp = DmaGroup()
                self.chan_lastgrp[chan] = (grp, o.grp)
                if prev is not None:
                    self._add_dep_dma_prev(o, prev)
            o.grp.last = o.idx
            self.chan_lastop[chan] = o
            self.pending_dma[eng].append(o)
        else:
            o.idx = self.nops[eng]
        self.nops[eng] += 1
        for t in reads:
            for p in t.w.values():
                self._add_dep(o, p, True)
        for t in writes:
            for p in t.w.values():
                self._add_dep(o, p, False)
            for p in t.r.values():
                self._add_dep(o, p, False)
        st = self._stream(o)
        for t in reads:
            t.r[st] = o
        for t in writes:
            t.w[st] = o
        self.ops[eng].append(o)
        return o

    def _add_dep_dma_prev(self, o, prev):
        seen = self.seen[o.eng]
        ps = ("dma", prev.chan)
        if seen.get(ps, -1) >= prev.idx:
            return
        seen[ps] = prev.idx
        prev.grp.closed = True
        o.deps.append(prev)

    def flush(self, name=None, drain_dma=True):
        nc = self.nc
        tail = {e: [] for e in ENGS}
        if drain_dma:
            for e in ENGS:
                chans = {}
                for o in self.pending_dma[e]:
                    chans[o.chan] = o
                for ch, o in chans.items():
                    o.grp.closed = True
                    tail[e].append((self.chan_sem[ch], 16 * o.grp.last))
                    self.seen[e][("dma", ch)] = max(self.seen[e].get(("dma", ch), -1), o.idx)
                self.pending_dma[e] = []
        for e in ENGS:
            for o in self.ops[e]:
                if (not o.dma) and o.signal:
                    self.nsig[e] += 1
                    o.ordinal = self.nsig[e]
        sched = self

        def emit(e):
            def body(engine):
                for o in sched.ops[e]:
                    for p in o.deps:
                        if p.dma:
                            engine.wait_ge(sched.chan_sem[p.chan], 16 * p.grp.last)
                        else:
                            engine.wait_ge(sched.esem[p.eng], p.ordinal)
                    ins = o.fn(engine)
                    if o.dma:
                        ins.then_inc(sched.chan_sem[o.chan], 16)
                    elif o.signal:
                        ins.then_inc(sched.esem[e], 1)
                for (sem, val) in tail[e]:
                    engine.wait_ge(sem, val)
            return body

        with nc.Block() as block:
            if self.ops["sp"] or tail["sp"]:
                block.sync(emit("sp"))
            if self.ops["pe"] or tail["pe"]:
                block.tensor(emit("pe"))
            if self.ops["act"] or tail["act"]:
                block.scalar(emit("act"))
            if self.ops["dve"] or tail["dve"]:
                block.vector(emit("dve"))
            if self.ops["pool"] or tail["pool"]:
                block.gpsimd(emit("pool"))
        for e in ENGS:
            for s in ("pe", "act", "dve", "pool"):
                self.seen[e][s] = self.nops[s]
            self.ops[e] = []


F32 = mybir.dt.float32; BF16 = mybir.dt.bfloat16
AF = mybir.ActivationFunctionType
ALU = mybir.AluOpType
AX = mybir.AxisListType
LN_EPS = 1e-5
DN_ALPHA = float(4 ** 0.25)
NEG = -30000.0


def mkE(S):
    def E(eng, meth, *args, reads=(), writes=(), chan=None, grp=None, **kw):
        return S.op(eng, lambda e: getattr(e, meth)(*args, **kw), reads=reads, writes=writes, chan=chan, grp=grp)
    return E


def attn_pipeline(E, groups, SC, tSC, PT, tPT, n):
    its = [(gi, kt) for gi, g in enumerate(groups) for kt in range(g["nkt"])]

    def qk(j):
        gi, kt = its[j]
        g = groups[gi]
        if kt == 0 and g.get("pre") is not None:
            g["pre"]()
        g["qk"](kt, SC[j % 2], tSC[j % 2])

    pend = []
    qk(0)
    for j, (gi, kt) in enumerate(its):
        g = groups[gi]
        if j + 1 < len(its):
            qk(j + 1)
        E("act", "activation", out=PT[j % 3][:, 0:n], in_=SC[j % 2][:, 0:n], func=AF.Exp, scale=g["scale"], reads=[tSC[j % 2]], writes=[tPT[j % 3]])
        g["pv"](kt, PT[j % 3], tPT[j % 3], kt == 0, kt == g["nkt"] - 1)
        while pend and pend[0][0] <= j:
            pend.pop(0)[1]()
        if kt == g["nkt"] - 1:
            pend += [(j + 1, g["fin1"]), (j + 2, g["fin2"]), (j + 3, g["fin3"])]
            pend.sort(key=lambda t: t[0])
    for item in pend:
        item[1]()


def emit_l0_mixer(nc, S, NL, d, li=0, tag=""):
    E = mkE(S)
    EXT = NL + 256
    NC = EXT + 256
    T = NL + 256
    es = ExitStack()
    sb = lambda name, shape, dt: es.enter_context(nc.sbuf_tensor("m0" + tag + "_" + name, shape, dt))
    ps = lambda name, shape, dt: es.enter_context(nc.psum_tensor("m0p" + tag + "_" + name, shape, dt))
    XB = sb("XB", [128, 8, 512], F32)
    UB = sb("UB", [128, 8, 512], BF16)
    HL = sb("HL", [128, 4, EXT], BF16)
    HC = sb("HC", [128, 4, 286], BF16)
    KT = [sb("KT%d" % g, [65, NC], BF16) for g in range(2)]
    VA = sb("VA", [128, NC // 128, 2, 65], BF16)
    WA = sb("WA", [128, 8, 1408], BF16)
    WOC = sb("WOC", [128, 4, 1024], BF16)
    WOA = sb("WOA", [64, 8, 1024], BF16)
    DG = sb("DG", [128, 124, 128], BF16)
    QT = [sb("QT%d" % g, [65, 4, 4, 128], BF16) for g in range(2)]
    PT = [sb("PT%d" % i, [128, 512], BF16) for i in range(3)]
    AO = sb("AO", [64, 8, 512], BF16)
    CV = sb("CV", [128, 4, 512], F32)
    CO = sb("CO", [128, 4, 512], BF16)
    LT = [sb("LT%d" % i, [128, 512], F32) for i in range(4)]
    TB = [sb("TB%d" % i, [64, 512], F32) for i in range(2)]
    R1 = sb("R1", [128, 512], F32)
    R2 = sb("R2", [128, 512], F32)
    OTS = sb("OTS", [65, 512], F32)
    DROW = sb("DROW", [65, 512], F32)
    OUTA = [sb("OUTA%d" % i, [128, 512], F32) for i in range(1)]
    OUTB = [sb("OUTB%d" % i, [128, 512], F32) for i in range(1)]
    modv = sb("modv", [128, 48, 2], F32)
    sc1p1 = sb("sc1p1", [128, 8, 2], F32)
    g1p1 = sb("g1p1", [128, 8, 2], F32)
    sc2p1 = sb("sc2p1", [128, 8, 2], F32)
    b128 = sb("b128", [128, 8], F32)
    b64 = sb("b64", [64, 20], F32)
    bvb = sb("bvb", [128, 128], F32)
    cw = sb("cw", [128, 4, 31], F32)
    cvec = sb("cvec", [128, 3, 4], F32)
    bout = sb("bout", [128, 8], F32)
    ln1 = sb("ln1", [128, 2, 8], F32)
    vmask = sb("vmask", [128, 256], BF16)
    MB = sb("MB", [128, 2, 512], BF16)
    identb = sb("identb", [128, 128], BF16)
    identf = sb("identf", [128, 128], F32)
    ones1k = sb("ones1k", [128, 128], F32)
    ones512 = sb("ones512", [128, 128], F32)
    mhalf = sb("mhalf", [128, 8], F32)
    epsc = sb("epsc", [128, 2], F32)
    ones65 = sb("ones65", [65, 128], F32)
    sink8 = sb("sink8", [65, 8], F32)
    P = [ps("P%d" % i, [128, 512], F32) for i in range(8)]
    tP = [Tile("P%d" % i) for i in range(8)]
    tC = Tile("const")
    tXB = Tile("XB"); tUB = Tile("UB")
    NCB = (NC + 511) // 512
    tH = [Tile("H%d" % i) for i in range(NCB + 1)]
    tKT = [Tile("KT%d" % i) for i in range(NCB)]
    tVA = [Tile("VA%d" % i) for i in range(NCB)]
    tWA = Tile("WA"); tWO = Tile("WO"); tDG = Tile("DG")
    tQT = [Tile("QT%d" % g) for g in range(2)]
    tPT = [Tile("PT%d" % i) for i in range(3)]
    tAO = Tile("AO"); tCV = Tile("CV"); tCO = Tile("CO")
    tLT = [Tile("LT%d" % i) for i in range(4)]
    tTB = Tile("TB"); tR1 = Tile("R1"); tR2 = Tile("R2"); tOTS = Tile("OTS"); tDROW = Tile("DROW")
    tOUTA = [Tile("OUTA%d" % i) for i in range(1)]
    tOUTB = [Tile("OUTB%d" % i) for i in range(1)]

    cst = dict(writes=[tC], chan="k0", grp=0)
    E("sp", "dma_start", out=modv[:], in_=d["modv"], **cst)
    E("sp", "dma_start", out=b128[:], in_=d["b128"], **cst)
    E("sp", "dma_start", out=b64[:], in_=d["b64"], **cst)
    E("sp", "dma_start", out=bvb[:], in_=bass.AP(d["bv"].tensor, 0, [[0, 128], [1, 128]]), **cst)
    E("sp", "dma_start", out=cw[:], in_=d["cw"], **cst)
    E("sp", "dma_start", out=cvec[:], in_=d["cvec"], **cst)
    E("sp", "dma_start", out=bout[:], in_=d["bout"], **cst)
    E("sp", "dma_start", out=ln1[:], in_=d["ln1"], **cst)
    E("sp", "dma_start", out=identf[:], in_=d["ident"], **cst)
    E("sp", "dma_start", out=sink8[64:65, :], in_=bass.AP(d["sink"].tensor, 0, [[0, 1], [1, 8]]), **cst)
    cst2 = dict(writes=[tC], chan="k1", grp=0)
    E("pool", "dma_start", out=vmask[:], in_=d["vmask"], **cst2)
    E("pool", "dma_start", out=MB[:], in_=d["mb"], **cst2)
    E("pool", "dma_start", out=identb[:], in_=d["ident"], **cst2)
    E("pool", "dma_start", out=WA[:], in_=d["win"][:, :, 0:1408], writes=[tWA], chan="k2")
    E("pool", "dma_start", out=WOC[:], in_=d["woc"], writes=[tWO], chan="k3", grp=0)
    E("pool", "dma_start", out=WOA[:], in_=d["woa"], writes=[tWO], chan="k3", grp=0)
    for g in range(2):
        E("pool", "dma_start", out=KT[g][64:65, :], in_=bass.AP(d["kbias"].tensor, 0, [[0, 1], [1, NC]]), writes=[tKT[i] for i in range(NCB)], chan="k4", grp=0)
    E("dve", "memset", ones1k[:], 1.0 / 1024.0, writes=[tC])
    E("dve", "memset", ones512[:], 1.0 / 512.0, writes=[tC])
    E("dve", "memset", epsc[:, 0:1], LN_EPS, writes=[tC])
    E("dve", "memset", epsc[:, 1:2], 1e-6, writes=[tC])
    E("dve", "memset", ones65[:], 1.0, writes=[tC])
    E("dve", "memset", HC[:], 0.0, writes=[tH[NCB]])
    E("pool", "memset", VA[:, :, :, 64:65], 1.0, writes=tVA)
    for g in range(2):
        E("pool", "memset", QT[g][64:65, :, :, :], 1.0, writes=[tQT[g]])
    E("dve", "tensor_scalar", sc1p1[:], modv[:, 8:16, :], 1.0, None, ALU.add, reads=[tC], writes=[tC])
    E("dve", "tensor_scalar", g1p1[:], modv[:, 16:24, :], 1.0, None, ALU.add, reads=[tC], writes=[tC])
    E("dve", "tensor_scalar", sc2p1[:], modv[:, 32:40, :], 1.0, None, ALU.add, reads=[tC], writes=[tC])
    E("act", "activation", out=sink8[64:65, :], in_=sink8[64:65, :], func=AF.Exp, reads=[tC], writes=[tC])
    for c in range(4):
        for k in range(31):
            if (c * 31 + k) % 2 == 0:
                E("dve", "tensor_scalar", DG[:, c * 31 + k, :], identf[:], cw[:, c, k:k + 1], None, ALU.mult, reads=[tC], writes=[tDG])
            else:
                E("act", "activation", out=DG[:, c * 31 + k, :], in_=identf[:], func=AF.Identity, scale=cw[:, c, k:k + 1], reads=[tC], writes=[tDG])

    def make_u(c0, n):
        segs = []
        if c0 < EXT:
            segs.append((0, min(n, EXT - c0), 0))
        if c0 + n > EXT:
            s0 = max(0, EXT - c0)
            segs.append((s0, n, 1))
        for k in range(8):
            for (a, b, m) in segs:
                eng = ("dve", "act", "pool", "dve", "act", "dve", "act", "pool")[k]
                if eng == "act":
                    E("act", "activation", out=UB[:, k, a:b], in_=XB[:, k, a:b], func=AF.Identity, scale=sc1p1[:, k, m:m + 1], bias=modv[:, k, m:m + 1],
                      reads=[tXB, tC], writes=[tUB])
                else:
                    E(eng, "tensor_scalar", UB[:, k, a:b], XB[:, k, a:b], sc1p1[:, k, m:m + 1], modv[:, k, m:m + 1], ALU.mult, ALU.add,
                      reads=[tXB, tC], writes=[tUB])
        return segs

    cblocks = [(c0, min(512, NC - c0)) for c0 in range(0, NC, 512)]
    for bi, (c0, n) in enumerate(cblocks):
        E("sp", "dma_start", out=XB[:, :, 0:n], in_=d["xe"][:, :, c0:c0 + n], writes=[tXB], chan="xb")
        E("sp", "dma_start", out=TB[0][:, 0:n], in_=d["tabc"][:, c0:c0 + n], writes=[tTB], chan="tb", grp=bi)
        E("sp", "dma_start", out=TB[1][:, 0:n], in_=d["tabs"][:, c0:c0 + n], writes=[tTB], chan="tb", grp=bi)
        segs = make_u(c0, n)
        for c in range(4):
            pg, pv = P[0], P[1]
            for k in range(8):
                E("pe", "matmul", pg[:, 0:n], WA[:, k, 512 + c * 128:512 + (c + 1) * 128], UB[:, k, 0:n], start=(k == 0), stop=(k == 7),
                  reads=[tWA, tUB], writes=[tP[0]])
            for k in range(8):
                E("pe", "matmul", pv[:, 0:n], WA[:, k, c * 128:(c + 1) * 128], UB[:, k, 0:n], start=(k == 0), stop=(k == 7),
                  reads=[tWA, tUB], writes=[tP[1]])
            E("act", "activation", out=R1[:, 0:n], in_=pg[:, 0:n], func=AF.Sigmoid, bias=b128[:, 4 + c:5 + c], reads=[tP[0], tC], writes=[tR1])
            for (a, b, m) in segs:
                if m == 0:
                    dst = HL[:, c, c0 + a:c0 + b]
                    th = tH[bi]
                else:
                    dst = HC[:, c, 15 + c0 + a - EXT:15 + c0 + b - EXT]
                    th = tH[NCB]
                E("dve", "scalar_tensor_tensor", dst, pv[:, a:b], b128[:, c:c + 1], R1[:, a:b], ALU.add, ALU.mult,
                  reads=[tP[1], tR1, tC], writes=[th])
        for g in range(2):
            pk, pkp = P[2], P[3]
            for k in range(8):
                E("pe", "matmul", pk[0:64, 0:n], WA[:, k, 1024 + g * 64:1024 + (g + 1) * 64], UB[:, k, 0:n], start=(k == 0), stop=(k == 7),
                  reads=[tWA, tUB], writes=[tP[2]])
            for k in range(8):
                E("pe", "matmul", pkp[0:64, 0:n], WA[:, k, 1152 + g * 64:1152 + (g + 1) * 64], UB[:, k, 0:n], start=(k == 0), stop=(k == 7),
                  reads=[tWA, tUB], writes=[tP[3]])
            E("dve", "scalar_tensor_tensor", R2[0:64, 0:n], pk[0:64, 0:n], b64[:, 16 + g:17 + g], TB[0][:, 0:n], ALU.add, ALU.mult,
              reads=[tP[2], tTB, tC], writes=[tR2])
            E("dve", "scalar_tensor_tensor", LT[3][0:64, 0:n], pkp[0:64, 0:n], b64[:, 18 + g:19 + g], TB[1][:, 0:n], ALU.add, ALU.mult,
              reads=[tP[3], tTB, tC], writes=[tLT[3]])
            E("pool", "tensor_tensor", KT[g][0:64, c0:c0 + n], R2[0:64, 0:n], LT[3][0:64, 0:n], ALU.add, reads=[tR2, tLT[3]], writes=[tKT[bi]])
        for tt in range(n // 128):
            for k in range(8):
                E("pe", "matmul", P[4][:, 0:128], UB[:, k, tt * 128:(tt + 1) * 128], WA[:, k, 1280:1408], start=(k == 0), stop=(k == 7),
                  reads=[tWA, tUB], writes=[tP[4]])
            E("dve", "tensor_tensor", VA[:, c0 // 128 + tt, :, 0:64], P[4][:, 0:128].rearrange("p (g e) -> p g e", g=2),
              bvb[:].rearrange("p (g e) -> p g e", g=2), ALU.add, reads=[tP[4], tC], writes=[tVA[bi]])
    for c in range(4):
        E("dve", "tensor_tensor", HL[:, c, 0:128], HL[:, c, 0:128], vmask[:, 0:128], ALU.mult, reads=[tH[0], tC], writes=[tH[0]])
        lastb = (EXT - 1) // 512
        E("dve", "tensor_tensor", HL[:, c, EXT - 128:EXT], HL[:, c, EXT - 128:EXT], vmask[:, 128:256], ALU.mult, reads=[tH[lastb], tC], writes=[tH[lastb]])
    E("pool", "dma_start", out=WA[:, :, 0:1024], in_=d["win"][:, :, 1408:2432], writes=[tWA], chan="k2")

    oblocks = [(t0, 512, 0) for t0 in range(0, NL, 512)] + [(NL, 256, 1)]
    allH = tH
    allKT = tKT
    allVA = tVA
    for bi, (t0, n, m) in enumerate(oblocks):
        c0 = (128 + t0) if m == 0 else EXT
        E("sp", "dma_start", out=XB[:, :, 0:n], in_=d["xe"][:, :, c0:c0 + n], writes=[tXB], chan="xb")
        E("sp", "dma_start", out=TB[0][:, 0:n], in_=d["tabc"][:, c0:c0 + n], writes=[tTB], chan="tb", grp=100 + bi)
        E("sp", "dma_start", out=TB[1][:, 0:n], in_=d["tabs"][:, c0:c0 + n], writes=[tTB], chan="tb", grp=100 + bi)
        make_u(c0, n)
        ntl = n // 128
        for hh in range(8):
            g, i = hh // 4, hh % 4
            for k in range(8):
                E("pe", "matmul", P[0][0:64, 0:n], WA[:, k, hh * 64:(hh + 1) * 64], UB[:, k, 0:n], start=(k == 0), stop=(k == 7),
                  reads=[tWA, tUB], writes=[tP[0]])
            for k in range(8):
                E("pe", "matmul", P[1][0:64, 0:n], WA[:, k, 512 + hh * 64:512 + (hh + 1) * 64], UB[:, k, 0:n], start=(k == 0), stop=(k == 7),
                  reads=[tWA, tUB], writes=[tP[1]])
            E("dve", "scalar_tensor_tensor", R1[0:64, 0:n], P[0][0:64, 0:n], b64[:, hh:hh + 1], TB[0][:, 0:n], ALU.add, ALU.mult,
              reads=[tP[0], tTB, tC], writes=[tR1])
            E("dve", "scalar_tensor_tensor", R2[0:64, 0:n], P[1][0:64, 0:n], b64[:, 8 + hh:9 + hh], TB[1][:, 0:n], ALU.add, ALU.mult,
              reads=[tP[1], tTB, tC], writes=[tR2])
            E("pool", "tensor_tensor", QT[g][0:64, 0:ntl, i, :], R1[0:64, 0:n].rearrange("p (t q) -> p t q", q=128),
              R2[0:64, 0:n].rearrange("p (t q) -> p t q", q=128), ALU.add, reads=[tR1, tR2], writes=[tQT[g]])
        groups = []
        for tl in range(ntl):
            for g in range(2):
                gi = len(groups)
                acc, tacc = P[4 + 2 * (gi % 2)], tP[4 + 2 * (gi % 2)]
                if m == 0:
                    qi = t0 // 128 + tl
                    chunks = [(EXT // 128, None), (EXT // 128 + 1, None), (qi, 0), (qi + 1, None), (qi + 2, 1)]
                else:
                    chunks = [(EXT // 128, None), (EXT // 128 + 1, None)]
                rhs_q = QT[g][:, tl, :, :].rearrange("p i q -> p (i q)")

                def qk(ci, ps_, tps, g=g, rhs_q=rhs_q, chunks=chunks):
                    kt, mk = chunks[ci]
                    E("pe", "matmul", ps_[:, :], KT[g][:, kt * 128:(kt + 1) * 128], rhs_q, start=True, stop=(mk is None),
                      reads=[tQT[g], allKT[(kt * 128) // 512]], writes=[tps])
                    if mk is not None:
                        E("pe", "matmul", ps_[:, :], identb[:], MB[:, mk, :], start=False, stop=True, reads=[tC], writes=[tps])

                def pv(ci, pt, tpt, st, sp, g=g, chunks=chunks, acc=acc, tacc=tacc):
                    kt = chunks[ci][0]
                    E("pe", "matmul", acc[0:65, :], VA[:, kt, g, :], pt[:], start=st, stop=sp, reads=[tpt, allVA[(kt * 128) // 512]], writes=[tacc])

                def fin1(g=g, acc=acc, tacc=tacc):
                    for i4 in range(4):
                        E("dve", "tensor_scalar", DROW[64:65, i4 * 128:(i4 + 1) * 128], acc[64:65, i4 * 128:(i4 + 1) * 128],
                          sink8[64:65, g * 4 + i4:g * 4 + i4 + 1], None, ALU.add, reads=[tacc, tC], writes=[tDROW])
                    E("dve", "reciprocal", DROW[64:65, :], DROW[64:65, :], reads=[tDROW], writes=[tDROW])
                    E("act", "copy", out=OTS[0:64, :], in_=acc[0:64, :], reads=[tacc], writes=[tOTS])

                def fin2():
                    E("pe", "matmul", P[5][0:64, :], ones65[64:65, 0:64], DROW[64:65, :], start=True, stop=True, reads=[tDROW, tC], writes=[tP[5]])

                def fin3(g=g, tl=tl):
                    E("dve", "tensor_tensor", AO[:, g * 4:(g + 1) * 4, tl * 128:(tl + 1) * 128], OTS[0:64, :].rearrange("p (i q) -> p i q", q=128),
                      P[5][0:64, :].rearrange("p (i q) -> p i q", q=128), ALU.mult, reads=[tOTS, tP[5]], writes=[tAO])

                groups.append(dict(pre=None, nkt=len(chunks), qk=qk, scale=0.125, pv=pv, fin1=fin1, fin2=fin2, fin3=fin3))
        attn_pipeline(E, groups, [P[2], P[3]], [tP[2], tP[3]], PT, tPT, 512)
        for c in range(4):
            for k in range(31):
                if m == 0:
                    rhs = HL[:, c, c0 + k - 15:c0 + k - 15 + n]
                else:
                    rhs = HC[:, c, k:k + n]
                E("pe", "matmul", P[6][:, 0:n], DG[:, c * 31 + k, :], rhs, start=(k == 0), stop=(k == 30), reads=[tDG] + allH, writes=[tP[6]])
            E("act", "activation", out=CV[:, c, 0:n], in_=P[6][:, 0:n], func=AF.Identity, bias=cvec[:, 0, c:c + 1], reads=[tP[6], tC], writes=[tCV])
        for c in range(4):
            E("pe", "matmul", P[5][:, 0:n], ones512[:], CV[:, c, 0:n], start=(c == 0), stop=(c == 3), reads=[tCV, tC], writes=[tP[5]])
        for c in range(4):
            E("act", "activation", out=LT[2][:, 0:n], in_=CV[:, c, 0:n], func=AF.Square, reads=[tCV], writes=[tLT[2]])
            E("pe", "matmul", P[7][:, 0:n], ones512[:], LT[2][:, 0:n], start=(c == 0), stop=(c == 3), reads=[tLT[2], tC], writes=[tP[7]])
        E("act", "copy", out=LT[0][:, 0:n], in_=P[5][:, 0:n], reads=[tP[5]], writes=[tLT[0]])
        E("dve", "tensor_tensor", LT[1][:, 0:n], LT[0][:, 0:n], LT[0][:, 0:n], ALU.mult, reads=[tLT[0]], writes=[tLT[1]])
        E("dve", "tensor_tensor", LT[1][:, 0:n], P[7][:, 0:n], LT[1][:, 0:n], ALU.subtract, reads=[tP[7], tLT[1]], writes=[tLT[1]])
        E("act", "activation", out=LT[1][:, 0:n], in_=LT[1][:, 0:n], func=AF.Sqrt, bias=epsc[:, 0:1], reads=[tLT[1], tC], writes=[tLT[1]])
        E("dve", "reciprocal", LT[1][:, 0:n], LT[1][:, 0:n], reads=[tLT[1]], writes=[tLT[1]])
        for c in range(4):
            E("dve", "tensor_tensor", CV[:, c, 0:n], CV[:, c, 0:n], LT[0][:, 0:n], ALU.subtract, reads=[tCV, tLT[0]], writes=[tCV])
            E("pool", "tensor_tensor", CV[:, c, 0:n], CV[:, c, 0:n], LT[1][:, 0:n], ALU.mult, reads=[tCV, tLT[1]], writes=[tCV])
            E("act", "activation", out=CO[:, c, 0:n], in_=CV[:, c, 0:n], func=AF.Silu, scale=cvec[:, 1, c:c + 1], bias=cvec[:, 2, c:c + 1],
              reads=[tCV, tC], writes=[tCO])
        for dk in range(8):
            pb = 6 + (dk % 2)
            for c in range(4):
                E("pe", "matmul", P[pb][:, 0:n], WOC[:, c, dk * 128:(dk + 1) * 128], CO[:, c, 0:n], start=(c == 0), stop=False,
                  reads=[tWO, tCO], writes=[tP[pb]])
            for hh in range(8):
                E("pe", "matmul", P[pb][:, 0:n], WOA[:, hh, dk * 128:(dk + 1) * 128], AO[:, hh, 0:n], start=False, stop=(hh == 7),
                  reads=[tWO, tAO], writes=[tP[pb]])
            E("dve", "tensor_scalar", LT[3][:, 0:n], P[pb][:, 0:n], bout[:, dk:dk + 1], g1p1[:, dk, m:m + 1], ALU.add, ALU.mult,
              reads=[tP[pb], tC], writes=[tLT[3]])
            E("dve", "scalar_tensor_tensor", XB[:, dk, 0:n], XB[:, dk, 0:n], DN_ALPHA, LT[3][:, 0:n], ALU.mult, ALU.add,
              reads=[tXB, tLT[3]], writes=[tXB])
        for k in range(8):
            E("pe", "matmul", P[5][:, 0:n], ones1k[:], XB[:, k, 0:n], start=(k == 0), stop=(k == 7), reads=[tXB, tC], writes=[tP[5]])
        for k in range(8):
            E("act", "activation", out=LT[2][:, 0:n], in_=XB[:, k, 0:n], func=AF.Square, reads=[tXB], writes=[tLT[2]])
            E("pe", "matmul", P[4][:, 0:n], ones1k[:], LT[2][:, 0:n], start=(k == 0), stop=(k == 7), reads=[tLT[2], tC], writes=[tP[4]])
        E("act", "copy", out=LT[0][:, 0:n], in_=P[5][:, 0:n], reads=[tP[5]], writes=[tLT[0]])
        E("dve", "tensor_tensor", LT[1][:, 0:n], LT[0][:, 0:n], LT[0][:, 0:n], ALU.mult, reads=[tLT[0]], writes=[tLT[1]])
        E("dve", "tensor_tensor", LT[1][:, 0:n], P[4][:, 0:n], LT[1][:, 0:n], ALU.subtract, reads=[tP[4], tLT[1]], writes=[tLT[1]])
        E("act", "activation", out=LT[1][:, 0:n], in_=LT[1][:, 0:n], func=AF.Sqrt, bias=epsc[:, 0:1], reads=[tLT[1], tC], writes=[tLT[1]])
        E("dve", "reciprocal", LT[1][:, 0:n], LT[1][:, 0:n], reads=[tLT[1]], writes=[tLT[1]])
        for k in range(8):
            ob = 0
            E("dve", "tensor_tensor", XB[:, k, 0:n], XB[:, k, 0:n], LT[0][:, 0:n], ALU.subtract, reads=[tXB, tLT[0]], writes=[tXB])
            E("pool", "tensor_tensor", XB[:, k, 0:n], XB[:, k, 0:n], LT[1][:, 0:n], ALU.mult, reads=[tXB, tLT[1]], writes=[tXB])
            E("act", "activation", out=XB[:, k, 0:n], in_=XB[:, k, 0:n], func=AF.Identity, scale=ln1[:, 0, k:k + 1], bias=ln1[:, 1, k:k + 1],
              reads=[tXB, tC], writes=[tXB])
            E("act", "mul", OUTA[ob][:, 0:n], XB[:, k, 0:n], DN_ALPHA, reads=[tXB], writes=[tOUTA[ob]])
            E("dve", "tensor_scalar", OUTB[ob][:, 0:n], XB[:, k, 0:n], sc2p1[:, k, m:m + 1], modv[:, 24 + k, m:m + 1], ALU.mult, ALU.add,
              reads=[tXB, tC], writes=[tOUTB[ob]])
            E("sp", "dma_start", out=d["xa"][:, k, t0:t0 + n], in_=OUTA[ob][:, 0:n], reads=[tOUTA[ob]], chan="oa%d" % ob)
            E("sp", "dma_start", out=d["u2f"][:, k, t0:t0 + n], in_=OUTB[ob][:, 0:n], reads=[tOUTB[ob]], chan="ob%d" % ob)
    S.flush()
    es.close()


F32 = mybir.dt.float32; BF16 = mybir.dt.bfloat16
AF = mybir.ActivationFunctionType
ALU = mybir.AluOpType
LN_EPS = 1e-5
RMS_EPS = 1e-6
DN_ALPHA = float(4 ** 0.25)


def emit_l1_mixer(nc, S, NL, SEQ, d):
    E = mkE(S)
    NK = SEQ + 256
    NKT = NK // 128
    T = NL
    es = ExitStack()
    sb = lambda name, shape, dt: es.enter_context(nc.sbuf_tensor("m1_" + name, shape, dt))
    ps = lambda name, shape, dt: es.enter_context(nc.psum_tensor("m1p_" + name, shape, dt))
    XB = sb("XB", [128, 8, 512], F32)
    UB = sb("UB", [128, 8, 512], BF16)
    KT = sb("KT", [128, NK], BF16)
    VG = sb("VG", [128, NKT, 2, 65], BF16)
    KVN = sb("KVN", [128, NK], BF16)
    KR = sb("KR", [128, NK], BF16)
    VM = sb("VM", [128, NKT, 4, 65], BF16)
    WQ = sb("WQ", [128, 8, 1280], BF16)
    WUQ = sb("WUQ", [128, 2, 8, 128], BF16)
    WUKT = sb("WUKT", [64, 8, 128], BF16)
    WUV = sb("WUV", [128, 512], BF16)
    WOS = [sb("WOS%d" % i, [64, 16, 128], BF16) for i in range(2)]
    QTZ = [sb("QTZ%d" % g, [128, 4, 4, 128], BF16) for g in range(2)]
    QCN = sb("QCN", [128, 2, 512], BF16)
    QN = sb("QN", [64, 512], BF16)
    QABS = sb("QABS", [128, 512], BF16)
    QR = sb("QR", [128, 512], BF16)
    PT = [sb("PT%d" % i, [128, 512], BF16) for i in range(3)]
    AO = sb("AO", [64, 16, 512], BF16)
    LT = [sb("LT%d" % i, [128, 512], F32) for i in range(4)]
    R1 = sb("R1", [128, 512], F32)
    R2 = sb("R2", [128, 512], F32)
    RS = sb("RS", [128, 512], F32)
    CG = sb("CG", [128, 512], F32)
    SG = sb("SG", [128, 512], F32)
    TB = [sb("TB%d" % i, [128, 512], F32) for i in range(2)]
    TM = [sb("TM%d" % i, [32, 512], F32) for i in range(2)]
    OTS = sb("OTS", [65, 512], F32)
    DROW = sb("DROW", [65, 512], F32)
    OUTA = [sb("OUTA%d" % i, [128, 512], F32) for i in range(2)]
    OUTB = [sb("OUTB%d" % i, [128, 512], F32) for i in range(2)]
    modv = sb("modv", [128, 48, 2], F32)
    sc1p1 = sb("sc1p1", [128, 8, 2], F32)
    g1p1 = sb("g1p1", [128, 8, 2], F32)
    sc2p1 = sb("sc2p1", [128, 8, 2], F32)
    cvec = sb("cvec", [128, 24], F32)
    bvb = sb("bvb", [128, 128], F32)
    bout = sb("bout", [128, 8], F32)
    ln1 = sb("ln1", [128, 2, 8], F32)
    bd64 = sb("bd64", [128, 128], F32)
    ones128 = sb("ones128", [128, 128], F32)
    ones256 = sb("ones256", [128, 128], F32)
    ones1k = sb("ones1k", [128, 128], F32)
    mhalf = sb("mhalf", [128, 8], F32)
    epsc = sb("epsc", [128, 2], F32)
    ones65 = sb("ones65", [65, 64], F32)
    P = [ps("P%d" % i, [128, 512], F32) for i in range(8)]
    tP = [Tile("P%d" % i) for i in range(8)]
    tC = Tile("const"); tXB = Tile("XB"); tUB = Tile("UB")
    NKB = (NK + 511) // 512
    tKT = [Tile("KT%d" % i) for i in range(NKB)]
    tVG = [Tile("VG%d" % i) for i in range(NKB)]
    tKVN = [Tile("KVN%d" % i) for i in range(NKB)]
    tKR = [Tile("KR%d" % i) for i in range(NKB)]
    tVM = Tile("VM")
    tWQ = Tile("WQ"); tWU = Tile("WU"); tWOS = [Tile("WOS%d" % i) for i in range(2)]
    tQT = Tile("QT"); tQCN = Tile("QCN"); tQN = Tile("QN"); tQABS = Tile("QABS"); tQR = Tile("QR")
    tPT = [Tile("PT%d" % i) for i in range(3)]
    tAO = Tile("AO")
    tLT = [Tile("LT%d" % i) for i in range(4)]
    tR1 = Tile("R1"); tR2 = Tile("R2"); tRS = Tile("RS"); tCG = Tile("CG"); tTB = Tile("TB"); tTM = Tile("TM")
    tOTS = Tile("OTS"); tDROW = Tile("DROW")
    tOUTA = [Tile("OUTA%d" % i) for i in range(2)]
    tOUTB = [Tile("OUTB%d" % i) for i in range(2)]

    cst = dict(writes=[tC], chan="k0", grp=0)
    E("sp", "dma_start", out=modv[:], in_=d["modv"], **cst)
    E("sp", "dma_start", out=cvec[:], in_=d["cvec"], **cst)
    E("sp", "dma_start", out=bvb[:], in_=bass.AP(d["bv"].tensor, 0, [[0, 128], [1, 128]]), **cst)
    E("sp", "dma_start", out=bout[:], in_=d["bout"], **cst)
    E("sp", "dma_start", out=ln1[:], in_=d["ln1"], **cst)
    E("sp", "dma_start", out=bd64[:], in_=d["bd64"], **cst)
    E("pool", "dma_start", out=WQ[:, :, 0:576], in_=d["wa"], writes=[tWQ], chan="k2")
    cu = dict(writes=[tWU], chan="k3", grp=0)
    E("pool", "dma_start", out=WUQ[:], in_=d["wuq"], **cu)
    E("pool", "dma_start", out=WUKT[:], in_=d["wukt"], **cu)
    E("pool", "dma_start", out=WUV[:], in_=d["wuv"], **cu)
    E("dve", "memset", ones128[:], 1.0 / 128.0, writes=[tC])
    E("dve", "memset", ones256[:], 1.0 / 256.0, writes=[tC])
    E("dve", "memset", ones1k[:], 1.0 / 1024.0, writes=[tC])
    E("dve", "memset", epsc[:, 0:1], LN_EPS, writes=[tC])
    E("dve", "memset", epsc[:, 1:2], 1e-6, writes=[tC])
    E("dve", "memset", ones65[:], 1.0, writes=[tC])
    E("pool", "memset", VG[:, :, :, 64:65], 1.0, writes=tVG)
    E("pool", "memset", VM[:, :, :, 64:65], 1.0, writes=[tVM])
    E("pool", "memset", KR[:], 0.0, writes=tKR)
    E("pool", "memset", QR[:], 0.0, writes=[tQR])
    for g in range(2):
        E("pool", "memset", QTZ[g][:], 0.0, writes=[tQT])
    E("dve", "tensor_scalar", sc1p1[:], modv[:, 8:16, :], 1.0, None, ALU.add, reads=[tC], writes=[tC])
    E("dve", "tensor_scalar", g1p1[:], modv[:, 16:24, :], 1.0, None, ALU.add, reads=[tC], writes=[tC])
    E("dve", "tensor_scalar", sc2p1[:], modv[:, 32:40, :], 1.0, None, ALU.add, reads=[tC], writes=[tC])

    def make_u(n, m):
        for k in range(8):
            eng = ("dve", "act", "pool", "dve", "act", "dve", "act", "pool")[k]
            if eng == "act":
                E("act", "activation", out=UB[:, k, 0:n], in_=XB[:, k, 0:n], func=AF.Identity, scale=sc1p1[:, k, m:m + 1], bias=modv[:, k, m:m + 1],
                  reads=[tXB, tC], writes=[tUB])
            else:
                E(eng, "tensor_scalar", UB[:, k, 0:n], XB[:, k, 0:n], sc1p1[:, k, m:m + 1], modv[:, k, m:m + 1], ALU.mult, ALU.add,
                  reads=[tXB, tC], writes=[tUB])

    def rstd_from(psrc, np_, n, bias_ap, ones_ap, extra=None):
        for j, (pap, ptile, bap) in enumerate(psrc):
            E("act", "activation", out=LT[2][0:np_, 0:n], in_=pap, func=AF.Square, bias=bap, reads=[ptile, tC], writes=[tLT[2]])
            E("pe", "matmul", P[5][0:np_, 0:n], ones_ap, LT[2][0:np_, 0:n], start=(j == 0), stop=(j == len(psrc) - 1), reads=[tLT[2], tC], writes=[tP[5]])
        E("act", "activation", out=RS[0:np_, 0:n], in_=P[5][0:np_, 0:n], func=AF.Sqrt, bias=epsc[0:np_, 1:2], reads=[tP[5], tC], writes=[tRS])
        E("dve", "reciprocal", RS[0:np_, 0:n], RS[0:np_, 0:n], reads=[tRS], writes=[tRS])

    kblocks = [(c0, min(512, NK - c0)) for c0 in range(0, NK, 512)]
    for bi, (c0, n) in enumerate(kblocks):
        m = 0 if c0 < SEQ else 1
        E("sp", "dma_start", out=XB[:, :, 0:n], in_=(d["xk"](c0, n) if callable(d["xk"]) else d["xk"][:, :, c0:c0 + n]), writes=[tXB], chan="xb")
        tg = dict(writes=[tTB], chan="tb", grp=bi)
        E("sp", "dma_start", out=TB[0][:, 0:n], in_=d["tkc"][:, c0:c0 + n], **tg)
        E("sp", "dma_start", out=TB[1][:, 0:n], in_=d["tks"][:, c0:c0 + n], **tg)
        tg2 = dict(writes=[tTM], chan="tm", grp=bi)
        E("sp", "dma_start", out=TM[0][:, 0:n], in_=d["tmc"][:, c0:c0 + n], **tg2)
        E("sp", "dma_start", out=TM[1][:, 0:n], in_=d["tms"][:, c0:c0 + n], **tg2)
        make_u(n, m)
        for k in range(8):
            E("pe", "matmul", P[0][:, 0:n], WQ[:, k, 0:128], UB[:, k, 0:n], start=(k == 0), stop=(k == 7), reads=[tWQ, tUB], writes=[tP[0]])
        for k in range(8):
            E("pe", "matmul", P[1][:, 0:n], WQ[:, k, 128:256], UB[:, k, 0:n], start=(k == 0), stop=(k == 7), reads=[tWQ, tUB], writes=[tP[1]])
        rstd_from([(P[0][:, 0:n], tP[0], cvec[:, 0:1])], 128, n, None, bd64[:])
        E("dve", "tensor_scalar", CG[:, 0:n], TB[0][:, 0:n], cvec[:, 3:4], None, ALU.mult, reads=[tTB, tC], writes=[tCG])
        E("dve", "tensor_scalar", SG[:, 0:n], TB[1][:, 0:n], cvec[:, 4:5], None, ALU.mult, reads=[tTB, tC], writes=[tCG])
        E("dve", "scalar_tensor_tensor", R1[:, 0:n], P[0][:, 0:n], cvec[:, 0:1], CG[:, 0:n], ALU.add, ALU.mult, reads=[tP[0], tCG, tC], writes=[tR1])
        E("dve", "scalar_tensor_tensor", R2[:, 0:n], P[1][:, 0:n], cvec[:, 1:2], SG[:, 0:n], ALU.add, ALU.mult, reads=[tP[1], tCG, tC], writes=[tR2])
        E("pool", "tensor_tensor", R1[:, 0:n], R1[:, 0:n], R2[:, 0:n], ALU.add, reads=[tR1, tR2], writes=[tR1])
        E("pool", "tensor_tensor", KT[:, c0:c0 + n], R1[:, 0:n], RS[:, 0:n], ALU.mult, reads=[tR1, tRS], writes=[tKT[bi]])
        for tt in range(n // 128):
            for k in range(8):
                E("pe", "matmul", P[4][:, 0:128], UB[:, k, tt * 128:(tt + 1) * 128], WQ[:, k, 256:384], start=(k == 0), stop=(k == 7),
                  reads=[tWQ, tUB], writes=[tP[4]])
            E("dve", "tensor_tensor", VG[:, c0 // 128 + tt, :, 0:64], P[4][:, 0:128].rearrange("p (g e) -> p g e", g=2),
              bvb[:].rearrange("p (g e) -> p g e", g=2), ALU.add, reads=[tP[4], tC], writes=[tVG[bi]])
        for k in range(8):
            E("pe", "matmul", P[2][:, 0:n], WQ[:, k, 384:512], UB[:, k, 0:n], start=(k == 0), stop=(k == 7), reads=[tWQ, tUB], writes=[tP[2]])
        rstd_from([(P[2][:, 0:n], tP[2], cvec[:, 2:3])], 128, n, None, ones128[:])
        E("dve", "scalar_tensor_tensor", R1[:, 0:n], P[2][:, 0:n], cvec[:, 2:3], RS[:, 0:n], ALU.add, ALU.mult, reads=[tP[2], tRS, tC], writes=[tR1])
        E("act", "activation", out=KVN[:, c0:c0 + n], in_=R1[:, 0:n], func=AF.Identity, scale=cvec[:, 5:6], reads=[tR1, tC], writes=[tKVN[bi]])
        for k in range(8):
            E("pe", "matmul", P[3][0:32, 0:n], WQ[:, k, 512:544], UB[:, k, 0:n], start=(k == 0), stop=(k == 7), reads=[tWQ, tUB], writes=[tP[3]])
        for k in range(8):
            E("pe", "matmul", P[6][0:32, 0:n], WQ[:, k, 544:576], UB[:, k, 0:n], start=(k == 0), stop=(k == 7), reads=[tWQ, tUB], writes=[tP[6]])
        E("dve", "scalar_tensor_tensor", R1[0:32, 0:n], P[3][0:32, 0:n], cvec[0:32, 20:21], TM[0][:, 0:n], ALU.add, ALU.mult, reads=[tP[3], tTM, tC], writes=[tR1])
        E("dve", "scalar_tensor_tensor", R2[0:32, 0:n], P[6][0:32, 0:n], cvec[0:32, 21:22], TM[1][:, 0:n], ALU.add, ALU.mult, reads=[tP[6], tTM, tC], writes=[tR2])
        E("pool", "tensor_tensor", KR[0:32, c0:c0 + n], R1[0:32, 0:n], R2[0:32, 0:n], ALU.add, reads=[tR1, tR2], writes=[tKR[bi]])
    E("pool", "dma_start", out=WQ[:], in_=d["wq"], writes=[tWQ], chan="k2")

    pcnt = [0]

    def normalise(nq_ap_out, reads_extra=()):
        E("dve", "reciprocal", DROW[64:65, :], P[4][64:65, :], reads=[tP[4]], writes=[tDROW])
        E("pe", "matmul", P[5][0:64, :], ones65[64:65, :], DROW[64:65, :], start=True, stop=True, reads=[tDROW, tC], writes=[tP[5]])
        E("act", "copy", out=OTS[0:64, :], in_=P[4][0:64, :], reads=[tP[4]], writes=[tOTS])

    oblocks = [(t0, 512) for t0 in range(0, NL, 512)]
    for bi, (t0, n) in enumerate(oblocks):
        E("sp", "dma_start", out=XB[:, :, 0:n], in_=d["xo"][:, :, t0:t0 + n], writes=[tXB], chan="xb")
        tg = dict(writes=[tTB], chan="tb", grp=100 + bi)
        E("sp", "dma_start", out=TB[0][:, 0:n], in_=d["tqc"][:, t0:t0 + n], **tg)
        E("sp", "dma_start", out=TB[1][:, 0:n], in_=d["tqs"][:, t0:t0 + n], **tg)
        tg2 = dict(writes=[tTM], chan="tm", grp=100 + bi)
        E("sp", "dma_start", out=TM[0][:, 0:n], in_=d["tmqc"][:, t0:t0 + n], **tg2)
        E("sp", "dma_start", out=TM[1][:, 0:n], in_=d["tmqs"][:, t0:t0 + n], **tg2)
        make_u(n, 0)
        E("dve", "tensor_scalar", CG[:, 0:n], TB[0][:, 0:n], cvec[:, 16:17], None, ALU.mult, reads=[tTB, tC], writes=[tCG])
        E("dve", "tensor_scalar", SG[:, 0:n], TB[1][:, 0:n], cvec[:, 17:18], None, ALU.mult, reads=[tTB, tC], writes=[tCG])
        for i in range(4):
            for k in range(8):
                E("pe", "matmul", P[0][:, 0:n], WQ[:, k, i * 128:(i + 1) * 128], UB[:, k, 0:n], start=(k == 0), stop=(k == 7), reads=[tWQ, tUB], writes=[tP[0]])
            for k in range(8):
                E("pe", "matmul", P[1][:, 0:n], WQ[:, k, 512 + i * 128:512 + (i + 1) * 128], UB[:, k, 0:n], start=(k == 0), stop=(k == 7), reads=[tWQ, tUB], writes=[tP[1]])
            rstd_from([(P[0][:, 0:n], tP[0], cvec[:, 6 + i:7 + i])], 128, n, None, bd64[:])
            E("dve", "scalar_tensor_tensor", R1[:, 0:n], P[0][:, 0:n], cvec[:, 6 + i:7 + i], CG[:, 0:n], ALU.add, ALU.mult, reads=[tP[0], tCG, tC], writes=[tR1])
            E("dve", "scalar_tensor_tensor", R2[:, 0:n], P[1][:, 0:n], cvec[:, 10 + i:11 + i], SG[:, 0:n], ALU.add, ALU.mult, reads=[tP[1], tCG, tC], writes=[tR2])
            E("pool", "tensor_tensor", R1[:, 0:n], R1[:, 0:n], R2[:, 0:n], ALU.add, reads=[tR1, tR2], writes=[tR1])
            for g in range(2):
                E("pool", "tensor_tensor", QTZ[g][g * 64:(g + 1) * 64, :, i, :], R1[g * 64:(g + 1) * 64, 0:n].rearrange("p (t q) -> p t q", q=128),
                  RS[g * 64:(g + 1) * 64, 0:n].rearrange("p (t q) -> p t q", q=128), ALU.mult, reads=[tR1, tRS], writes=[tQT])
        groups = []
        for tl in range(n // 128):
            for g in range(2):
                gi = len(groups)
                acc, tacc, bc, tbc = P[4 + 2 * (gi % 2)], tP[4 + 2 * (gi % 2)], P[5 + 2 * (gi % 2)], tP[5 + 2 * (gi % 2)]
                rhs_q = QTZ[g][:, tl, :, :].rearrange("p i q -> p (i q)")

                def qk(kt, ps_, tps, g=g, rhs_q=rhs_q):
                    E("pe", "matmul", ps_[:, :], KT[:, kt * 128:(kt + 1) * 128], rhs_q, start=True, stop=True,
                      reads=[tQT, tKT[kt // 4]], writes=[tps])

                def pv(kt, pt, tpt, st, sp, g=g, acc=acc, tacc=tacc):
                    E("pe", "matmul", acc[0:65, :], VG[:, kt, g, :], pt[:], start=st, stop=sp, reads=[tpt, tVG[kt // 4]], writes=[tacc])

                def fin1(acc=acc, tacc=tacc):
                    E("dve", "reciprocal", DROW[64:65, :], acc[64:65, :], reads=[tacc], writes=[tDROW])
                    E("act", "copy", out=OTS[0:64, :], in_=acc[0:64, :], reads=[tacc], writes=[tOTS])

                def fin2(bc=bc, tbc=tbc):
                    E("pe", "matmul", bc[0:64, :], ones65[64:65, :], DROW[64:65, :], start=True, stop=True, reads=[tDROW, tC], writes=[tbc])

                def fin3(bc=bc, tbc=tbc, g=g, tl=tl):
                    E("dve", "tensor_tensor", AO[:, g * 4:(g + 1) * 4, tl * 128:(tl + 1) * 128], OTS[0:64, :].rearrange("p (i q) -> p i q", q=128),
                      bc[0:64, :].rearrange("p (i q) -> p i q", q=128), ALU.mult, reads=[tOTS, tbc], writes=[tAO])

                groups.append(dict(pre=None, nkt=NKT, qk=qk, scale=0.125, pv=pv, fin1=fin1, fin2=fin2, fin3=fin3))
        attn_pipeline(E, groups, [P[2], P[3]], [tP[2], tP[3]], PT, tPT, 512)
        for j in range(2):
            for k in range(8):
                E("pe", "matmul", P[j][:, 0:n], WQ[:, k, 1024 + j * 128:1024 + (j + 1) * 128], UB[:, k, 0:n], start=(k == 0), stop=(k == 7),
                  reads=[tWQ, tUB], writes=[tP[j]])
        rstd_from([(P[0][:, 0:n], tP[0], cvec[:, 14:15]), (P[1][:, 0:n], tP[1], cvec[:, 15:16])], 128, n, None, ones256[:])
        for j in range(2):
            E("dve", "scalar_tensor_tensor", R1[:, 0:n], P[j][:, 0:n], cvec[:, 14 + j:15 + j], RS[:, 0:n], ALU.add, ALU.mult, reads=[tP[j], tRS, tC], writes=[tR1])
            E("act", "activation", out=QCN[:, j, 0:n], in_=R1[:, 0:n], func=AF.Identity, scale=cvec[:, 18 + j:19 + j], reads=[tR1, tC], writes=[tQCN])
        for hp in range(2):
            for kt in range(NKT):
                pb = 6 + (kt % 2)
                E("pe", "matmul", P[pb][:, 0:256], KVN[:, kt * 128:(kt + 1) * 128], WUV[:, hp * 256:(hp + 1) * 256], start=True, stop=True,
                  reads=[tKVN[kt // 4], tWU], writes=[tP[pb]])
                eng = "act" if kt % 2 == 0 else "dve"
                if eng == "act":
                    E("act", "copy", out=VM[:, kt, :, 0:64], in_=P[pb][:, 0:256].rearrange("p (h e) -> p h e", h=4), reads=[tP[pb]], writes=[tVM])
                else:
                    E("dve", "tensor_copy", out=VM[:, kt, :, 0:64], in_=P[pb][:, 0:256].rearrange("p (h e) -> p h e", h=4), reads=[tP[pb]], writes=[tVM])
            groups = []
            for hl in range(4):
                h = hp * 4 + hl
                acc, tacc = P[4 + (hl % 2)], tP[4 + (hl % 2)]

                def pre(h=h):
                    for j in range(2):
                        E("pe", "matmul", P[7][0:32, 0:n], WUQ[:, j, h, 64:96], QCN[:, j, 0:n], start=(j == 0), stop=(j == 1), reads=[tWU, tQCN], writes=[tP[7]])
                    for j in range(2):
                        E("pe", "matmul", P[1][0:32, 0:n], WUQ[:, j, h, 96:128], QCN[:, j, 0:n], start=(j == 0), stop=(j == 1), reads=[tWU, tQCN], writes=[tP[1]])
                    for j in range(2):
                        E("pe", "matmul", P[0][0:64, 0:n], WUQ[:, j, h, 0:64], QCN[:, j, 0:n], start=(j == 0), stop=(j == 1), reads=[tWU, tQCN], writes=[tP[0]])
                    E("act", "copy", out=QN[:, 0:n], in_=P[0][0:64, 0:n], reads=[tP[0]], writes=[tQN])
                    E("dve", "tensor_tensor", R1[0:32, 0:n], P[7][0:32, 0:n], TM[0][:, 0:n], ALU.mult, reads=[tP[7], tTM], writes=[tR1])
                    E("dve", "tensor_tensor", R2[0:32, 0:n], P[1][0:32, 0:n], TM[1][:, 0:n], ALU.mult, reads=[tP[1], tTM], writes=[tR2])
                    E("pool", "tensor_tensor", QR[0:32, 0:n], R1[0:32, 0:n], R2[0:32, 0:n], ALU.add, reads=[tR1, tR2], writes=[tQR])
                    E("pe", "matmul", P[1][:, 0:n], WUKT[:, h, :], QN[:, 0:n], start=True, stop=True, reads=[tWU, tQN], writes=[tP[1]])
                    E("act", "copy", out=QABS[:, 0:n], in_=P[1][:, 0:n], reads=[tP[1]], writes=[tQABS])

                def qk(kt, ps_, tps):
                    E("pe", "matmul", ps_[:, 0:n], KVN[:, kt * 128:(kt + 1) * 128], QABS[:, 0:n], start=True, stop=False,
                      reads=[tQABS, tKVN[kt // 4]], writes=[tps])
                    E("pe", "matmul", ps_[:, 0:n], KR[:, kt * 128:(kt + 1) * 128], QR[:, 0:n], start=False, stop=True,
                      reads=[tQR, tKR[kt // 4]], writes=[tps])

                def pv(kt, pt, tpt, st, sp, hl=hl, acc=acc, tacc=tacc):
                    E("pe", "matmul", acc[0:65, 0:n], VM[:, kt, hl, :], pt[:, 0:n], start=st, stop=sp, reads=[tpt, tVM], writes=[tacc])

                def fin1(acc=acc, tacc=tacc):
                    E("dve", "reciprocal", DROW[64:65, :], acc[64:65, :], reads=[tacc], writes=[tDROW])
                    E("act", "copy", out=OTS[0:64, :], in_=acc[0:64, :], reads=[tacc], writes=[tOTS])

                def fin2():
                    E("pe", "matmul", P[6][0:64, :], ones65[64:65, :], DROW[64:65, :], start=True, stop=True, reads=[tDROW, tC], writes=[tP[6]])

                def fin3(h=h):
                    E("dve", "tensor_tensor", AO[:, 8 + h, 0:n], OTS[0:64, 0:n], P[6][0:64, 0:n], ALU.mult, reads=[tOTS, tP[6]], writes=[tAO])

                groups.append(dict(pre=pre, nkt=NKT, qk=qk, scale=float(96 ** -0.5), pv=pv, fin1=fin1, fin2=fin2, fin3=fin3))
            attn_pipeline(E, groups, [P[2], P[3]], [tP[2], tP[3]], PT, tPT, n)
        for dk in range(8):
            pb = 6 + (dk % 2); wb = dk % 2
            E("pool", "dma_start", out=WOS[wb][:], in_=d["wo"][:, :, dk * 128:(dk + 1) * 128], writes=[tWOS[wb]], chan="wo%d" % wb)
            for hh in range(16):
                E("pe", "matmul", P[pb][:, 0:n], WOS[wb][:, hh, :], AO[:, hh, 0:n], start=(hh == 0), stop=(hh == 15),
                  reads=[tWOS[wb], tAO], writes=[tP[pb]])
            E("dve", "tensor_scalar", LT[3][:, 0:n], P[pb][:, 0:n], bout[:, dk:dk + 1], g1p1[:, dk, 0:1], ALU.add, ALU.mult,
              reads=[tP[pb], tC], writes=[tLT[3]])
            E("dve", "scalar_tensor_tensor", XB[:, dk, 0:n], XB[:, dk, 0:n], DN_ALPHA, LT[3][:, 0:n], ALU.mult, ALU.add,
              reads=[tXB, tLT[3]], writes=[tXB])
        for k in range(8):
            E("pe", "matmul", P[5][:, 0:n], ones1k[:], XB[:, k, 0:n], start=(k == 0), stop=(k == 7), reads=[tXB, tC], writes=[tP[5]])
        for k in range(8):
            E("act", "activation", out=LT[2][:, 0:n], in_=XB[:, k, 0:n], func=AF.Square, reads=[tXB], writes=[tLT[2]])
            E("pe", "matmul", P[4][:, 0:n], ones1k[:], LT[2][:, 0:n], start=(k == 0), stop=(k == 7), reads=[tLT[2], tC], writes=[tP[4]])
        E("act", "copy", out=LT[0][:, 0:n], in_=P[5][:, 0:n], reads=[tP[5]], writes=[tLT[0]])
        E("dve", "tensor_tensor", LT[1][:, 0:n], LT[0][:, 0:n], LT[0][:, 0:n], ALU.mult, reads=[tLT[0]], writes=[tLT[1]])
        E("dve", "tensor_tensor", LT[1][:, 0:n], P[4][:, 0:n], LT[1][:, 0:n], ALU.subtract, reads=[tP[4], tLT[1]], writes=[tLT[1]])
        E("act", "activation", out=LT[1][:, 0:n], in_=LT[1][:, 0:n], func=AF.Sqrt, bias=epsc[:, 0:1], reads=[tLT[1], tC], writes=[tLT[1]])
        E("dve", "reciprocal", LT[1][:, 0:n], LT[1][:, 0:n], reads=[tLT[1]], writes=[tLT[1]])
        for k in range(8):
            ob = k % 2
            E("dve", "tensor_tensor", XB[:, k, 0:n], XB[:, k, 0:n], LT[0][:, 0:n], ALU.subtract, reads=[tXB, tLT[0]], writes=[tXB])
            E("pool", "tensor_tensor", XB[:, k, 0:n], XB[:, k, 0:n], LT[1][:, 0:n], ALU.mult, reads=[tXB, tLT[1]], writes=[tXB])
            E("act", "activation", out=XB[:, k, 0:n], in_=XB[:, k, 0:n], func=AF.Identity, scale=ln1[:, 0, k:k + 1], bias=ln1[:, 1, k:k + 1],
              reads=[tXB, tC], writes=[tXB])
            E("act", "mul", OUTA[ob][:, 0:n], XB[:, k, 0:n], DN_ALPHA, reads=[tXB], writes=[tOUTA[ob]])
            E("dve", "tensor_scalar", OUTB[ob][:, 0:n], XB[:, k, 0:n], sc2p1[:, k, 0:1], modv[:, 24 + k, 0:1], ALU.mult, ALU.add,
              reads=[tXB, tC], writes=[tOUTB[ob]])
            E("sp", "dma_start", out=d["xa"][:, k, t0:t0 + n], in_=OUTA[ob][:, 0:n], reads=[tOUTA[ob]], chan="oa%d" % ob)
            E("sp", "dma_start", out=d["u2f"][:, k, t0:t0 + n], in_=OUTB[ob][:, 0:n], reads=[tOUTB[ob]], chan="ob%d" % ob)
    S.flush()
    es.close()


F32 = mybir.dt.float32; BF16 = mybir.dt.bfloat16
AF = mybir.ActivationFunctionType
ALU = mybir.AluOpType
AX = mybir.AxisListType
NE = 32
LN_EPS = 1e-5


def emit_moe(nc, S, T, d, nctx=0, tag=""):
    blocks = []
    t0 = 0
    while t0 < T - nctx:
        n = min(512, T - nctx - t0)
        blocks.append((t0, n))
        t0 += n
    bmode = [0] * len(blocks)
    if nctx:
        blocks.append((T - nctx, nctx))
        bmode.append(1)
    NB = len(blocks)
    es = ExitStack()
    sb = lambda name, shape, dt: es.enter_context(nc.sbuf_tensor("moe" + tag + "_" + name, shape, dt))
    ps = lambda name, shape, dt: es.enter_context(nc.psum_tensor("moep" + tag + "_" + name, shape, dt))
    XACC = sb("XACC", [128, 8, T], F32)
    U2 = sb("U2", [128, 8, T], BF16)
    WTT = sb("WTT", [64, T], BF16)
    u2f = [sb("u2f%d" % i, [128, 8, 128], F32) for i in range(2)]
    wr = sb("wr", [128, 8, 36], F32)
    brow = sb("brow", [128, 36], F32)
    ident = sb("ident", [128, 128], BF16)
    sel = sb("sel", [64, NE, 128], BF16)
    g2p1 = sb("g2p1", [128, 8, 2], F32)
    modv = sb("modv", [128, 48, 2], F32)
    ln2 = sb("ln2", [128, 2, 8], F32)
    ones_f = sb("ones_f", [128, 128], F32)
    mhalf = sb("mhalf", [128, 8], F32)
    epsc = sb("epsc", [128, 2], F32)
    W1 = [sb("W1_%d" % i, [128, 8, 512], BF16) for i in range(2)]
    W3 = [sb("W3_%d" % i, [128, 8, 512], BF16) for i in range(2)]
    W2 = [sb("W2_%d" % i, [128, 4, 1024], BF16) for i in range(2)]
    HID = [sb("HID%d" % i, [128, 4, 512], BF16) for i in range(2)]
    SIG = [sb("SIG%d" % i, [128, 512], BF16) for i in range(2)]
    TM = [sb("TM%d" % i, [128, 512], F32) for i in range(2)]
    MSK = [sb("MSK%d" % i, [128, 512], F32) for i in range(2)]
    RS = sb("RS", [128, 128], F32)
    WHL = sb("WHL", [128, 64], BF16)
    RS2 = sb("RS2", [128, 128], F32)
    WHL2 = sb("WHL2", [128, 64], BF16)
    PH1 = [ps("PH1_%d" % i, [128, 512], F32) for i in range(2)]
    PH3 = [ps("PH3_%d" % i, [128, 512], F32) for i in range(2)]
    PM = ps("PM", [128, 512], F32)
    PO = [ps("PO%d" % i, [128, 512], F32) for i in range(2)]
    PTR = ps("PTR", [128, 128], BF16)

    tXACC = [[Tile("xacc%d_%d" % (k, b)) for b in range(NB)] for k in range(8)]
    tU2 = [Tile("u2_%d" % b) for b in range(NB)]
    tWTT = [Tile("wtt_%d" % b) for b in range(NB)]
    tu2f = [Tile("u2f%d" % i) for i in range(2)]
    tconst = Tile("const")
    tW1 = [Tile("w1_%d" % i) for i in range(2)]
    tW3 = [Tile("w3_%d" % i) for i in range(2)]
    tW2 = [Tile("w2_%d" % i) for i in range(2)]
    tHID = [[Tile("hid%d_%d" % (i, f)) for f in range(4)] for i in range(2)]
    tSIG = [Tile("sig%d" % i) for i in range(2)]
    tTM = [Tile("tm%d" % i) for i in range(2)]
    tMSK = [Tile("msk%d" % i) for i in range(2)]
    SQ = MSK[1]; tSQ = tMSK[1]; LT = [TM[0], TM[1], MSK[0]]; tLT = [tTM[0], tTM[1], tMSK[0]]
    tRS = Tile("rs"); tWHL = Tile("whl")
    tPH1 = [Tile("ph1_%d" % i) for i in range(2)]
    tPH3 = [Tile("ph3_%d" % i) for i in range(2)]
    tPM = Tile("pm")
    tPO = [Tile("po%d" % i) for i in range(2)]
    tPTR = Tile("ptr")

    S.op("sp", lambda e: e.dma_start(out=wr[:], in_=d["wr"]), writes=[tconst], chan="c0", grp=0)
    S.op("sp", lambda e: e.dma_start(out=brow[:], in_=bass.AP(d["brow"].tensor, 0, [[0, 128], [1, 36]])), writes=[tconst], chan="c0", grp=0)
    S.op("sp", lambda e: e.dma_start(out=modv[:], in_=d["modv"]), writes=[tconst], chan="c0", grp=0)
    S.op("sp", lambda e: e.dma_start(out=ln2[:], in_=d["ln2"]), writes=[tconst], chan="c0", grp=0)
    S.op("pool", lambda e: e.dma_start(out=ident[:], in_=d["ident"]), writes=[tconst], chan="c1", grp=0)
    S.op("pool", lambda e: e.dma_start(out=sel[:], in_=d["sel"]), writes=[tconst], chan="c1", grp=0)
    S.op("dve", lambda e: e.tensor_scalar(g2p1[:], modv[:, 40:48, :], 1.0, None, ALU.add), reads=[tconst], writes=[tconst])
    S.op("dve", lambda e: e.memset(ones_f[:], 1.0 / 1024.0), writes=[tconst])
    S.op("dve", lambda e: e.memset(epsc[:, 0:1], LN_EPS), writes=[tconst])
    for k in range(8):
        for b, (t0, n) in enumerate(blocks):
            pass
    S.op("pool", lambda e: e.dma_start(out=W1[0][:], in_=d["w1"][0].rearrange("(k p) f -> p k f", p=128)), writes=[tW1[0]], chan="w1_0")
    S.op("pool", lambda e: e.dma_start(out=W3[0][:], in_=d["w3"][0].rearrange("(k p) f -> p k f", p=128)), writes=[tW3[0]], chan="w3_0")
    S.op("pool", lambda e: e.dma_start(out=W2[0][:], in_=d["w2"][0].rearrange("(k p) f -> p k f", p=128)), writes=[tW2[0]], chan="w2_0")

    NS = 2
    RSs = [RS, RS2]; WHLs = [WHL, WHL2]
    tRSs = [Tile("rs%d" % j) for j in range(NS)]; tWHLs = [Tile("whl%d" % j) for j in range(NS)]
    tPMs = [tPM for j in range(NS)]; tPTRs = [tPTR for j in range(NS)]; tSQs = [Tile("sqr%d" % j) for j in range(NS)]
    rtiles = [(b, t0, tt * 128) for b, (t0, n) in enumerate(blocks) for tt in range(n // 128)]

    def router_tile(idx, b, t0, c0):
        i = idx % NS
        ops = []
        A = ops.append
        RSj = RSs[i]; WHLj = WHLs[i]
        PMj = PM[:, 0:36]; PTRj = PTR[0:64, 0:128]
        R = [tRSs[i]]; a = t0 + c0
        A(lambda: S.op("sp", lambda e: e.dma_start(out=u2f[i][:], in_=d["u2f"][:, :, a:a + 128]), writes=[tu2f[i]], chan="u2f%d" % i))
        if i == 0:
            A(lambda: S.op("act", lambda e: e.copy(out=U2[:, :, a:a + 128], in_=u2f[i][:]), reads=[tu2f[i]], writes=[tU2[b]]))
        else:
            A(lambda: S.op("pool", lambda e: e.tensor_copy(out=U2[:, :, a:a + 128], in_=u2f[i][:]), reads=[tu2f[i]], writes=[tU2[b]]))

        def mm():
            for k in range(8):
                S.op("pe", (lambda k: lambda e: e.matmul(PMj, u2f[i][:, k, :], wr[:, k, :], start=(k == 0), stop=(k == 7)))(k),
                     reads=[tu2f[i], tconst], writes=[tPMs[i]])
        A(mm)
        L = RSj[:, 0:36]; gmax = RSj[:, 36:37]; ohg = RSj[:, 40:44]; dg = RSj[:, 44:48]; gsum = RSj[:, 48:49]; gw = RSj[:, 49:50]
        el = RSj[:, 52:60]; m1 = RSj[:, 60:61]; oh1 = RSj[:, 64:72]; el2 = RSj[:, 72:80]; m2 = RSj[:, 80:81]; oh2 = RSj[:, 84:92]
        d12 = RSj[:, 92:93]; ee = RSj[:, 93:94]; den = RSj[:, 94:95]; w1s = RSj[:, 95:96]; w2s = RSj[:, 96:97]; a1 = RSj[:, 97:98]; a2 = RSj[:, 98:99]
        tq = RSj[:, 100:108]; ing = RSj[:, 108:116]
        wtok = SQ[:, 32 * i:32 * i + 32]
        D = lambda fn, reads=R, writes=R: A(lambda: S.op("dve", fn, reads=reads, writes=writes))
        D(lambda e: e.tensor_tensor(L, PMj, brow[:], ALU.add), reads=[tPMs[i], tconst])
        D(lambda e: e.tensor_reduce(gmax, RSj[:, 0:4], AX.X, ALU.max))
        D(lambda e: e.tensor_scalar(ohg, RSj[:, 0:4], gmax, None, ALU.is_equal))
        D(lambda e: e.tensor_scalar(dg, RSj[:, 0:4], gmax, None, ALU.subtract))
        A(lambda: S.op("act", lambda e: e.activation(out=dg, in_=dg, func=AF.Exp, accum_out=gsum), reads=R, writes=R))
        D(lambda e: e.reciprocal(gw, gsum))
        D(lambda e: e.tensor_scalar(el, RSj[:, 4:12], RSj[:, 40:41], None, ALU.mult))
        for g in range(1, 4):
            D((lambda g: lambda e: e.scalar_tensor_tensor(el, RSj[:, 4 + 8 * g:12 + 8 * g], RSj[:, 40 + g:41 + g], el, ALU.mult, ALU.add))(g))
        D(lambda e: e.tensor_reduce(m1, el, AX.X, ALU.max))
        D(lambda e: e.tensor_scalar(oh1, el, m1, None, ALU.is_equal))
        D(lambda e: e.scalar_tensor_tensor(el2, oh1, -1e30, el, ALU.mult, ALU.add))
        D(lambda e: e.tensor_reduce(m2, el2, AX.X, ALU.max))
        D(lambda e: e.tensor_scalar(oh2, el2, m2, None, ALU.is_equal))
        D(lambda e: e.tensor_tensor(d12, m2, m1, ALU.subtract))
        A(lambda: S.op("act", lambda e: e.activation(out=ee, in_=d12, func=AF.Exp), reads=R, writes=R))
        D(lambda e: e.tensor_scalar(den, ee, 1.0, None, ALU.add))
        D(lambda e: e.reciprocal(w1s, den))
        D(lambda e: e.tensor_tensor(w2s, ee, w1s, ALU.mult))
        D(lambda e: e.tensor_tensor(a1, w1s, gw, ALU.mult))
        D(lambda e: e.tensor_tensor(a2, w2s, gw, ALU.mult))
        D(lambda e: e.tensor_scalar(tq, oh1, a1, None, ALU.mult))
        D(lambda e: e.scalar_tensor_tensor(ing, oh2, a2, tq, ALU.mult, ALU.add))
        for g in range(4):
            D((lambda g: lambda e: e.tensor_scalar(wtok[:, 8 * g:8 * g + 8], ing, RSj[:, 40 + g:41 + g], None, ALU.mult))(g), writes=[tSQs[i]])
        D(lambda e: e.tensor_copy(out=WHLj[:, 0:32], in_=wtok), reads=[tSQs[i]], writes=[tWHLs[i]])
        D(lambda e: e.tensor_tensor(WHLj[:, 32:64], wtok, WHLj[:, 0:32], ALU.subtract), reads=[tSQs[i], tWHLs[i]], writes=[tWHLs[i]])
        A(lambda: S.op("pe", lambda e: e.transpose(PTRj, WHLj[:, :], ident[:]), reads=[tWHLs[i], tconst], writes=[tPTRs[i]]))
        A(lambda: S.op("act", lambda e: e.copy(out=WTT[:, a:a + 128], in_=PTRj), reads=[tPTRs[i]], writes=[tWTT[b]]))
        return ops

    for base in range(0, len(rtiles), NS):
        lists = [router_tile(base + u, *rtiles[base + u]) for u in range(NS) if base + u < len(rtiles)]
        LAG = 3
        for step in range(max(len(l) for l in lists) + LAG * (len(lists) - 1)):
            for u, l in enumerate(lists):
                if 0 <= step - LAG * u < len(l):
                    l[step - LAG * u]()

    def load_w(e_, slot):
        S.op("pool", lambda e: e.dma_start(out=W1[slot][:], in_=d["w1"][e_].rearrange("(k p) f -> p k f", p=128)), writes=[tW1[slot]], chan="w1_%d" % slot)
        S.op("pool", lambda e: e.dma_start(out=W3[slot][:], in_=d["w3"][e_].rearrange("(k p) f -> p k f", p=128)), writes=[tW3[slot]], chan="w3_%d" % slot)
        S.op("pool", lambda e: e.dma_start(out=W2[slot][:], in_=d["w2"][e_].rearrange("(k p) f -> p k f", p=128)), writes=[tW2[slot]], chan="w2_%d" % slot)

    def load_w13(e_, slot):
        S.op("pool", lambda e: e.dma_start(out=W1[slot][:], in_=d["w1"][e_].rearrange("(k p) f -> p k f", p=128)), writes=[tW1[slot]], chan="w1_%d" % slot)
        S.op("pool", lambda e: e.dma_start(out=W3[slot][:], in_=d["w3"][e_].rearrange("(k p) f -> p k f", p=128)), writes=[tW3[slot]], chan="w3_%d" % slot)

    def load_w2(e_, slot):
        S.op("pool", lambda e: e.dma_start(out=W2[slot][:], in_=d["w2"][e_].rearrange("(k p) f -> p k f", p=128)), writes=[tW2[slot]], chan="w2_%d" % slot)

    ocnt = [0]

    def phase1(ex, b, hb):
        slot = ex % 2
        t0, n = blocks[b]
        S.op("pe", (lambda ex, t0, n: lambda e: e.matmul(PM[:, 0:n], sel[:, ex, :], WTT[:, t0:t0 + n], start=True, stop=True))(ex, t0, n),
             reads=[tconst, tWTT[b]], writes=[tPM] + tPMs)
        S.op("act", (lambda hb, n: lambda e: e.copy(out=MSK[hb][:, 0:n], in_=PM[:, 0:n]))(hb, n), reads=[tPM], writes=[tMSK[hb]] + (tSQs if hb == 1 else []))
        for fc in range(4):
            pb = fc % 2
            for k in range(8):
                S.op("pe", (lambda slot, fc, k, pb, t0, n: lambda e: e.matmul(PH1[pb][:, 0:n], W1[slot][:, k, fc * 128:(fc + 1) * 128], U2[:, k, t0:t0 + n], start=(k == 0), stop=(k == 7)))(slot, fc, k, pb, t0, n),
                     reads=[tW1[slot], tU2[b]], writes=[tPH1[pb]])
            for k in range(8):
                S.op("pe", (lambda slot, fc, k, pb, t0, n: lambda e: e.matmul(PH3[pb][:, 0:n], W3[slot][:, k, fc * 128:(fc + 1) * 128], U2[:, k, t0:t0 + n], start=(k == 0), stop=(k == 7)))(slot, fc, k, pb, t0, n),
                     reads=[tW3[slot], tU2[b]], writes=[tPH3[pb]])
            S.op("act", (lambda pb, n: lambda e: e.activation(out=SIG[pb][:, 0:n], in_=PH1[pb][:, 0:n], func=AF.Silu))(pb, n), reads=[tPH1[pb]], writes=[tSIG[pb]])
            S.op("dve", (lambda pb, hb, n: lambda e: e.tensor_tensor(TM[pb][:, 0:n], PH3[pb][:, 0:n], MSK[hb][:, 0:n], ALU.mult))(pb, hb, n), reads=[tPH3[pb], tMSK[hb]], writes=[tTM[pb]])
            S.op("pool", (lambda pb, hb, fc, n: lambda e: e.tensor_tensor(HID[hb][:, fc, 0:n], SIG[pb][:, 0:n], TM[pb][:, 0:n], ALU.mult))(pb, hb, fc, n), reads=[tSIG[pb], tTM[pb]], writes=[tHID[hb][fc]])

    def phase2(ex, b, hb):
        slot = ex % 2
        t0, n = blocks[b]
        for dk in range(8):
            ob = ocnt[0] % 2
            ocnt[0] += 1
            for fc in range(4):
                S.op("pe", (lambda slot, fc, dk, ob, hb, n: lambda e: e.matmul(PO[ob][:, 0:n], W2[slot][:, fc, dk * 128:(dk + 1) * 128], HID[hb][:, fc, 0:n], start=(fc == 0), stop=(fc == 3)))(slot, fc, dk, ob, hb, n),
                     reads=[tW2[slot], tHID[hb][fc]], writes=[tPO[ob]])
            S.op("dve", (lambda dk, ob, t0, n, m: lambda e: e.scalar_tensor_tensor(XACC[:, dk, t0:t0 + n], PO[ob][:, 0:n], g2p1[:, dk, m:m + 1], XACC[:, dk, t0:t0 + n], ALU.mult, ALU.add))(dk, ob, t0, n, bmode[b]),
                 reads=[tPO[ob], tconst, tXACC[dk][b]], writes=[tXACC[dk][b]])

    for k in range(8):
        S.op("sp", (lambda k: lambda e: e.dma_start(out=XACC[:, k, :], in_=d["xa"][:, k, :]))(k),
             writes=[tXACC[k][b] for b in range(NB)], chan="xa%d" % (k % 2), grp=0)
    items = [(ex, b) for ex in range(NE) for b in range(NB)]
    prev = None
    for idx, (ex, b) in enumerate(items):
        hb = idx % 2
        if b == 0 and ex + 1 < NE:
            load_w13(ex + 1, (ex + 1) % 2)
        phase1(ex, b, hb)
        if prev is not None:
            phase2(*prev)
        if b == 0 and ex + 1 < NE:
            load_w2(ex + 1, (ex + 1) % 2)
        prev = (ex, b, hb)
    phase2(*prev)

    for b, (t0, n) in enumerate(blocks):
        for k in range(8):
            S.op("pe", (lambda k, t0, n: lambda e: e.matmul(PH1[0][:, 0:n], ones_f[:], XACC[:, k, t0:t0 + n], start=(k == 0), stop=(k == 7)))(k, t0, n),
                 reads=[tconst, tXACC[k][b]], writes=[tPH1[0]])
        for k in range(8):
            S.op("act", (lambda k, t0, n: lambda e: e.activation(out=SQ[:, 0:n], in_=XACC[:, k, t0:t0 + n], func=AF.Square))(k, t0, n), reads=[tXACC[k][b]], writes=[tSQ])
            S.op("pe", (lambda k, n: lambda e: e.matmul(PH3[0][:, 0:n], ones_f[:], SQ[:, 0:n], start=(k == 0), stop=(k == 7)))(k, n), reads=[tconst, tSQ], writes=[tPH3[0]])
        mean, var, rstd = LT[0], LT[1], LT[2]
        S.op("act", (lambda n: lambda e: e.copy(out=mean[:, 0:n], in_=PH1[0][:, 0:n]))(n), reads=[tPH1[0]], writes=[tLT[0]])
        S.op("dve", (lambda n: lambda e: e.tensor_tensor(var[:, 0:n], mean[:, 0:n], mean[:, 0:n], ALU.mult))(n), reads=[tLT[0]], writes=[tLT[1]])
        S.op("dve", (lambda n: lambda e: e.tensor_tensor(var[:, 0:n], PH3[0][:, 0:n], var[:, 0:n], ALU.subtract))(n), reads=[tPH3[0], tLT[1]], writes=[tLT[1]])
        S.op("act", (lambda n: lambda e: e.activation(out=rstd[:, 0:n], in_=var[:, 0:n], func=AF.Sqrt, bias=epsc[:, 0:1]))(n), reads=[tLT[1], tconst], writes=[tLT[2]])
        S.op("dve", (lambda n: lambda e: e.reciprocal(rstd[:, 0:n], rstd[:, 0:n]))(n), reads=[tLT[2]], writes=[tLT[2]])
        for k in range(8):
            S.op("dve", (lambda k, t0, n: lambda e: e.tensor_tensor(XACC[:, k, t0:t0 + n], XACC[:, k, t0:t0 + n], mean[:, 0:n], ALU.subtract))(k, t0, n),
                 reads=[tXACC[k][b], tLT[0]], writes=[tXACC[k][b]])
            S.op("pool", (lambda k, t0, n: lambda e: e.tensor_tensor(XACC[:, k, t0:t0 + n], XACC[:, k, t0:t0 + n], rstd[:, 0:n], ALU.mult))(k, t0, n),
                 reads=[tXACC[k][b], tLT[2]], writes=[tXACC[k][b]])
            S.op("act", (lambda k, t0, n: lambda e: e.activation(out=XACC[:, k, t0:t0 + n], in_=XACC[:, k, t0:t0 + n], func=AF.Identity, scale=ln2[:, 0, k:k + 1], bias=ln2[:, 1, k:k + 1]))(k, t0, n),
                 reads=[tXACC[k][b], tconst], writes=[tXACC[k][b]])
            if bmode[b] == 0:
                dst = d["xs"][:, k, t0:t0 + n]
            else:
                dst = d["xsc"][:, k, 0:n]
            S.op("sp", (lambda dst, k, t0, n: lambda e: e.dma_start(out=dst, in_=XACC[:, k, t0:t0 + n]))(dst, k, t0, n),
                 reads=[tXACC[k][b]], chan="out%d" % (k % 2), grp=b)
    S.flush()
    es.close()


F32 = mybir.dt.float32
AF = mybir.ActivationFunctionType
ALU = mybir.AluOpType


def emit_ada(nc, S, d):
    E = mkE(S)
    es = ExitStack()
    sb = lambda name, shape, dt: es.enter_context(nc.sbuf_tensor("ada_" + name, shape, dt))
    ps = lambda name, shape, dt: es.enter_context(nc.psum_tensor("adap_" + name, shape, dt))
    SC = sb("SC", [128, 8, 2], F32)
    AB = sb("AB", [128, 2, 48], F32)
    AW = [sb("AW%d" % i, [128, 8, 1024], F32) for i in range(2)]
    MV = sb("MV", [128, 48, 2], F32)
    PA = ps("PA", [128, 96], F32)
    tC = Tile("c"); tAW = [Tile("aw%d" % i) for i in range(2)]; tPA = Tile("pa"); tMV = Tile("mv")
    E("sp", "dma_start", out=SC[:], in_=d["cin"], writes=[tC], chan="a0", grp=0)
    E("sp", "dma_start", out=AB[:], in_=d["adab"], writes=[tC], chan="a0", grp=0)
    E("act", "activation", out=SC[:], in_=SC[:], func=AF.Silu, reads=[tC], writes=[tC])
    cnt = 0
    for l in range(2):
        for fg in range(6):
            i = cnt % 2
            cnt += 1
            src = d["adaw"][l].rearrange("(k p) f -> p k f", p=128)[:, :, fg * 1024:(fg + 1) * 1024]
            E("sp" if i == 0 else "act", "dma_start", out=AW[i][:], in_=src, writes=[tAW[i]], chan="aw%d" % i)
            for jj in range(8):
                j = fg * 8 + jj
                for k in range(8):
                    E("pe", "matmul", PA[:, 2 * j:2 * j + 2], AW[i][:, k, jj * 128:(jj + 1) * 128], SC[:, k, :], start=(k == 0), stop=(k == 7),
                      reads=[tAW[i], tC], writes=[tPA])
        for col in range(2):
            E("dve", "tensor_tensor", MV[:, :, col], PA[:].rearrange("p (j c) -> p j c", c=2)[:, :, col], AB[:, l, :], ALU.add,
              reads=[tPA, tC], writes=[tMV])
        E("sp", "dma_start", out=d["modv%d" % l], in_=MV[:], reads=[tMV], chan="a1")
    S.flush()
    es.close()

D_MODEL = 1024
SEQ_FULL = 4096
NLC = 2048
NCTX = 256
PERM = np.array([d + 16 if (d % 32) < 16 else d - 16 for d in range(64)])
PERM32 = np.array([d + 8 if (d % 16) < 8 else d - 8 for d in range(32)])


def fm(a):
    Dd = a.shape[1]
    return np.ascontiguousarray(a.T.reshape(Dd // 128, 128, -1).transpose(1, 0, 2))


def unfm(a):
    return a.transpose(1, 0, 2).reshape(a.shape[0] * a.shape[1], -1).T


def pv(v, p=128):
    return np.ascontiguousarray(np.asarray(v).reshape(-1, p).T)


def rope_tabs(pos, rot):
    axis = rot // 2
    inv = (10000.0 ** (-np.arange(0, axis, 2, dtype=np.float32) / axis)).astype(np.float32)
    row = (pos // 64).astype(np.float32); col = (pos % 64).astype(np.float32)
    ar = row[None, :] * inv[:, None]; ac = col[None, :] * inv[:, None]
    C = np.concatenate([np.cos(ar), np.cos(ar), np.cos(ac), np.cos(ac)], 0)
    Sg = np.concatenate([-np.sin(ar), np.sin(ar), -np.sin(ac), np.sin(ac)], 0)
    return C.astype(np.float32), Sg.astype(np.float32)


def prep_l0_weights(p, j=0):
    w_in = np.asarray(p['even_w_in'][j]); b_in = np.asarray(p['even_b_in'][j])
    qcols = 1024 + np.arange(512); kcols = 1536 + np.arange(128); vcols = 1664 + np.arange(128)
    def permcols(cols):
        c = cols.reshape(-1, 64)
        return c[:, PERM].reshape(-1)
    order = np.concatenate([np.arange(1024), kcols, permcols(kcols), vcols, qcols, permcols(qcols)])
    win = np.ascontiguousarray(w_in[:, order].reshape(8, 128, -1).transpose(1, 0, 2))
    b64 = np.zeros((64, 20), np.float32)
    for hh in range(8):
        b64[:, hh] = b_in[1024 + hh * 64 + np.arange(64)]
        b64[:, 8 + hh] = b_in[1024 + hh * 64 + PERM]
    for g in range(2):
        b64[:, 16 + g] = b_in[1536 + g * 64 + np.arange(64)]
        b64[:, 18 + g] = b_in[1536 + g * 64 + PERM]
    b128 = pv(b_in[0:1024])
    w_out = np.asarray(p['even_w_out'][j])
    mb = np.zeros((128, 2, 4, 128), np.float32)
    kk = np.arange(128)[:, None]; qq = np.arange(128)[None, :]
    mb[:, 0] = np.where(kk >= qq, 0.0, -30000.0)[:, None, :]
    mb[:, 1] = np.where(kk <= qq, 0.0, -30000.0)[:, None, :]
    return dict(win=win, b64=b64, b128=b128, bv=np.ascontiguousarray(b_in[1664:1792]),
                cw=np.ascontiguousarray(np.asarray(p['even_conv_w'][j])[:, 0, :].reshape(31, 4, 128).transpose(2, 1, 0)),
                cvec0=np.ascontiguousarray(np.stack([pv(p['even_conv_b'][j]), pv(p['even_conv_ln_g'][j]), pv(p['even_conv_ln_b'][j])], 1)),
                sink=np.ascontiguousarray(np.asarray(p['even_sink'][j])),
                woc=np.ascontiguousarray(w_out[0:512].reshape(4, 128, 1024).transpose(1, 0, 2)),
                woa=np.ascontiguousarray(w_out[512:1024].reshape(8, 64, 1024).transpose(1, 0, 2)),
                bout0=pv(p['even_b_out'][j]),
                ln1_0=np.ascontiguousarray(np.stack([pv(p['ln1_g'][0]), pv(p['ln1_b'][0])], 1)),
                mb=np.ascontiguousarray(mb.reshape(128, 2, 512)), ident=np.eye(128, dtype=np.float32))


def prep_l0_core(h, NL, S, x_b, ctx_b):
    EXT = NL + 256; NC = EXT + 256
    pos = h * NL - 128 + np.arange(EXT)
    valid = (pos >= 0) & (pos < S)
    xe = np.zeros((NC, D_MODEL), np.float32)
    xe[:EXT][valid] = x_b[pos[valid]]
    xe[EXT:] = ctx_b
    C, Sg = rope_tabs(np.clip(pos, 0, S - 1), 64)
    tabc = np.concatenate([C, np.ones((64, 256), np.float32)], 1)
    tabs = np.concatenate([Sg, np.zeros((64, 256), np.float32)], 1)
    kbias = np.concatenate([np.where(valid, 0.0, -30000.0), np.zeros(256)]).astype(np.float32)
    vm = np.concatenate([valid[:128], valid[EXT - 128:]]).astype(np.float32)
    return dict(xe=fm(xe), tabc=np.ascontiguousarray(tabc), tabs=np.ascontiguousarray(tabs), kbias=kbias,
                vmask=np.ascontiguousarray(np.broadcast_to(vm[None, :], (128, 256))))


def prep_l1_weights(p, j=0, li=1):
    w_in = np.asarray(p['odd_w_in'][j]); b_in = np.asarray(p['odd_b_in'][j])
    def permc(cols, perm=PERM):
        return cols.reshape(-1, len(perm))[:, perm].reshape(-1)
    kc = 768 + np.arange(128); vc = 896 + np.arange(128); kvc = 1024 + np.arange(128); krc = 1152 + np.arange(32)
    qpair = np.concatenate([np.concatenate([(0 * 4 + i) * 64 + np.arange(64), (1 * 4 + i) * 64 + np.arange(64)]) for i in range(4)])
    qc = 512 + np.arange(256)
    wa_cols = np.concatenate([kc, permc(kc), vc, kvc, krc, permc(krc, PERM32)])
    wq_cols = np.concatenate([qpair, permc(qpair), qc])
    lay = lambda cols: np.ascontiguousarray(w_in[:, cols].reshape(8, 128, -1).transpose(1, 0, 2))
    cvec = np.zeros((128, 24), np.float32)
    cvec[:, 0] = b_in[kc]; cvec[:, 1] = b_in[permc(kc)]; cvec[:, 2] = b_in[kvc]
    gk = np.asarray(p['odd_k_norm'][j]); gq = np.asarray(p['odd_q_norm'][j])
    cvec[:, 3] = np.tile(gk, 2); cvec[:, 4] = np.tile(gk[PERM], 2); cvec[:, 5] = np.asarray(p['odd_mla_kv_norm'][j])
    pq = permc(qpair)
    for i in range(4):
        cvec[:, 6 + i] = b_in[qpair[i * 128:(i + 1) * 128]]; cvec[:, 10 + i] = b_in[pq[i * 128:(i + 1) * 128]]
    cvec[:, 14] = b_in[512:640]; cvec[:, 15] = b_in[640:768]
    cvec[:, 16] = np.tile(gq, 2); cvec[:, 17] = np.tile(gq[PERM], 2)
    gqc = np.asarray(p['odd_mla_q_norm'][j]); cvec[:, 18] = gqc[:128]; cvec[:, 19] = gqc[128:]
    cvec[:32, 20] = b_in[krc]; cvec[:32, 21] = b_in[permc(krc, PERM32)]
    w_uq = np.asarray(p['odd_mla_w_uq'][j]); w_ukv = np.asarray(p['odd_mla_w_ukv'][j]); w_out = np.asarray(p['odd_w_out'][j])
    wuq = np.zeros((128, 2, 8, 128), np.float32)
    for jj in range(2):
        for h in range(8):
            blk = w_uq[jj * 128:(jj + 1) * 128, h * 96:(h + 1) * 96]
            wuq[:, jj, h, 0:64] = blk[:, 0:64]; wuq[:, jj, h, 64:96] = blk[:, 64:96]; wuq[:, jj, h, 96:128] = blk[:, 64 + PERM32]
    wukt = np.zeros((64, 8, 128), np.float32); wuv = np.zeros((128, 512), np.float32)
    for h in range(8):
        wukt[:, h, :] = w_ukv[:, h * 128:h * 128 + 64].T
        wuv[:, h * 64:(h + 1) * 64] = w_ukv[:, h * 128 + 64:(h + 1) * 128]
    bd64 = np.zeros((128, 128), np.float32); bd64[:64, :64] = 1 / 64; bd64[64:, 64:] = 1 / 64
    return dict(wa=lay(wa_cols), wq=lay(wq_cols), cvec1=cvec, bv1=np.ascontiguousarray(b_in[vc]), wuq=wuq, wukt=wukt, wuv=wuv,
                wo=np.ascontiguousarray(w_out.reshape(16, 64, 1024).transpose(1, 0, 2)), bout1=pv(p['odd_b_out'][j]),
                ln1_1=np.ascontiguousarray(np.stack([pv(p['ln1_g'][li]), pv(p['ln1_b'][li])], 1)), bd64=bd64)


def prep_l1_tables(SEQ):
    pos = np.arange(SEQ)
    C, Sg = rope_tabs(pos, 64); Cm, Sm = rope_tabs(pos, 32)
    one = lambda r: np.ones((r, 256), np.float32); zero = lambda r: np.zeros((r, 256), np.float32)
    t2 = lambda a: np.concatenate([a, a], 0)
    shared = dict(tkc=np.ascontiguousarray(np.concatenate([t2(C), one(128)], 1)), tks=np.ascontiguousarray(np.concatenate([t2(Sg), zero(128)], 1)),
                  tmc=np.ascontiguousarray(np.concatenate([Cm, one(32)], 1)), tms=np.ascontiguousarray(np.concatenate([Sm, zero(32)], 1)))
    per_h = []
    for h in range(2):
        sl = slice(h * NLC, (h + 1) * NLC)
        per_h.append(dict(tqc=np.ascontiguousarray(t2(C)[:, sl]), tqs=np.ascontiguousarray(t2(Sg)[:, sl]),
                          tmqc=np.ascontiguousarray(Cm[:, sl]), tmqs=np.ascontiguousarray(Sm[:, sl])))
    return shared, per_h


def prep_moe(p, l):
    w_rg = np.asarray(p['moe_w_rg'][l]); w_re = np.asarray(p['moe_w_re'][l])
    wr = np.concatenate([w_rg, w_re.transpose(1, 0, 2).reshape(D_MODEL, 32)], axis=1)
    wr_l = np.ascontiguousarray(wr.reshape(8, 128, 36).transpose(1, 0, 2))
    brow = np.concatenate([np.asarray(p['moe_b_rg'][l]), np.asarray(p['moe_b_re'][l]).reshape(-1)]).astype(np.float32)
    ln2 = np.ascontiguousarray(np.stack([pv(p['ln2_g'][l]), pv(p['ln2_b'][l])], 1))
    return {"wr%d" % l: wr_l, "brow%d" % l: brow, "ln2_%d" % l: ln2,
            "w1_%d" % l: np.asarray(p['moe_w1'][l]), "w3_%d" % l: np.asarray(p['moe_w3'][l]), "w2_%d" % l: np.asarray(p['moe_w2'][l])}


def make_sel():
    sel = np.zeros((64, NE, 128), np.float32)
    for e in range(NE):
        sel[e, e, :] = 1; sel[32 + e, e, :] = 1
    return sel


_PROG = {}


def _decl(nc, d, name, shape, kind="ExternalInput"):
    if kind is None:
        d[name] = nc.dram_tensor(name, list(shape), F32).ap()
    else:
        d[name] = nc.dram_tensor(name, list(shape), F32, kind=kind).ap()


def build_fused():
    nc = bass.Bass("TRN2", target_bir_lowering=False)
    NL = NLC; EXT = NL + 256; NC = EXT + 256; T = NL + 256; SEQ = SEQ_FULL; NK = SEQ + 256
    d = {}
    I = lambda n, s: _decl(nc, d, n, s)
    I("cin", [128, 8, 2]); I("adaw", [2, 1024, 6144]); I("adab", [128, 2, 48])
    for sg in ("A", "B"):
        I("xe" + sg, [128, 8, NC]); I("tabc" + sg, [64, NC]); I("tabs" + sg, [64, NC]); I("kbias" + sg, [NC]); I("vmask" + sg, [128, 256])
    I("win", [128, 8, 2432]); I("b128", [128, 8]); I("b64", [64, 20]); I("bv", [128])
    I("cw", [128, 4, 31]); I("cvec0", [128, 3, 4]); I("sink", [8]); I("woc", [128, 4, 1024]); I("woa", [64, 8, 1024]); I("bout0", [128, 8])
    I("ln1_0", [128, 2, 8]); I("mb", [128, 2, 512]); I("ident", [128, 128])
    I("wr0", [128, 8, 36]); I("brow0", [36]); I("ln2_0", [128, 2, 8]); I("sel", [64, NE, 128])
    I("w1_0", [NE, 1024, 512]); I("w3_0", [NE, 1024, 512]); I("w2_0", [NE, 512, 1024])
    I("wa", [128, 8, 576]); I("wq", [128, 8, 1280]); I("cvec1", [128, 24])
    I("bv1", [128]); I("wuq", [128, 2, 8, 128]); I("wukt", [64, 8, 128]); I("wuv", [128, 512]); I("wo", [64, 16, 1024]); I("bout1", [128, 8])
    I("ln1_1", [128, 2, 8]); I("bd64", [128, 128])
    I("tkc", [128, NK]); I("tks", [128, NK]); I("tmc", [32, NK]); I("tms", [32, NK])
    I("tqc", [128, NL]); I("tqs", [128, NL]); I("tmqc", [32, NL]); I("tmqs", [32, NL])
    I("wr1", [128, 8, 36]); I("brow1", [36]); I("ln2_1", [128, 2, 8])
    I("w1_1", [NE, 1024, 512]); I("w3_1", [NE, 1024, 512]); I("w2_1", [NE, 512, 1024])
    for nm, shp in (("modv0", [128, 48, 2]), ("modv1", [128, 48, 2]), ("xa0", [128, 8, T]), ("u2f0", [128, 8, T]),
                    ("xl0", [128, 8, SEQ]), ("xc0", [128, 8, 256]), ("xa1", [128, 8, NL]), ("u2f1", [128, 8, NL])):
        _decl(nc, d, nm, shp, None)
    _decl(nc, d, "xs1", [128, 8, NL], "ExternalOutput")
    S = Sched(nc)
    emit_ada(nc, S, dict(cin=d["cin"], adaw=d["adaw"], adab=d["adab"], modv0=d["modv0"], modv1=d["modv1"]))
    for si, sg in enumerate(("A", "B")):
        emit_l0_mixer(nc, S, NL, dict(xe=d["xe" + sg], modv=d["modv0"], win=d["win"], b128=d["b128"], b64=d["b64"], bv=d["bv"], cw=d["cw"], cvec=d["cvec0"],
                                      sink=d["sink"], woc=d["woc"], woa=d["woa"], bout=d["bout0"], ln1=d["ln1_0"], tabc=d["tabc" + sg], tabs=d["tabs" + sg],
                                      kbias=d["kbias" + sg], vmask=d["vmask" + sg], mb=d["mb"], ident=d["ident"], xa=d["xa0"], u2f=d["u2f0"]), tag=sg)
        Tm = T if si == 0 else NL
        emit_moe(nc, S, Tm, dict(u2f=d["u2f0"][:, :, 0:Tm], xa=d["xa0"][:, :, 0:Tm], modv=d["modv0"], wr=d["wr0"], brow=d["brow0"], w1=d["w1_0"], w3=d["w3_0"], w2=d["w2_0"],
                                 ln2=d["ln2_0"], ident=d["ident"], sel=d["sel"], xs=d["xl0"][:, :, si * NL:(si + 1) * NL], xsc=d["xc0"]), nctx=(256 if si == 0 else 0), tag="0" + sg)

    def xk_src(c0, n):
        if c0 < SEQ:
            return d["xl0"][:, :, c0:c0 + n]
        return d["xc0"][:, :, c0 - SEQ:c0 - SEQ + n]

    emit_l1_mixer(nc, S, NL, SEQ, dict(xk=xk_src, xo=d["xl0"][:, :, 0:NL], modv=d["modv1"], wa=d["wa"], wq=d["wq"], cvec=d["cvec1"], bv=d["bv1"], wuq=d["wuq"],
                                       wukt=d["wukt"], wuv=d["wuv"], wo=d["wo"], bout=d["bout1"], ln1=d["ln1_1"], bd64=d["bd64"],
                                       tkc=d["tkc"], tks=d["tks"], tmc=d["tmc"], tms=d["tms"], tqc=d["tqc"], tqs=d["tqs"], tmqc=d["tmqc"], tmqs=d["tmqs"],
                                       xa=d["xa1"], u2f=d["u2f1"]))
    emit_moe(nc, S, NL, dict(u2f=d["u2f1"], xa=d["xa1"], modv=d["modv1"], wr=d["wr1"], brow=d["brow1"], w1=d["w1_1"], w3=d["w3_1"], w2=d["w2_1"],
                             ln2=d["ln2_1"], ident=d["ident"], sel=d["sel"], xs=d["xs1"]), nctx=0, tag="1")
    S.close()
    return nc


def prep_l1_tables_core(h, SEQ):
    pos = np.concatenate([h * NLC + np.arange(NLC), (1 - h) * NLC + np.arange(NLC)])
    C, Sg = rope_tabs(pos, 64); Cm, Sm = rope_tabs(pos, 32)
    one = lambda r: np.ones((r, 256), np.float32); zero = lambda r: np.zeros((r, 256), np.float32)
    t2 = lambda a: np.concatenate([a, a], 0)
    return dict(tkc=np.ascontiguousarray(np.concatenate([t2(C), one(128)], 1)), tks=np.ascontiguousarray(np.concatenate([t2(Sg), zero(128)], 1)),
                tmc=np.ascontiguousarray(np.concatenate([Cm, one(32)], 1)), tms=np.ascontiguousarray(np.concatenate([Sm, zero(32)], 1)),
                tqc=np.ascontiguousarray(t2(C)[:, :NLC]), tqs=np.ascontiguousarray(t2(Sg)[:, :NLC]),
                tmqc=np.ascontiguousarray(Cm[:, :NLC]), tmqs=np.ascontiguousarray(Sm[:, :NLC]))


def build_l1():
    nc = bass.Bass("TRN2", target_bir_lowering=False)
    NL = NLC; EXT = NL + 256; NC = EXT + 256; T = NL + 256
    d = {}
    I = lambda n, s: _decl(nc, d, n, s)
    I("cin", [128, 8, 2]); I("adaw", [2, 1024, 6144]); I("adab", [128, 2, 48])
    I("xeA", [128, 8, NC]); I("tabcA", [64, NC]); I("tabsA", [64, NC]); I("kbiasA", [NC]); I("vmaskA", [128, 256])
    I("win", [128, 8, 2432]); I("b128", [128, 8]); I("b64", [64, 20]); I("bv", [128])
    I("cw", [128, 4, 31]); I("cvec0", [128, 3, 4]); I("sink", [8]); I("woc", [128, 4, 1024]); I("woa", [64, 8, 1024]); I("bout0", [128, 8])
    I("ln1_0", [128, 2, 8]); I("mb", [128, 2, 512]); I("ident", [128, 128])
    I("wr0", [128, 8, 36]); I("brow0", [36]); I("ln2_0", [128, 2, 8]); I("sel", [64, NE, 128])
    I("w1_0", [NE, 1024, 512]); I("w3_0", [NE, 1024, 512]); I("w2_0", [NE, 512, 1024])
    for nm, shp in (("modv0", [128, 48, 2]), ("xa0", [128, 8, T]), ("u2f0", [128, 8, T])):
        _decl(nc, d, nm, shp, None)
    _decl(nc, d, "modv1", [128, 48, 2], "ExternalOutput"); _decl(nc, d, "xs0", [128, 8, T], "ExternalOutput")
    S = Sched(nc)
    emit_ada(nc, S, dict(cin=d["cin"], adaw=d["adaw"], adab=d["adab"], modv0=d["modv0"], modv1=d["modv1"]))
    emit_l0_mixer(nc, S, NL, dict(xe=d["xeA"], modv=d["modv0"], win=d["win"], b128=d["b128"], b64=d["b64"], bv=d["bv"], cw=d["cw"], cvec=d["cvec0"],
                                  sink=d["sink"], woc=d["woc"], woa=d["woa"], bout=d["bout0"], ln1=d["ln1_0"], tabc=d["tabcA"], tabs=d["tabsA"],
                                  kbias=d["kbiasA"], vmask=d["vmaskA"], mb=d["mb"], ident=d["ident"], xa=d["xa0"], u2f=d["u2f0"]), tag="A")
    emit_moe(nc, S, T, dict(u2f=d["u2f0"], xa=d["xa0"], modv=d["modv0"], wr=d["wr0"], brow=d["brow0"], w1=d["w1_0"], w3=d["w3_0"], w2=d["w2_0"],
                            ln2=d["ln2_0"], ident=d["ident"], sel=d["sel"], xs=d["xs0"][:, :, 0:NL], xsc=d["xs0"][:, :, NL:T]), nctx=256, tag="0A")
    S.close()
    return nc


def build_l2():
    nc = bass.Bass("TRN2", target_bir_lowering=False)
    NL = NLC; SEQ = SEQ_FULL; NK = SEQ + 256
    d = {}
    I = lambda n, s: _decl(nc, d, n, s)
    I("modv1", [128, 48, 2]); I("xl0", [128, 8, SEQ]); I("xc0", [128, 8, 256])
    I("wa", [128, 8, 576]); I("wq", [128, 8, 1280]); I("cvec1", [128, 24])
    I("bv1", [128]); I("wuq", [128, 2, 8, 128]); I("wukt", [64, 8, 128]); I("wuv", [128, 512]); I("wo", [64, 16, 1024]); I("bout1", [128, 8])
    I("ln1_1", [128, 2, 8]); I("bd64", [128, 128])
    I("tkc", [128, NK]); I("tks", [128, NK]); I("tmc", [32, NK]); I("tms", [32, NK])
    I("tqc", [128, NL]); I("tqs", [128, NL]); I("tmqc", [32, NL]); I("tmqs", [32, NL])
    I("wr1", [128, 8, 36]); I("brow1", [36]); I("ln2_1", [128, 2, 8]); I("sel", [64, NE, 128]); I("ident", [128, 128])
    I("w1_1", [NE, 1024, 512]); I("w3_1", [NE, 1024, 512]); I("w2_1", [NE, 512, 1024])
    _decl(nc, d, "xa1", [128, 8, NL], None); _decl(nc, d, "u2f1", [128, 8, NL], None)
    _decl(nc, d, "xs1", [128, 8, NL], "ExternalOutput")
    S = Sched(nc)

    def xk_src(c0, n):
        if c0 < SEQ:
            return d["xl0"][:, :, c0:c0 + n]
        return d["xc0"][:, :, c0 - SEQ:c0 - SEQ + n]

    emit_l1_mixer(nc, S, NL, SEQ, dict(xk=xk_src, xo=d["xl0"][:, :, 0:NL], modv=d["modv1"], wa=d["wa"], wq=d["wq"], cvec=d["cvec1"], bv=d["bv1"], wuq=d["wuq"],
                                       wukt=d["wukt"], wuv=d["wuv"], wo=d["wo"], bout=d["bout1"], ln1=d["ln1_1"], bd64=d["bd64"],
                                       tkc=d["tkc"], tks=d["tks"], tmc=d["tmc"], tms=d["tms"], tqc=d["tqc"], tqs=d["tqs"], tmqc=d["tmqc"], tmqs=d["tmqs"],
                                       xa=d["xa1"], u2f=d["u2f1"]))
    emit_moe(nc, S, NL, dict(u2f=d["u2f1"], xa=d["xa1"], modv=d["modv1"], wr=d["wr1"], brow=d["brow1"], w1=d["w1_1"], w3=d["w3_1"], w2=d["w2_1"],
                             ln2=d["ln2_1"], ident=d["ident"], sel=d["sel"], xs=d["xs1"]), nctx=0, tag="1")
    S.close()
    return nc


def kernel(**inputs):
    p = {k: np.asarray(v) for k, v in inputs.items()}
    x = p["x"]; ctx = p["ctx"]; c = p["c"]; c_ctx = p["c_ctx"]
    B = x.shape[0]
    ncores = 8
    W0 = prep_l0_weights(p); M0 = prep_moe(p, 0)
    sel = make_sel()
    adab = np.ascontiguousarray(p["ada_b"].reshape(2, 48, 128).transpose(2, 0, 1))
    in_maps = []
    for core in range(ncores):
        b, h = core // 2, core % 2
        m = dict(W0); m.update(M0)
        for k, v in prep_l0_core(h, NLC, SEQ_FULL, x[b], ctx[b]).items():
            m[k + "A"] = v
        m["cin"] = np.ascontiguousarray(np.stack([pv(c[b]), pv(c_ctx)], -1))
        m["adaw"] = p["ada_w"]; m["adab"] = adab; m["sel"] = sel
        in_maps.append(m)
    if "l1" not in _PROG:
        _PROG["l1"] = build_l1()
    r1 = run_bass_kernel_spmd(_PROG["l1"], in_maps, core_ids=list(range(ncores))).results
    W1 = prep_l1_weights(p); M1 = prep_moe(p, 1)
    tabs1 = [prep_l1_tables_core(h, SEQ_FULL) for h in range(2)]
    in_maps2 = []
    for core in range(ncores):
        b, h = core // 2, core % 2
        own = r1[core]["xs0"]; oth = r1[2 * b + (1 - h)]["xs0"]
        m = dict(W1); m.update(M1); m.update(tabs1[h])
        m["xl0"] = np.ascontiguousarray(np.concatenate([own[:, :, :NLC], oth[:, :, :NLC]], axis=2))
        m["xc0"] = np.ascontiguousarray(own[:, :, NLC:])
        m["modv1"] = r1[core]["modv1"]; m["sel"] = sel; m["ident"] = W0["ident"]
        in_maps2.append(m)
    if "l2" not in _PROG:
        _PROG["l2"] = build_l2()
    r2 = run_bass_kernel_spmd(_PROG["l2"], in_maps2, core_ids=list(range(ncores))).results
    out = np.zeros((B, SEQ_FULL, D_MODEL), np.float32)
    for core in range(ncores):
        b, h = core // 2, core % 2
        out[b, h * NLC:(h + 1) * NLC, :] = unfm(r2[core]["xs1"])
    return out
```
